# Optimizing a Trainium2 kernel written in Bass

```python
import math
import jax, jax.numpy as jnp
from jax import lax
import numpy as np

D_MODEL = 4096
BATCH = 4
SEQ = 2048
DEPTH = 1

EPS = 1e-6
ML_HEADS = 8
ML_QK = D_MODEL // 32
ML_V = D_MODEL // 16
ML_QK_W = ML_HEADS * ML_QK
ML_V_W = ML_HEADS * ML_V
ML_CHUNK = 128
CONV_W = 4
SB_HEADS = 16
SB_HD = D_MODEL // 32
SB_W = SB_HEADS * SB_HD
SB_BLOCK = 128
N_MEM = 256
XA_HEADS = 4
XA_HD = D_MODEL // XA_HEADS
N_GROUPS = 8
EXPERTS_PER_GROUP = 8
N_EXPERTS = N_GROUPS * EXPERTS_PER_GROUP
TOP_K = 2
D_EXPERT = D_MODEL // 8
MOE_BLOCK = 128
IN_SIZES = (ML_QK_W, ML_QK_W, ML_V_W, ML_V_W, ML_HEADS, ML_HEADS, SB_W, SB_W, SB_W, D_MODEL, D_MODEL)
IN_TOTAL = 2 * ML_QK_W + 2 * ML_V_W + 2 * ML_HEADS + 3 * SB_W + 2 * D_MODEL

kernel_name = "hybrid_mlstm_stickbreak_hmoe_block"


def rmsnorm(x, g):
    xf = x.astype(jnp.float32)
    y = xf * lax.rsqrt(jnp.mean(xf * xf, axis=-1, keepdims=True) + EPS)
    return (y * g.astype(jnp.float32)).astype(x.dtype)


def causal_depthwise_conv(x, w):
    c = x.shape[-1]
    return lax.conv_general_dilated(
        x, w[:, None, :].astype(x.dtype), window_strides=(1,),
        padding=[(CONV_W - 1, 0)], dimension_numbers=('NWC', 'WIO', 'NWC'),
        feature_group_count=c)


def to_heads(a, n_heads):
    b, s, w = a.shape
    return a.reshape(b, s, n_heads, w // n_heads).transpose(0, 2, 1, 3)


def mlstm_chunkwise(q, k, v, i_pre, f_pre):
    b_, h_, s_, dk = q.shape
    dv = v.shape[-1]
    nc = s_ // ML_CHUNK
    qf = q.astype(jnp.float32)
    kf = k.astype(jnp.float32) / math.sqrt(dk)
    vf = v.astype(jnp.float32)
    logi = i_pre.astype(jnp.float32)
    logf = jax.nn.log_sigmoid(f_pre.astype(jnp.float32))

    def to_chunks(a):
        a = a.reshape(a.shape[:2] + (nc, ML_CHUNK) + a.shape[3:])
        return jnp.moveaxis(a, 2, 0)

    xs = tuple(to_chunks(a) for a in (qf, kf, vf, logi, logf))
    causal = jnp.tril(jnp.ones((ML_CHUNK, ML_CHUNK), dtype=bool))

    def step(carry, chunk):
        c_st, n_st, m_st = carry
        qb, kb, vb, ib, fb = chunk
        bcum = jnp.cumsum(fb, axis=-1)
        g = bcum[..., -1]
        d_intra = bcum[..., :, None] - bcum[..., None, :] + ib[..., None, :]
        d_intra = jnp.where(causal, d_intra, -jnp.inf)
        inter = bcum + m_st[..., None]
        m_t = jnp.maximum(inter, jnp.max(d_intra, axis=-1))
        w_intra = jnp.exp(d_intra - m_t[..., None])
        w_inter = jnp.exp(inter - m_t)
        sc = jnp.einsum('bhtd,bhsd->bhts', qb, kb) * w_intra
        num = (w_inter[..., None] * jnp.einsum('bhtd,bhde->bhte', qb, c_st)
               + jnp.einsum('bhts,bhse->bhte', sc, vb))
        den = w_inter * jnp.einsum('bhtd,bhd->bht', qb, n_st) + jnp.sum(sc, axis=-1)
        h = num / jnp.maximum(jnp.abs(den), jnp.exp(-m_t))[..., None]
        d_state = g[..., None] - bcum + ib
        m_new = jnp.maximum(g + m_st, jnp.max(d_state, axis=-1))
        w_s = jnp.exp(d_state - m_new[..., None])
        decay = jnp.exp(g + m_st - m_new)
        c_new = decay[..., None, None] * c_st + jnp.einsum('bhs,bhsd,bhse->bhde', w_s, kb, vb)
        n_new = decay[..., None] * n_st + jnp.einsum('bhs,bhsd->bhd', w_s, kb)
        return (c_new, n_new, m_new), h

    init = (jnp.zeros((b_, h_, dk, dv), jnp.float32),
            jnp.zeros((b_, h_, dk), jnp.float32),
            jnp.full((b_, h_), -1e30, jnp.float32))
    _, hs = lax.scan(step, init, xs)
    return jnp.moveaxis(hs, 0, 2).reshape(b_, h_, s_, dv)


def stick_breaking_attention(q, k, v):
    s_ = q.shape[2]
    scale = 1.0 / math.sqrt(q.shape[-1])
    outs = []
    for blk in range(s_ // SB_BLOCK):
        t0 = blk * SB_BLOCK
        kv_len = t0 + SB_BLOCK
        qb = q[:, :, t0:kv_len]
        kb = k[:, :, :kv_len]
        vb = v[:, :, :kv_len]
        z = jnp.einsum('bhtd,bhsd->bhts', qb, kb).astype(jnp.float32) * scale
        t_idx = t0 + jnp.arange(SB_BLOCK)[:, None]
        s_idx = jnp.arange(kv_len)[None, :]
        strict = s_idx < t_idx
        log_1m = jnp.where(strict, jax.nn.log_sigmoid(-z), 0.0)
        rest = lax.cumsum(log_1m, axis=3, reverse=True) - log_1m
        a = jnp.where(strict, jnp.exp(jax.nn.log_sigmoid(z) + rest), 0.0)
        outs.append(jnp.einsum('bhts,bhsd->bhtd', a.astype(vb.dtype), vb))
    return jnp.concatenate(outs, axis=2)


def hybrid_mixer(xn, w_in, conv_qk, b_gates, g_mlstm, w_proj_a, w_proj_b, w_out):
    b_, s_, _ = xn.shape
    proj = xn @ w_in
    split_pts = np.cumsum(IN_SIZES)[:-1].tolist()
    q_m, k_m, v_m, o_m, i_m, f_m, q_s, k_s, v_s, gate_a, gate_b = jnp.split(proj, split_pts, axis=-1)
    qk = jax.nn.silu(causal_depthwise_conv(jnp.concatenate([q_m, k_m], axis=-1), conv_qk))
    q_m, k_m = jnp.split(qk, [ML_QK_W], axis=-1)
    i_pre = (i_m + b_gates[:ML_HEADS].astype(i_m.dtype)).transpose(0, 2, 1)
    f_pre = (f_m + b_gates[ML_HEADS:].astype(f_m.dtype)).transpose(0, 2, 1)
    hm = mlstm_chunkwise(to_heads(q_m, ML_HEADS), to_heads(k_m, ML_HEADS),
                         to_heads(v_m, ML_HEADS), i_pre, f_pre)
    hm = hm * lax.rsqrt(jnp.mean(hm * hm, axis=-1, keepdims=True) + EPS)
    hm = hm.transpose(0, 2, 1, 3).reshape(b_, s_, ML_V_W) * g_mlstm.astype(jnp.float32)
    hm = (jax.nn.sigmoid(o_m.astype(jnp.float32)) * hm).astype(xn.dtype)
    y_a = hm @ w_proj_a
    hs = stick_breaking_attention(to_heads(q_s, SB_HEADS), to_heads(k_s, SB_HEADS), to_heads(v_s, SB_HEADS))
    y_b = hs.transpose(0, 2, 1, 3).reshape(b_, s_, SB_W) @ w_proj_b
    y = jax.nn.sigmoid(gate_a) * y_a + jax.nn.sigmoid(gate_b) * y_b
    return y @ w_out


def memory_cross_attention(hn, mem, g_mem, w_q, w_kv, w_o):
    b_, s_, _ = hn.shape
    memn = rmsnorm(mem, g_mem)
    q = (hn @ w_q).reshape(b_, s_, XA_HEADS, XA_HD)
    k, v = jnp.split(memn @ w_kv, 2, axis=-1)
    k = k.reshape(b_, N_MEM, XA_HEADS, XA_HD)
    v = v.reshape(b_, N_MEM, XA_HEADS, XA_HD)
    sc = jnp.einsum('bshd,bmhd->bhsm', q, k).astype(jnp.float32) / math.sqrt(XA_HD)
    p = jax.nn.softmax(sc, axis=-1)
    o = jnp.einsum('bhsm,bmhd->bshd', p.astype(v.dtype), v).reshape(b_, s_, D_MODEL)
    return o @ w_o


def hierarchical_moe(hn, w_router_group, b_router_group, w_router_expert, b_router_expert,
                     w_gate, w_up, w_down):
    b_, s_, d_ = hn.shape
    t_ = b_ * s_
    xt = hn.reshape(t_, d_)
    g_logits = (xt @ w_router_group).astype(jnp.float32) + b_router_group.astype(jnp.float32)
    g_prob = jax.nn.softmax(g_logits, axis=-1)
    g_w, g_idx = lax.top_k(g_prob, 1)
    e_logits = ((xt @ w_router_expert).astype(jnp.float32)
                + b_router_expert.astype(jnp.float32)).reshape(t_, N_GROUPS, EXPERTS_PER_GROUP)
    e_sel = e_logits[jnp.arange(t_), g_idx[:, 0]]
    e_w, e_idx = lax.top_k(jax.nn.softmax(e_sel, axis=-1), TOP_K)
    e_w = e_w / jnp.sum(e_w, axis=-1, keepdims=True)
    weights = g_w * e_w
    expert_id = g_idx * EXPERTS_PER_GROUP + e_idx
    n_assign = t_ * TOP_K
    flat_e = expert_id.reshape(n_assign).astype(jnp.int32)
    flat_tok = jnp.repeat(jnp.arange(t_, dtype=jnp.int32), TOP_K)
    flat_w = weights.reshape(n_assign)
    order = jnp.argsort(flat_e)
    se, stok, sw = flat_e[order], flat_tok[order], flat_w[order]
    counts = jax.ops.segment_sum(jnp.ones_like(se), se, num_segments=N_EXPERTS)
    starts = jnp.cumsum(counts) - counts
    padded = (counts + MOE_BLOCK - 1) // MOE_BLOCK * MOE_BLOCK
    pad_ends = jnp.cumsum(padded)
    pad_starts = pad_ends - padded
    dest = pad_starts[se] + (jnp.arange(n_assign, dtype=jnp.int32) - starts[se])
    n_blocks = (n_assign + MOE_BLOCK - 1) // MOE_BLOCK + N_EXPERTS
    n_rows = n_blocks * MOE_BLOCK
    buf_tok = jnp.zeros((n_rows,), jnp.int32).at[dest].set(stok)
    buf_w = jnp.zeros((n_rows,), xt.dtype).at[dest].set(sw.astype(xt.dtype))
    block_start = jnp.arange(n_blocks, dtype=jnp.int32) * MOE_BLOCK
    block_expert = jnp.minimum(jnp.searchsorted(pad_ends, block_start, side='right'),
                               N_EXPERTS - 1).astype(jnp.int32)

    def expert_block(args):
        tok, w, e = args
        xb = xt[tok]
        hb = jax.nn.silu(xb @ w_gate[e]) * (xb @ w_up[e])
        return (hb @ w_down[e]) * w[:, None]

    yb = lax.map(expert_block, (buf_tok.reshape(n_blocks, MOE_BLOCK),
                                buf_w.reshape(n_blocks, MOE_BLOCK), block_expert))
    y = jnp.zeros((t_, d_), xt.dtype).at[buf_tok].add(yb.reshape(n_rows, d_))
    return y.reshape(b_, s_, d_)


def setup_inputs(seed: int = 0) -> dict:
    key = jax.random.key(seed)
    ks = jax.random.split(key, 24)
    f32 = jnp.float32

    def nrm(k, shape, scale):
        return jax.random.normal(k, shape, f32) * scale

    def gain(k, shape):
        return 1.0 + 0.02 * jax.random.normal(k, shape, f32)

    dsc = D_MODEL ** -0.5
    b_if = jnp.concatenate([
        0.1 * jax.random.normal(ks[4], (DEPTH, ML_HEADS), f32),
        jnp.broadcast_to(jnp.linspace(3.0, 6.0, ML_HEADS, dtype=f32), (DEPTH, ML_HEADS))
        + 0.1 * jax.random.normal(ks[5], (DEPTH, ML_HEADS), f32)], axis=-1)
    return {
        "x": nrm(ks[0], (BATCH, SEQ, D_MODEL), 1.0),
        "mem": nrm(ks[1], (BATCH, N_MEM, D_MODEL), 1.0),
        "norm_mix": gain(ks[2], (DEPTH, D_MODEL)),
        "w_in": nrm(ks[3], (DEPTH, D_MODEL, IN_TOTAL), dsc),
        "conv_qk": nrm(ks[6], (DEPTH, CONV_W, 2 * ML_QK_W), CONV_W ** -0.5),
        "b_gates": b_if,
        "g_mlstm": gain(ks[7], (DEPTH, ML_V_W)),
        "w_proj_a": nrm(ks[8], (DEPTH, ML_V_W, D_MODEL), ML_V_W ** -0.5),
        "w_proj_b": nrm(ks[9], (DEPTH, SB_W, D_MODEL), SB_W ** -0.5),
        "w_out": nrm(ks[10], (DEPTH, D_MODEL, D_MODEL), dsc),
        "norm_xattn": gain(ks[11], (DEPTH, D_MODEL)),
        "norm_mem": gain(ks[12], (DEPTH, D_MODEL)),
        "w_q_mem": nrm(ks[13], (DEPTH, D_MODEL, D_MODEL), dsc),
        "w_kv_mem": nrm(ks[14], (DEPTH, D_MODEL, 2 * D_MODEL), dsc),
        "w_o_mem": nrm(ks[15], (DEPTH, D_MODEL, D_MODEL), dsc),
        "norm_moe": gain(ks[16], (DEPTH, D_MODEL)),
        "w_router_group": nrm(ks[17], (DEPTH, D_MODEL, N_GROUPS), dsc),
        "b_router_group": nrm(ks[18], (DEPTH, N_GROUPS), 0.01),
        "w_router_expert": nrm(ks[19], (DEPTH, D_MODEL, N_EXPERTS), dsc),
        "b_router_expert": nrm(ks[20], (DEPTH, N_EXPERTS), 0.01),
        "w_gate": nrm(ks[21], (DEPTH, N_EXPERTS, D_MODEL, D_EXPERT), dsc),
        "w_up": nrm(ks[22], (DEPTH, N_EXPERTS, D_MODEL, D_EXPERT), dsc),
        "w_down": nrm(ks[23], (DEPTH, N_EXPERTS, D_EXPERT, D_MODEL), D_EXPERT ** -0.5),
        "norm_final": gain(jax.random.fold_in(key, 99), (D_MODEL,)),
    }


def reference(x, mem, norm_mix, w_in, conv_qk, b_gates, g_mlstm, w_proj_a, w_proj_b, w_out,
              norm_xattn, norm_mem, w_q_mem, w_kv_mem, w_o_mem, norm_moe,
              w_router_group, b_router_group, w_router_expert, b_router_expert,
              w_gate, w_up, w_down, norm_final):
    h = x
    for l in range(DEPTH):
        h = h + hybrid_mixer(rmsnorm(h, norm_mix[l]), w_in[l], conv_qk[l], b_gates[l],
                             g_mlstm[l], w_proj_a[l], w_proj_b[l], w_out[l])
        h = h + memory_cross_attention(rmsnorm(h, norm_xattn[l]), mem, norm_mem[l],
                                       w_q_mem[l], w_kv_mem[l], w_o_mem[l])
        h = h + hierarchical_moe(rmsnorm(h, norm_moe[l]), w_router_group[l], b_router_group[l],
                                 w_router_expert[l], b_router_expert[l],
                                 w_gate[l], w_up[l], w_down[l])
    return rmsnorm(h, norm_final)
```

```python
import numpy as np
from contextlib import ExitStack
import concourse.bass as bass
import concourse.mybir as mybir
from concourse.bass_utils import run_bass_kernel_spmd

F32 = mybir.dt.float32
F32R = mybir.dt.float32r
I32 = mybir.dt.int32
ALU = mybir.AluOpType
AF = mybir.ActivationFunctionType
AX = mybir.AxisListType

ENGS = ("pe", "act", "dve", "pool", "sp")
NDMASEM = {"sp": 24, "pool": 8, "act": 8}


def _region(ap):
    t = ap.tensor
    shape = list(t.shape)
    rowsize = 1
    for s in shape[1:]:
        rowsize *= int(s)
    off = int(ap.offset)
    r_lo, c_lo = off // rowsize, off % rowsize
    r_ext, c_ext = 0, 0
    for step, cnt in ap.ap:
        step, cnt = int(step), int(cnt)
        if cnt <= 1 or step == 0:
            continue
        if step % rowsize == 0:
            r_ext += (cnt - 1) * (step // rowsize)
        else:
            c_ext += (cnt - 1) * abs(step)
    return (t.name, r_lo, r_lo + r_ext + 1, c_lo, c_lo + c_ext + 1)


def _overlap(a, b):
    return a[1] < b[2] and b[1] < a[2] and a[3] < b[4] and b[3] < a[4]


def _covers(a, b):
    return a[1] <= b[1] and a[2] >= b[2] and a[3] <= b[3] and a[4] >= b[4]


class K:
    def __init__(self):
        self.nc = bass.Bass("TRN2", target_bir_lowering=False)
        self.es = ExitStack()
        self.streams = {e: [] for e in ENGS}
        self.cnt = {e: 0 for e in ENGS}
        self.esem = {}
        for e in ("pe", "act", "dve", "pool"):
            self.esem[e] = self.es.enter_context(self.nc.semaphore("sem_" + e))
        self.dsem = {}
        self.dcnt = {}
        self.dlast = {}
        for q, n in NDMASEM.items():
            self.dsem[q] = [self.es.enter_context(self.nc.semaphore("dsem_%s_%d" % (q, i))) for i in range(n)]
            self.dcnt[q] = 0
        self.track = {}
        self.waited = {e: {} for e in ENGS}
        self.n_wait = 0
        self.out_tokens = []

    def sbuf(self, name, shape, dt=F32):
        return self.es.enter_context(self.nc.sbuf_tensor(name, list(shape), dt))

    def psum(self, name, shape, dt=F32):
        return self.es.enter_context(self.nc.psum_tensor(name, list(shape), dt))

    def dram(self, name, shape, dt=F32, kind="Internal"):
        return self.nc.dram_tensor(name, list(shape), dt, kind=kind).ap()

    def _semkey(self, tok):
        if tok[0] == "c":
            return ("c", tok[1]), tok[2]
        return ("d", tok[1], tok[2]), tok[3]

    def _need_wait(self, eng, tok, kind_raw, is_dma=False):
        if tok[0] == "c" and tok[1] == eng and not is_dma:
            if eng == "pe":
                return False
            if not kind_raw:
                return False
        key, val = self._semkey(tok)
        return self.waited[eng].get(key, 0) < val

    def _add_wait(self, eng, tok, waits):
        key, val = self._semkey(tok)
        if self.waited[eng].get(key, 0) >= val:
            return
        self.waited[eng][key] = val
        if tok[0] == "c":
            sem = self.esem[tok[1]]
        else:
            sem = self.dsem[tok[1]][tok[2]]
        waits.append((sem, val))

    def _deps(self, eng, reads, writes, is_dma=False):
        waits = []
        for r in reads:
            for ent in self.track.get(r[0], ()):
                if ent[1] == "w" and _overlap(ent[0], r):
                    if self._need_wait(eng, ent[2], True, is_dma):
                        self._add_wait(eng, ent[2], waits)
        for w in writes:
            for ent in self.track.get(w[0], ()):
                if _overlap(ent[0], w):
                    if self._need_wait(eng, ent[2], False, is_dma):
                        self._add_wait(eng, ent[2], waits)
        return waits

    def _record(self, reads, writes, tok):
        for w in writes:
            lst = self.track.setdefault(w[0], [])
            lst[:] = [e for e in lst if not _covers(w, e[0])]
            lst.append([w, "w", tok])
        for r in reads:
            lst = self.track.setdefault(r[0], [])
            if tok[0] == "c":
                lst[:] = [e for e in lst if not (e[1] == "r" and e[2][0] == "c" and e[2][1] == tok[1] and _covers(r, e[0]))]
            lst.append([r, "r", tok])

    def op(self, eng, meth, writes, reads, *args, **kw):
        wr = [_region(a) for a in writes]
        rr = [_region(a) for a in reads]
        waits = self._deps(eng, rr, wr)
        self.cnt[eng] += 1
        tok = ("c", eng, self.cnt[eng])
        self._record(rr, wr, tok)
        self.streams[eng].append((waits, meth, args, kw, (self.esem[eng], 1)))
        self.n_wait += len(waits)
        return tok

    def dma(self, out, in_, q="sp", extra_reads=(), **kw):
        wr = [_region(out)]
        rr = [_region(in_)] + [_region(a) for a in extra_reads]
        waits = self._deps(q, rr, wr, True)
        i = self.dcnt[q]
        n = len(self.dsem[q])
        slot, gen = i % n, i // n
        if gen > 0:
            self._add_wait(q, ("d", q, slot, 16 * gen), waits)
        self.dcnt[q] += 1
        tok = ("d", q, slot, 16 * (gen + 1))
        self._record(rr, wr, tok)
        meth = kw.pop("_meth", "dma_start")
        self.streams[q].append((waits, meth, (), dict(out=out, in_=in_, **kw), (self.dsem[q][slot], 16)))
        return tok

    def wait_tok(self, eng, tok):
        waits = []
        self._add_wait(eng, tok, waits)
        if waits:
            self.streams[eng].append((waits, None, (), {}, None))

    def barrier(self):
        toks = []
        for e in ("pe", "act", "dve", "pool"):
            if self.cnt[e] > 0:
                toks.append(("c", e, self.cnt[e]))
        for q in self.dsem:
            n = len(self.dsem[q])
            for i in range(max(0, self.dcnt[q] - n), self.dcnt[q]):
                toks.append(("d", q, i % n, 16 * (i // n + 1)))
        for e in ENGS:
            for t in toks:
                if t[0] == "c" and t[1] == e:
                    continue
                self.wait_tok(e, t)
        self.track = {}

    def mm(self, out, lhsT, rhs, start=True, stop=True, **kw):
        return self.op("pe", "matmul", [out], [lhsT, rhs], out, lhsT, rhs, start=start, stop=stop, **kw)

    def transpose(self, out, in_, ident):
        return self.op("pe", "transpose", [out], [in_, ident], out, in_, ident)

    def act(self, out, in_, func, bias=None, scale=None, accum_out=None, eng="act"):
        reads = [in_]
        kw = {}
        if bias is not None:
            kw["bias"] = bias
            if not isinstance(bias, (int, float)):
                reads.append(bias)
        if scale is not None:
            kw["scale"] = scale
            if not isinstance(scale, (int, float)):
                reads.append(scale)
        writes = [out]
        if accum_out is not None:
            kw["accum_out"] = accum_out
            writes.append(accum_out)
        return self.op(eng, "activation", writes, reads, out, in_, func, **kw)

    def tt(self, out, in0, in1, op, eng="dve"):
        return self.op(eng, "tensor_tensor", [out], [in0, in1], out, in0, in1, op)

    def ts(self, out, in0, s1, s2=None, op0=ALU.mult, op1=None, eng="dve", accum_out=None):
        reads = [in0]
        if not isinstance(s1, (int, float)):
            reads.append(s1)
        if s2 is not None and not isinstance(s2, (int, float)):
            reads.append(s2)
        kw = {}
        if op1 is not None:
            kw["op1"] = op1
        writes = [out]
        if accum_out is not None:
            kw["accum_out"] = accum_out
            writes.append(accum_out)
        return self.op(eng, "tensor_scalar", writes, reads, out, in0, s1, s2, op0, **kw)

    def stt(self, out, in0, scalar, in1, op0, op1, eng="dve"):
        reads = [in0, in1]
        if not isinstance(scalar, (int, float)):
            reads.append(scalar)
        return self.op(eng, "scalar_tensor_tensor", [out], reads, out, in0, scalar, in1, op0, op1)

    def copy(self, out, in_, eng="dve"):
        return self.op(eng, "tensor_copy", [out], [in_], out, in_)

    def memset(self, out, val, eng="dve"):
        return self.op(eng, "memset", [out], [], out, val)

    def reduce(self, out, in_, op, axis=AX.X, eng="dve"):
        return self.op(eng, "tensor_reduce", [out], [in_], out, in_, axis, op)

    def finish(self):
        nc = self.nc
        eng_obj = {"pe": "tensor", "act": "scalar", "dve": "vector", "pool": "gpsimd", "sp": "sync"}
        self.barrier()
        with nc.Block() as block:
            for e in ENGS:
                items = self.streams[e]

                def body(eo, items=items):
                    for waits, meth, args, kw, inc in items:
                        for sem, val in waits:
                            eo.wait_ge(sem, val)
                        if meth is None:
                            continue
                        ins = getattr(eo, meth)(*args, **kw)
                        if inc is not None:
                            ins.then_inc(inc[0], inc[1])
                getattr(block, eng_obj[e])(body)
        return nc

    def stats(self):
        return {e: len(self.streams[e]) for e in ENGS}, self.n_wait

import math
D = 4096
T = 1024
S = 2048
EPS = 1e-6
C_QM, C_KM, C_VM, C_OM, C_I, C_F, C_QS, C_KS, C_VS, C_GA, C_GB = 0, 1024, 2048, 4096, 6144, 6152, 6160, 8208, 10256, 12304, 16400
SB_SCALE = 1.0 / math.sqrt(128.0)
LN_SQRT_DK = 0.5 * math.log(128.0)
NEXP = 64
CAP = 128


def build(upto=99, dbg=()):
    k = K()

    def ext(n, shp, dt=F32):
        return k.dram(n, shp, dt, kind="ExternalInput")

    def scr(n, shp, dt=F32):
        return k.dram(n, shp, dt, kind=("ExternalOutput" if n in dbg else "Internal"))

    xs = ext("xs", [S, D]); memx = ext("mem", [256, D])
    w_in = ext("w_in", [D, 20496]); w_pa = ext("w_proj_a", [2048, D]); w_pb = ext("w_proj_b", [2048, D])
    w_out = ext("w_out", [D, D]); w_q = ext("w_q_mem", [D, D]); w_kv = ext("w_kv_mem", [D, 2 * D]); w_o = ext("w_o_mem", [D, D])
    if upto >= 6:
        w_g = ext("w_gate", [NEXP * D, 512]); w_u = ext("w_up", [NEXP * D, 512]); w_d = ext("w_down", [NEXP * 512, D])
    w_r = ext("w_r", [D, 72]); b_r = ext("b_r", [128, 72])
    g_mix = ext("g_mix", [128, 32]); g_x = ext("g_x", [128, 32]); g_mem = ext("g_mem", [128, 32]); g_moe = ext("g_moe", [128, 32])
    g_fin = ext("g_fin", [128, D]); g_ml = ext("g_ml", [128, 16])
    convT = ext("convT", [2048, 4]); b_i = ext("b_i", [8, 1]); b_f = ext("b_f", [8, 1])
    c_ident = ext("c_ident", [128, 128]); c_ones = ext("c_ones", [128, 128]); c_ut = ext("c_ut", [128, 128]); c_lt = ext("c_lt", [128, 128])
    c_mm = ext("c_mm", [128, 896]); c_ms = ext("c_ms", [128, 896])
    c_ltpos = ext("c_ltpos", [128, 128]); c_iota = ext("c_iota", [128, 128]); c_erow = ext("c_erow", [128, 64])
    out = k.dram("out", [T, D], F32, kind="ExternalOutput")

    QKm = scr("QKm", [2048, S]); Vm = scr("Vm", [S, 2048]); Om = scr("Om", [2048, T])
    I8d = scr("I8d", [8, S]); F8d = scr("F8d", [8, S])
    Qs = scr("Qs", [2048, T]); Ks = scr("Ks", [2048, S]); Vs = scr("Vs", [S, 2048])
    GA = scr("GA", [D, T]); GB = scr("GB", [D, T])
    HM = scr("HM", [2048, T]); HS = scr("HS", [2048, T])
    H1 = scr("H1", [T, D]); H2 = scr("H2", [T, D])
    KX = scr("KX", [D, 256]); VX = scr("VX", [256, D]); QX = scr("QX", [D, T])
    XH = scr("XH", [T, D]); YB = scr("YB", [NEXP * CAP, D])

    AR = k.sbuf("AR", [128, 16384], F32R); WR = k.sbuf("WR", [128, 6144], F32R); G = k.sbuf("G", [128, 26624])
    WS = [G[:, i * 2048:(i + 1) * 2048] for i in range(3)]
    WRs = [WR[:, i * 2048:(i + 1) * 2048] for i in range(3)]
    Eb = [G[:, 6144 + i * 2048:6144 + (i + 1) * 2048] for i in range(2)]
    OV0 = 10240

    def ov(a, b_):
        return G[:, OV0 + a:OV0 + b_]

    def ovr(r0, r1, a, b_):
        return G[r0:r1, OV0 + a:OV0 + b_]
    Xb = [ov(0, 4096), ov(4096, 8192)]
    sqjunk = ov(8192, 12288)
    ident = k.sbuf("ident", [128, 128]); ones = k.sbuf("ones", [128, 128]); ut = k.sbuf("ut", [128, 128]); lt = k.sbuf("lt", [128, 128])
    mmk = k.sbuf("mmk", [128, 896]); msk = k.sbuf("msk", [128, 896])
    gmix = k.sbuf("gmix", [128, 32]); gx = k.sbuf("gx", [128, 32]); gmem = k.sbuf("gmem", [128, 32]); gmoe = k.sbuf("gmoe", [128, 32]); gml = k.sbuf("gml", [128, 16])
    st = k.sbuf("st", [128, 64])
    wif = k.sbuf("wif", [128, 32, 16])
    biasT = k.sbuf("biasT", [128, 128])
    ps = [k.psum("ps%d" % i, [128, 512]) for i in range(8)]

    for dst, src in ((ident, c_ident), (ones, c_ones), (ut, c_ut), (lt, c_lt), (mmk, c_mm), (msk, c_ms),
                     (gmix, g_mix), (gx, g_x), (gmem, g_mem), (gmoe, g_moe), (gml, g_ml)):
        k.dma(dst[:], src)
    k.dma(wif[:], w_in[:, C_I:C_I + 16].rearrange("(kc p) c -> p kc c", p=128))

    cnt = {"w": 0, "e": 0, "bank": 0, "x": 0, "t": 0, "alt": 0}

    def load_w(w2d):
        nk = w2d.shape[0] // 128
        nc_ = w2d.shape[1]
        i = cnt["w"] % 3
        cnt["w"] += 1
        sv = WS[i][:, 0:nk * nc_].rearrange("p (kc c) -> p kc c", c=nc_)
        rv = WRs[i][:, 0:nk * nc_].rearrange("p (kc c) -> p kc c", c=nc_)
        k.dma(sv, w2d.rearrange("(kc p) c -> p kc c", p=128))
        h = max(1, nk // 2)
        k.copy(rv[:, 0:h, :], sv[:, 0:h, :])
        if h < nk:
            k.act(rv[:, h:nk, :], sv[:, h:nk, :], AF.Copy)
        return rv

    def bankset():
        b = cnt["bank"] % 2
        cnt["bank"] += 1
        return [ps[b * 4 + i] for i in range(4)]

    def ebuf():
        e = Eb[cnt["e"] % 2]
        cnt["e"] += 1
        return e

    def evac(out_ap, in_ap, func=None):
        if func is not None:
            k.act(out_ap, in_ap, func)
            return
        cnt["alt"] += 1
        if cnt["alt"] % 2:
            k.act(out_ap, in_ap, AF.Copy)
        else:
            k.copy(out_ap, in_ap)

    def rstd_from_ss(ss, rs, scale, tmp):
        k.ts(tmp, ss, scale, EPS, op0=ALU.mult, op1=ALU.add)
        k.act(tmp, tmp, AF.Sqrt)
        k.op("dve", "reciprocal", [rs], [tmp], rs, tmp)

    def norm_tile(src, gT, dstT, col0, xhat_dst=None):
        xb = Xb[cnt["x"] % 2]
        cnt["x"] += 1
        k.dma(xb, src)
        ss = st[:, 0:1]; rs = st[:, 1:2]; tmp = st[:, 2:3]
        k.memset(ss, 0.0)
        k.act(sqjunk, xb, AF.Square, accum_out=ss)
        rstd_from_ss(ss, rs, 1.0 / D, tmp)
        k.ts(xb, xb, rs, None, op0=ALU.mult)
        if xhat_dst is not None:
            k.dma(xhat_dst, xb)
        for c4 in range(8):
            pt = ps[4 + cnt["t"] % 4]
            cnt["t"] += 1
            for ci in range(4):
                c = c4 * 4 + ci
                k.transpose(pt[:, ci * 128:(ci + 1) * 128], xb[:, c * 128:(c + 1) * 128], ident[:])
            k.tt(dstT[:, c4 * 4:c4 * 4 + 4, col0:col0 + 128], pt[:].rearrange("p (a b) -> p a b", b=128),
                 gT[:, c4 * 4:c4 * 4 + 4].unsqueeze(2).to_broadcast([128, 4, 128]), ALU.mult)

    def proj(actT, nK, w2d, c0, ncols, mode, evac_fn):
        banks = bankset()
        nb = ncols // 128 if mode == "fm" else 4
        kstep = 4
        for kq in range(nK // kstep):
            wt = load_w(w2d[kq * kstep * 128:(kq + 1) * kstep * 128, c0:c0 + ncols])
            for m in range(nb):
                for kc in range(kstep):
                    kk = kq * kstep + kc
                    if mode == "fm":
                        k.mm(banks[m][:, :], wt[:, kc, m * 128:(m + 1) * 128], actT[:, kk, :], start=(kk == 0), stop=(kk == nK - 1))
                    else:
                        k.mm(banks[m][:, 0:ncols], actT[:, kk, m * 128:(m + 1) * 128], wt[:, kc, :], start=(kk == 0), stop=(kk == nK - 1))
        evac_fn(banks)

    def store_fm(dst, r0, t0, func=None):
        def f(banks):
            e = ebuf().rearrange("p (m t) -> p m t", t=512)
            for m in range(4):
                evac(e[:, m, :], banks[m][:, :], func)
            k.dma(dst[r0:r0 + 512, t0:t0 + 512].rearrange("(m p) t -> p m t", p=128), e)
        return f

    def store_tm(dst, t0, c0):
        def f(banks):
            e = ebuf().rearrange("p (m t) -> p m t", t=512)
            for m in range(4):
                evac(e[:, m, :], banks[m][:, :])
            k.dma(dst[t0:t0 + 512, c0:c0 + 512].rearrange("(m p) c -> p m c", p=128), e)
        return f

    xnT = AR[:, :].rearrange("p (c t) -> p c t", t=512)

    for tb in range(4):
        own = tb >= 2
        for i in range(4):
            norm_tile(xs[tb * 512 + i * 128: tb * 512 + (i + 1) * 128, :], gmix, xnT, i * 128)
        t0 = tb * 512
        to = t0 - T
        for cb in range(4):
            proj(xnT, 32, w_in, C_QM + cb * 512, 512, "fm", store_fm(QKm, cb * 512, t0))
        for cb in range(4):
            proj(xnT, 32, w_in, C_VM + cb * 512, 512, "tm", store_tm(Vm, t0, cb * 512))
        for cb in range(4):
            proj(xnT, 32, w_in, C_KS + cb * 512, 512, "fm", store_fm(Ks, cb * 512, t0))
        for cb in range(4):
            proj(xnT, 32, w_in, C_VS + cb * 512, 512, "tm", store_tm(Vs, t0, cb * 512))
        for gi, dstd in ((0, I8d), (1, F8d)):
            pb = ps[cnt["bank"] % 8]
            for kk in range(32):
                k.mm(pb[0:8, :], wif[:, kk, gi * 8:(gi + 1) * 8], xnT[:, kk, :].bitcast(F32), start=(kk == 0), stop=(kk == 31))
            e = ebuf()
            evac(e[0:8, 0:512], pb[0:8, :])
            k.dma(dstd[:, t0:t0 + 512], e[0:8, 0:512])
        if own:
            for cb in range(4):
                proj(xnT, 32, w_in, C_OM + cb * 512, 512, "fm", store_fm(Om, cb * 512, to, AF.Sigmoid))
            for cb in range(4):
                proj(xnT, 32, w_in, C_QS + cb * 512, 512, "fm", store_fm(Qs, cb * 512, to))
            for cb in range(8):
                proj(xnT, 32, w_in, C_GA + cb * 512, 512, "fm", store_fm(GA, cb * 512, to, AF.Sigmoid))
            for cb in range(8):
                proj(xnT, 32, w_in, C_GB + cb * 512, 512, "fm", store_fm(GB, cb * 512, to, AF.Sigmoid))
    if upto <= 1:
        return k

    k.barrier()
    I8 = ovr(0, 8, 13000, 15048); Fa = G[0:8, 8192:10240]; Fb_ = G[0:8, 4096:6144]; G8 = G[0:8, 6144:8192]; tmp8 = ovr(0, 8, 15048, 15560)
    bi = st[0:8, 8:9]; bf = st[0:8, 9:10]; nbf = st[0:8, 10:11]
    k.dma(bi, b_i); k.dma(bf, b_f)
    k.dma(I8, I8d); k.dma(Fa, F8d)
    k.ts(nbf, bf, -1.0, None, op0=ALU.mult)
    k.ts(I8, I8, bi, None, op0=ALU.add)
    k.act(Fa, Fa, AF.Exp, bias=nbf, scale=-1.0)
    k.act(Fa, Fa, AF.Ln, bias=1.0)
    k.ts(Fa, Fa, -1.0, None, op0=ALU.mult)
    src, dstb = Fa, Fb_
    dd = 1
    while dd < S:
        k.tt(dstb[:, dd:S], src[:, dd:S], src[:, 0:S - dd], ALU.add)
        k.copy(dstb[:, 0:dd], src[:, 0:dd])
        src, dstb = dstb, src
        dd *= 2
    Fc = src
    k.tt(G8, I8, Fc, ALU.subtract)
    k.ts(G8, G8, -LN_SQRT_DK, None, op0=ALU.add)
    pbt = ps[0]
    for j in range(16):
        k.transpose(pbt[:, j * 8:(j + 1) * 8], G8[:, j * 128:(j + 1) * 128], ident[0:8, 0:8])
    k.copy(biasT[:], pbt[:, 0:128])

    Qc = AR[:, 0:1024]; Kc = AR[:, 1024:3072]
    Pt = [AR[:, 3072 + i * 512:3072 + (i + 1) * 512] for i in range(2)]
    Vh = AR[:, 4096:8192].rearrange("p (j c) -> p j c", c=256)
    onesR = AR[:, 8192:8320]
    k.copy(onesR, ones[:])
    ctmp = ov(0, 2048)
    FbS = ov(3072, 3584)
    Dt = [ov(3584 + i * 512, 3584 + (i + 1) * 512) for i in range(2)]
    misc = [ov(5632 + i * 512, 5632 + (i + 1) * 512) for i in range(8)]
    qpre = ov(9728, 10755); kpre = ov(10755, 12806)
    Vst = G[:, 0:4096].rearrange("p (j c) -> p j c", c=256)
    cw = st[:, 16:24]
    for h in range(8):
        k.dma(cw[:, 0:4], convT[h * 128:(h + 1) * 128, :])
        k.dma(cw[:, 4:8], convT[1024 + h * 128:1024 + (h + 1) * 128, :])
        k.dma(qpre, QKm[h * 128:(h + 1) * 128, T - 3:S])
        k.memset(kpre[:, 0:3], 0.0)
        k.dma(kpre[:, 3:2051], QKm[1024 + h * 128:1024 + (h + 1) * 128, :])
        k.dma(Vst, Vm[:, h * 256:(h + 1) * 256].rearrange("(j p) c -> p j c", p=128))
        k.copy(Vh[:, 0:8, :], Vst[:, 0:8, :])
        k.act(Vh[:, 8:16, :], Vst[:, 8:16, :], AF.Copy)
        for (dst_, pre, n, wo) in ((Qc, qpre, T, 0), (Kc, kpre, S, 4)):
            ct = ctmp[:, 0:n]
            k.ts(ct, pre[:, 0:n], cw[:, wo:wo + 1], None, op0=ALU.mult)
            for kk in range(1, 4):
                k.stt(ct, pre[:, kk:kk + n], cw[:, wo + kk:wo + kk + 1], ct, ALU.mult, ALU.add)
            k.act(dst_, ct, AF.Silu)
        for tb in range(2):
            pF = ps[7]
            k.ts(tmp8, Fc[:, T + tb * 512:T + (tb + 1) * 512], ident[0:8, h:h + 1], None, op0=ALU.mult)
            k.mm(pF[:, :], ones[0:8, :], tmp8)
            k.copy(FbS, pF[:, :])
            nfull = 8 + 4 * tb
            nj = nfull + 4
            num0, num1, den = ps[4], ps[5], ps[6]
            for j in range(nj):
                pS = ps[j % 2]
                k.mm(pS[:, :], Kc[:, j * 128:(j + 1) * 128], Qc[:, tb * 512:(tb + 1) * 512])
                d_ = Dt[j % 2]; p_ = Pt[j % 2]
                k.act(d_, FbS, AF.Exp, bias=biasT[:, j * 8 + h:j * 8 + h + 1])
                if j >= nfull:
                    jj = j - nfull
                    k.tt(d_, d_, mmk[:, 384 - 128 * jj:384 - 128 * jj + 512], ALU.mult, eng="pool")
                k.tt(p_, pS[:, :], d_, ALU.mult)
                k.mm(num0[:, :], Vh[:, j, 0:128], p_, start=(j == 0), stop=(j == nj - 1))
                k.mm(num1[:, :], Vh[:, j, 128:256], p_, start=(j == 0), stop=(j == nj - 1))
                k.mm(den[:, :], onesR, p_, start=(j == 0), stop=(j == nj - 1))
            rec = misc[0]; h0 = misc[1]; h1 = misc[2]; sq = misc[3]; rs_ = misc[4]; og = misc[5]
            k.act(rec, den[:, :], AF.Abs)
            k.ts(rec, rec, 1.0, None, op0=ALU.max)
            k.op("dve", "reciprocal", [rec], [rec], rec, rec)
            k.tt(h0, num0[:, :], rec, ALU.mult)
            k.tt(h1, num1[:, :], rec, ALU.mult)
            pq = ps[3]
            k.tt(sq, h0, h0, ALU.mult)
            k.mm(pq[:, :], ones[:], sq, start=True, stop=False)
            k.tt(misc[6], h1, h1, ALU.mult)
            k.mm(pq[:, :], ones[:], misc[6], start=False, stop=True)
            k.ts(rs_, pq[:, :], 1.0 / 256.0, EPS, op0=ALU.mult, op1=ALU.add)
            k.act(rs_, rs_, AF.Sqrt)
            k.op("dve", "reciprocal", [rs_], [rs_], rs_, rs_)
            e = ebuf().rearrange("p (m t) -> p m t", t=512)
            for c, hc in ((0, h0), (1, h1)):
                k.dma(og, Om[h * 256 + c * 128:h * 256 + (c + 1) * 128, tb * 512:(tb + 1) * 512])
                k.stt(hc, hc, gml[:, h * 2 + c:h * 2 + c + 1], rs_, ALU.mult, ALU.mult)
                k.tt(e[:, c, :], hc, og, ALU.mult)
            k.dma(HM[h * 256:(h + 1) * 256, tb * 512:(tb + 1) * 512].rearrange("(m p) t -> p m t", p=128), e[:, 0:2, :])
    if upto <= 2:
        return k

    k.barrier()
    Qh = AR[:, 0:1024]; Kh = AR[:, 1024:3072]
    Vsh = AR[:, 3072:5120].rearrange("p (j c) -> p j c", c=128)
    SPt = [AR[:, 5120 + i * 512:5120 + (i + 1) * 512] for i in range(2)]
    SMt = [AR[:, 6144 + i * 512:6144 + (i + 1) * 512] for i in range(2)]
    At = [AR[:, 7168 + i * 512:7168 + (i + 1) * 512] for i in range(2)]
    utR = AR[:, 8192:8320]; ltR = AR[:, 8320:8448]
    k.copy(utR, ut[:]); k.copy(ltR, lt[:])
    qst = ov(0, 1024); kst = ov(1024, 3072); vst = ov(3072, 5120).rearrange("p (j c) -> p j c", c=128)
    Et = [ov(5120 + i * 512, 5120 + (i + 1) * 512) for i in range(2)]
    ARt = [ov(6144 + i * 512, 6144 + (i + 1) * 512) for i in range(2)]
    negm = ov(7168, 8064)
    k.ts(negm, msk[:], 1.0, 1.0e4, op0=ALU.subtract, op1=ALU.mult)
    for h in range(16):
        k.dma(qst, Qs[h * 128:(h + 1) * 128, :])
        k.dma(kst, Ks[h * 128:(h + 1) * 128, :])
        k.dma(vst, Vs[:, h * 128:(h + 1) * 128].rearrange("(j p) c -> p j c", p=128))
        k.copy(Qh, qst)
        k.act(Kh, kst, AF.Copy)
        k.copy(Vsh, vst)
        for tb in range(2):
            nfull = 8 + 4 * tb
            nj = nfull + 4
            acc = ps[4 + tb % 2]; po = ps[6 + tb % 2]
            for idx_, j in enumerate(range(nj - 1, -1, -1)):
                pz = ps[idx_ % 2]
                e_ = Et[idx_ % 2]; sp_ = SPt[idx_ % 2]; ar_ = ARt[idx_ % 2]; a_ = At[idx_ % 2]
                k.mm(pz[:, :], Kh[:, j * 128:(j + 1) * 128], Qh[:, tb * 512:(tb + 1) * 512])
                k.act(e_, pz[:, :], AF.Exp, scale=SB_SCALE)
                k.act(sp_, e_, AF.Ln, bias=1.0)
                diag = j >= nfull
                if diag:
                    jj = j - nfull
                    sl = slice(384 - 128 * jj, 384 - 128 * jj + 512)
                    spm = SMt[idx_ % 2]
                    k.tt(spm, sp_.bitcast(F32), msk[:, sl], ALU.mult)
                else:
                    spm = sp_
                k.mm(acc[:, :], utR, spm, start=(idx_ == 0), stop=False, skip_group_check=True)
                k.stt(ar_, pz[:, :], SB_SCALE, sp_.bitcast(F32), ALU.mult, ALU.subtract)
                k.tt(ar_, ar_, acc[:, :], ALU.subtract)
                if diag:
                    k.tt(ar_, ar_, negm[:, sl], ALU.add, eng="pool")
                k.act(a_, ar_, AF.Exp)
                k.mm(acc[:, :], ltR, spm, start=False, stop=(idx_ == nj - 1), skip_group_check=True)
                k.mm(po[:, :], Vsh[:, j, :], a_, start=(idx_ == 0), stop=(idx_ == nj - 1))
            e = ebuf()
            evac(e[:, 0:512], po[:, :])
            k.dma(HS[h * 128:(h + 1) * 128, tb * 512:(tb + 1) * 512], e[:, 0:512])
    if upto <= 3:
        return k

    k.barrier()
    yT = AR[:, :].rearrange("p (c t) -> p c t", t=512)
    gtile = ov(0, 2048).rearrange("p (m t) -> p m t", t=512)
    xres = ov(2048, 4096).rearrange("p (m t) -> p m t", t=512)

    def resid_store(src_res, dst, t0, c0):
        def f(banks):
            k.dma(xres, src_res[t0:t0 + 512, c0:c0 + 512].rearrange("(m p) c -> p m c", p=128))
            e = ebuf().rearrange("p (m t) -> p m t", t=512)
            for m in range(4):
                k.tt(e[:, m, :], banks[m][:, :], xres[:, m, :], ALU.add)
            k.dma(dst[t0:t0 + 512, c0:c0 + 512].rearrange("(m p) c -> p m c", p=128), e)
        return f

    for tb in range(2):
        t0 = tb * 512
        for pi, (hsrc, wsrc, gsrc) in enumerate(((HM, w_pa, GA), (HS, w_pb, GB))):
            for cb in range(8):
                banks = bankset()
                for kq in range(4):
                    at = load_w(hsrc[kq * 512:(kq + 1) * 512, t0:t0 + 512])
                    wt = load_w(wsrc[kq * 512:(kq + 1) * 512, cb * 512:(cb + 1) * 512])
                    for m in range(4):
                        for kc in range(4):
                            kk = kq * 4 + kc
                            k.mm(banks[m][:, :], wt[:, kc, m * 128:(m + 1) * 128], at[:, kc, :], start=(kk == 0), stop=(kk == 15))
                k.dma(gtile, gsrc[cb * 512:(cb + 1) * 512, t0:t0 + 512].rearrange("(m p) t -> p m t", p=128))
                for m in range(4):
                    dsty = yT[:, cb * 4 + m, :]
                    if pi == 0:
                        k.tt(dsty, banks[m][:, :], gtile[:, m, :], ALU.mult)
                    else:
                        k.tt(gtile[:, m, :], banks[m][:, :], gtile[:, m, :], ALU.mult)
                        k.tt(dsty, dsty.bitcast(F32), gtile[:, m, :], ALU.add)
        for cb in range(8):
            proj(yT, 32, w_out, cb * 512, 512, "tm", resid_store(xs[T:S, :], H1, t0, cb * 512))
    if upto <= 4:
        return k

    k.barrier()
    memT = AR[:, 0:8192].rearrange("p (c t) -> p c t", t=256)
    for i in range(2):
        norm_tile(memx[i * 128:(i + 1) * 128, :], gmem, memT, i * 128)
    for cb in range(8):
        banks = bankset()
        for kq in range(8):
            wt = load_w(w_kv[kq * 512:(kq + 1) * 512, cb * 512:(cb + 1) * 512])
            for m in range(4):
                for kc in range(4):
                    kk = kq * 4 + kc
                    k.mm(banks[m][:, 0:256], wt[:, kc, m * 128:(m + 1) * 128], memT[:, kk, :], start=(kk == 0), stop=(kk == 31))
        e = ebuf().rearrange("p (m t) -> p m t", t=512)
        for m in range(4):
            evac(e[:, m, 0:256], banks[m][:, 0:256])
        k.dma(KX[cb * 512:(cb + 1) * 512, :].rearrange("(m p) t -> p m t", p=128), e[:, :, 0:256])
    for cb in range(8):
        banks = bankset()
        for kq in range(8):
            wt = load_w(w_kv[kq * 512:(kq + 1) * 512, D + cb * 512:D + (cb + 1) * 512])
            for m in range(2):
                for kc in range(4):
                    kk = kq * 4 + kc
                    k.mm(banks[m][:, :], memT[:, kk, m * 128:(m + 1) * 128], wt[:, kc, :], start=(kk == 0), stop=(kk == 31))
        e = ebuf().rearrange("p (m t) -> p m t", t=512)
        for m in range(2):
            evac(e[:, m, :], banks[m][:, :])
        k.dma(VX[:, cb * 512:(cb + 1) * 512].rearrange("(m p) c -> p m c", p=128), e[:, 0:2, :])

    oT = AR[:, :].rearrange("p (c t) -> p c t", t=512)
    qh = ov(0, 4096).rearrange("p (c t) -> p c t", t=512)
    khT = ov(4096, 6144).rearrange("p (c m) -> p c m", m=256)
    vh = ov(6144, 8192).rearrange("p (m d) -> p m d", d=1024)
    pT = ov(8192, 9216).rearrange("p (m t) -> p m t", t=512)
    pbuf = [ov(9216 + i * 256, 9216 + (i + 1) * 256) for i in range(2)]
    xres = ov(12288, 14336).rearrange("p (m t) -> p m t", t=512)
    for tb in range(2):
        t0 = tb * 512
        for i in range(4):
            norm_tile(H1[t0 + i * 128:t0 + (i + 1) * 128, :], gx, xnT, i * 128)
        for cb in range(8):
            proj(xnT, 32, w_q, cb * 512, 512, "fm", store_fm(QX, cb * 512, t0))
        for h in range(4):
            k.dma(qh, QX[h * 1024:(h + 1) * 1024, t0:t0 + 512].rearrange("(c p) t -> p c t", p=128))
            k.dma(khT, KX[h * 1024:(h + 1) * 1024, :].rearrange("(c p) m -> p c m", p=128))
            k.dma(vh, VX[:, h * 1024:(h + 1) * 1024].rearrange("(m p) d -> p m d", p=128))
            for i in range(4):
                psc = ps[i % 2]
                for c in range(8):
                    k.mm(psc[:, 0:256], qh[:, c, i * 128:(i + 1) * 128], khT[:, c, :], start=(c == 0), stop=(c == 7))
                mx = st[:, 32:33]; nb_ = st[:, 33:34]; sm = st[:, 34:35]; rsm = st[:, 35:36]
                k.reduce(mx, psc[:, 0:256], ALU.max)
                k.ts(nb_, mx, -1.0 / 32.0, None, op0=ALU.mult)
                pb_ = pbuf[i % 2]
                k.memset(sm, 0.0)
                k.act(pb_, psc[:, 0:256], AF.Exp, bias=nb_, scale=1.0 / 32.0, accum_out=sm)
                k.op("dve", "reciprocal", [rsm], [sm], rsm, sm)
                k.ts(pb_, pb_, rsm, None, op0=ALU.mult)
                ptp = ps[2 + i % 2]
                for m in range(2):
                    k.transpose(ptp[:, m * 128:(m + 1) * 128], pb_[:, m * 128:(m + 1) * 128], ident[:])
                k.copy(pT[:, :, i * 128:(i + 1) * 128], ptp[:, 0:256].rearrange("p (m t) -> p m t", t=128))
            for dc in range(8):
                pso = ps[4 + dc % 4]
                for m in range(2):
                    k.mm(pso[:, :], vh[:, m, dc * 128:(dc + 1) * 128], pT[:, m, :], start=(m == 0), stop=(m == 1))
                evac(oT[:, h * 8 + dc, :], pso[:, :])
        for cb in range(8):
            proj(oT, 32, w_o, cb * 512, 512, "tm", resid_store(H1, H2, t0, cb * 512))
    if upto <= 5:
        return k

    k.barrier()
    RB = 12288
    RL = ov(RB, RB + 576).rearrange("p (i c) -> p i c", c=72)
    Aasg = ov(RB + 576, RB + 1088).rearrange("p (i c) -> p i c", c=64)
    OH1 = ov(RB + 1088, RB + 1600).rearrange("p (i c) -> p i c", c=64)
    OH2 = ov(RB + 1600, RB + 2112).rearrange("p (i c) -> p i c", c=64)
    POS = ov(RB + 2112, RB + 2624).rearrange("p (i c) -> p i c", c=64)
    rw = ov(RB + 2624, RB + 3136)
    CW = ov(RB + 3136, RB + 3152).rearrange("p (i c) -> p i c", c=2)
    RI = ov(RB + 3152, RB + 3168).bitcast(I32).rearrange("p (i c) -> p i c", c=2)
    hb = ov(RB + 3168, RB + 3680)
    bR = ov(RB + 3680, RB + 3752); erow = ov(RB + 3752, RB + 3816); iota = ov(RB + 3816, RB + 3944); ltpos = ov(RB + 3944, RB + 4072)
    k.dma(bR, b_r); k.dma(erow, c_erow); k.dma(iota, c_iota); k.dma(ltpos, c_ltpos)
    wr = G[:, 0:2304].rearrange("p (kc c) -> p kc c", c=72)
    for tb in range(2):
        for i in range(4):
            r0 = tb * 512 + i * 128
            norm_tile(H2[r0:r0 + 128, :], gmoe, xnT, i * 128, xhat_dst=XH[r0:r0 + 128, :])
        k.dma(wr, w_r.rearrange("(kc p) c -> p kc c", p=128))
        for i in range(4):
            it = tb * 4 + i
            pr = ps[i % 4]
            for kk in range(32):
                k.mm(pr[:, 0:72], xnT[:, kk, i * 128:(i + 1) * 128].bitcast(F32), wr[:, kk, :], start=(kk == 0), stop=(kk == 31))
            k.tt(RL[:, it, :], pr[:, 0:72], bR, ALU.add)
    s_ = lambda a, b_: st[:, a:b_]
    for it in range(8):
        lg = RL[:, it, 0:8]; le = RL[:, it, 8:72]
        m1 = s_(40, 41); nm1 = s_(41, 42); sg = s_(42, 43); gw = s_(43, 44); m1e = s_(44, 45); m2e = s_(45, 46)
        dl = s_(46, 47); w1 = s_(47, 48); w2 = s_(48, 49)
        ohg = rw[:, 0:8]; pen = rw[:, 8:16]; egx = rw[:, 16:24]; lem = rw[:, 64:128]; lem2 = rw[:, 128:192]
        k.reduce(m1, lg, ALU.max)
        k.ts(ohg, lg, m1, None, op0=ALU.is_equal)
        k.ts(nm1, m1, -1.0, None, op0=ALU.mult)
        k.memset(sg, 0.0)
        k.act(egx, lg, AF.Exp, bias=nm1, accum_out=sg)
        k.op("dve", "reciprocal", [gw], [sg], gw, sg)
        k.ts(pen, ohg, 1.0, 1e30, op0=ALU.subtract, op1=ALU.mult)
        k.tt(lem.rearrange("p (g e) -> p g e", e=8), le.rearrange("p (g e) -> p g e", e=8),
             pen.unsqueeze(2).to_broadcast([128, 8, 8]), ALU.add)
        k.reduce(m1e, lem, ALU.max)
        k.ts(OH1[:, it, :], lem, m1e, None, op0=ALU.is_equal)
        k.stt(lem2, OH1[:, it, :], -1e30, lem, ALU.mult, ALU.add)
        k.reduce(m2e, lem2, ALU.max)
        k.ts(OH2[:, it, :], lem2, m2e, None, op0=ALU.is_equal)
        k.tt(dl, m2e, m1e, ALU.subtract)
        k.act(dl, dl, AF.Exp)
        k.ts(w1, dl, 1.0, None, op0=ALU.add)
        k.op("dve", "reciprocal", [w1], [w1], w1, w1)
        k.tt(w2, dl, w1, ALU.mult)
        k.tt(CW[:, it, 0:1], w1, gw, ALU.mult)
        k.tt(CW[:, it, 1:2], w2, gw, ALU.mult)
        k.tt(Aasg[:, it, :], OH1[:, it, :], OH2[:, it, :], ALU.add)
    for it in range(8):
        pp = ps[4 + it % 4]
        for i2 in range(it):
            k.mm(pp[:, 0:64], ones[:], Aasg[:, i2, :], start=(i2 == 0), stop=False)
        k.mm(pp[:, 0:64], ltpos, Aasg[:, it, :], start=(it == 0), stop=True)
        k.copy(POS[:, it, :], pp[:, 0:64])
        t64 = rw[:, 192:256]; t64b = rw[:, 256:320]; rf = s_(50, 51)
        k.tt(t64, POS[:, it, :], erow, ALU.add)
        for kk_, OH in ((0, OH1), (1, OH2)):
            k.tt(t64b, t64, OH[:, it, :], ALU.mult)
            k.reduce(rf, t64b, ALU.add)
            k.copy(RI[:, it, kk_:kk_ + 1], rf)

    XeT = AR[:, 0:8192].rearrange("p (c s) -> p c s", s=256)
    Sel = AR[:, 8192:10240].rearrange("p (i s) -> p i s", s=256)
    hbT = AR[:, 10240:10752].rearrange("p (f s) -> p f s", s=128)
    for eg in range(NEXP // 2):
        for it in range(8):
            for el in range(2):
                e = eg * 2 + el
                k.ts(Sel[:, it, el * 128:(el + 1) * 128], iota, POS[:, it, e:e + 1], Aasg[:, it, e:e + 1],
                     op0=ALU.is_equal, op1=ALU.mult)
        for db in range(8):
            banks = bankset()
            for hf in range(2):
                xh = load_w(XH[hf * 512:(hf + 1) * 512, db * 512:(db + 1) * 512])
                for dc in range(4):
                    for i4 in range(4):
                        it = hf * 4 + i4
                        k.mm(banks[dc][:, 0:256], xh[:, i4, dc * 128:(dc + 1) * 128], Sel[:, it, :], start=(it == 0), stop=(it == 7))
            for dc in range(4):
                c = db * 4 + dc
                k.ts(XeT[:, c, :], banks[dc][:, 0:256], gmoe[:, c:c + 1], None, op0=ALU.mult)
        for el in range(2):
            e = eg * 2 + el
            banks = bankset()
            pg, pu, pt_, _ = banks
            for wsrc, pacc in ((w_g, pg), (w_u, pu)):
                for kq in range(8):
                    wt = load_w(wsrc[e * D + kq * 512:e * D + (kq + 1) * 512, :])
                    for kc in range(4):
                        kk = kq * 4 + kc
                        k.mm(pacc[:, :], XeT[:, kk, el * 128:(el + 1) * 128], wt[:, kc, :], start=(kk == 0), stop=(kk == 31))
            k.act(hb, pg[:, :], AF.Silu)
            k.tt(hb, hb, pu[:, :], ALU.mult)
            for fc in range(4):
                k.transpose(pt_[:, fc * 128:(fc + 1) * 128], hb[:, fc * 128:(fc + 1) * 128], ident[:])
            k.copy(hbT, pt_[:, :].rearrange("p (f s) -> p f s", s=128))
            for dq2 in range(4):
                yb = bankset()
                ee = ebuf()
                for hf in range(2):
                    dq = dq2 * 2 + hf
                    wd = load_w(w_d[e * 512:(e + 1) * 512, dq * 512:(dq + 1) * 512])
                    for fc in range(4):
                        k.mm(yb[hf][:, :], hbT[:, fc, :], wd[:, fc, :], start=(fc == 0), stop=(fc == 3))
                    evac(ee[:, hf * 512:(hf + 1) * 512], yb[hf][:, :])
                k.dma(YB[e * CAP:(e + 1) * CAP, dq2 * 1024:(dq2 + 1) * 1024], ee[:, 0:1024])

    Y1 = ov(0, 4096); Y2 = ov(4096, 8192); junk = ov(8192, 12288)
    gfin = G[:, 0:4096]; h2t = G[:, 4096:8192]
    k.dma(gfin, g_fin)
    for it in range(8):
        r0 = it * 128
        k.dma(h2t, H2[r0:r0 + 128, :])
        for kk_, Y in ((0, Y1), (1, Y2)):
            k.dma(Y, YB, q="pool", extra_reads=[RI[:, it, kk_:kk_ + 1]], _meth="indirect_dma_start", out_offset=None,
                  in_offset=bass.IndirectOffsetOnAxis(ap=RI[:, it, kk_:kk_ + 1], axis=0))
            k.stt(h2t, Y, CW[:, it, kk_:kk_ + 1], h2t, ALU.mult, ALU.add)
        ss = s_(52, 53); rs = s_(53, 54); tmp = s_(54, 55)
        k.memset(ss, 0.0)
        k.act(junk, h2t, AF.Square, accum_out=ss)
        rstd_from_ss(ss, rs, 1.0 / D, tmp)
        k.ts(h2t, h2t, rs, None, op0=ALU.mult)
        k.tt(h2t, h2t, gfin, ALU.mult)
        k.dma(out[r0:r0 + 128, :], h2t)
    return k


def _consts():
    f = np.float32
    i = np.arange(128)
    c = {}
    c["c_ident"] = np.eye(128, dtype=f)
    c["c_ones"] = np.ones((128, 128), f)
    c["c_ut"] = (i[:, None] > i[None, :]).astype(f)
    c["c_lt"] = (i[:, None] <= i[None, :]).astype(f)
    u = np.arange(896)
    c["c_mm"] = ((u[None, :] - 384) >= i[:, None]).astype(f)
    c["c_ms"] = ((u[None, :] - 384) > i[:, None]).astype(f)
    c["c_ltpos"] = (i[:, None] < i[None, :]).astype(f)
    c["c_iota"] = np.broadcast_to(np.arange(128, dtype=f)[None, :], (128, 128)).copy()
    c["c_erow"] = np.broadcast_to((np.arange(64, dtype=f) * 128.0)[None, :], (128, 64)).copy()
    return c


def _gl(g, n):
    return np.ascontiguousarray(np.asarray(g, np.float32).reshape(n, 128).T)


def make_in_maps(inp, cores=range(8), moe=True):
    f = np.float32
    x = np.asarray(inp["x"], f); mem = np.asarray(inp["mem"], f)
    sh = _consts()
    sh["w_in"] = np.asarray(inp["w_in"], f)[0]
    sh["w_proj_a"] = np.asarray(inp["w_proj_a"], f)[0]
    sh["w_proj_b"] = np.asarray(inp["w_proj_b"], f)[0]
    sh["w_out"] = np.asarray(inp["w_out"], f)[0]
    sh["w_q_mem"] = np.asarray(inp["w_q_mem"], f)[0]
    sh["w_kv_mem"] = np.asarray(inp["w_kv_mem"], f)[0]
    sh["w_o_mem"] = np.asarray(inp["w_o_mem"], f)[0]
    if moe:
        sh["w_gate"] = np.asarray(inp["w_gate"], f)[0].reshape(NEXP * D, 512)
        sh["w_up"] = np.asarray(inp["w_up"], f)[0].reshape(NEXP * D, 512)
        sh["w_down"] = np.asarray(inp["w_down"], f)[0].reshape(NEXP * 512, D)
    sh["w_r"] = np.ascontiguousarray(np.concatenate([np.asarray(inp["w_router_group"], f)[0], np.asarray(inp["w_router_expert"], f)[0]], axis=1))
    br = np.concatenate([np.asarray(inp["b_router_group"], f)[0], np.asarray(inp["b_router_expert"], f)[0]])
    sh["b_r"] = np.broadcast_to(br[None, :], (128, 72)).copy()
    sh["g_mix"] = _gl(inp["norm_mix"][0], 32); sh["g_x"] = _gl(inp["norm_xattn"][0], 32)
    sh["g_mem"] = _gl(inp["norm_mem"][0], 32); sh["g_moe"] = _gl(inp["norm_moe"][0], 32)
    sh["g_fin"] = np.broadcast_to(np.asarray(inp["norm_final"], f)[None, :], (128, D)).copy()
    sh["g_ml"] = _gl(inp["g_mlstm"][0], 16)
    sh["convT"] = np.ascontiguousarray(np.asarray(inp["conv_qk"], f)[0].T)
    bg = np.asarray(inp["b_gates"], f)[0]
    sh["b_i"] = bg[:8].reshape(8, 1).copy(); sh["b_f"] = bg[8:].reshape(8, 1).copy()
    maps = []
    for c in cores:
        b, half = c // 2, c % 2
        xs_ = np.zeros((S, D), f)
        if half == 1:
            xs_[:] = x[b]
        else:
            xs_[T:] = x[b, :T]
        m = dict(sh)
        m["xs"] = xs_
        m["mem"] = np.ascontiguousarray(mem[b])
        maps.append(m)
    return maps


def kernel(**inputs):
    kb = build()
    nc = kb.finish()
    maps = make_in_maps(inputs)
    res = run_bass_kernel_spmd(nc, maps, core_ids=list(range(8)))
    outp = np.zeros((4, S, D), np.float32)
    for c in range(8):
        b, half = c // 2, c % 2
        outp[b, half * T:(half + 1) * T] = res.results[c]["out"]
    return outp
```

```python
import numpy as np
from contextlib import ExitStack
import concourse.bass as bass
import concourse.mybir as mybir
from concourse.bass_utils import run_bass_kernel_spmd

F32 = mybir.dt.float32
F32R = mybir.dt.float32r
I32 = mybir.dt.int32
ALU = mybir.AluOpType
AF = mybir.ActivationFunctionType
AX = mybir.AxisListType

ENGS = ("pe", "act", "dve", "pool", "sp")
NDMASEM = {"sp": 24, "pool": 8, "act": 8}


def _region(ap):
    t = ap.tensor
    shape = list(t.shape)
    rowsize = 1
    for s in shape[1:]:
        rowsize *= int(s)
    off = int(ap.offset)
    r_lo, c_lo = off // rowsize, off % rowsize
    r_ext, c_ext = 0, 0
    for step, cnt in ap.ap:
        step, cnt = int(step), int(cnt)
        if cnt <= 1 or step == 0:
            continue
        if step % rowsize == 0:
            r_ext += (cnt - 1) * (step // rowsize)
        else:
            c_ext += (cnt - 1) * abs(step)
    return (t.name, r_lo, r_lo + r_ext + 1, c_lo, c_lo + c_ext + 1)


def _overlap(a, b):
    return a[1] < b[2] and b[1] < a[2] and a[3] < b[4] and b[3] < a[4]


def _covers(a, b):
    return a[1] <= b[1] and a[2] >= b[2] and a[3] <= b[3] and a[4] >= b[4]


class K:
    def __init__(self):
        self.nc = bass.Bass("TRN2", target_bir_lowering=False)
        self.es = ExitStack()
        self.streams = {e: [] for e in ENGS}
        self.cnt = {e: 0 for e in ENGS}
        self.esem = {}
        for e in ("pe", "act", "dve", "pool"):
            self.esem[e] = self.es.enter_context(self.nc.semaphore("sem_" + e))
        self.dsem = {}
        self.dcnt = {}
        self.dlast = {}
        for q, n in NDMASEM.items():
            self.dsem[q] = [self.es.enter_context(self.nc.semaphore("dsem_%s_%d" % (q, i))) for i in range(n)]
            self.dcnt[q] = 0
        self.track = {}
        self.waited = {e: {} for e in ENGS}
        self.n_wait = 0
        self.out_tokens = []

    def sbuf(self, name, shape, dt=F32):
        return self.es.enter_context(self.nc.sbuf_tensor(name, list(shape), dt))

    def psum(self, name, shape, dt=F32):
        return self.es.enter_context(self.nc.psum_tensor(name, list(shape), dt))

    def dram(self, name, shape, dt=F32, kind="Internal"):
        return self.nc.dram_tensor(name, list(shape), dt, kind=kind).ap()

    def _semkey(self, tok):
        if tok[0] == "c":
            return ("c", tok[1]), tok[2]
        return ("d", tok[1], tok[2]), tok[3]

    def _need_wait(self, eng, tok, kind_raw, is_dma=False):
        if tok[0] == "c" and tok[1] == eng and not is_dma:
            if eng == "pe":
                return False
            if not kind_raw:
                return False
        key, val = self._semkey(tok)
        return self.waited[eng].get(key, 0) < val

    def _add_wait(self, eng, tok, waits):
        key, val = self._semkey(tok)
        if self.waited[eng].get(key, 0) >= val:
            return
        self.waited[eng][key] = val
        if tok[0] == "c":
            sem = self.esem[tok[1]]
        else:
            sem = self.dsem[tok[1]][tok[2]]
        waits.append((sem, val))

    def _deps(self, eng, reads, writes, is_dma=False):
        waits = []
        for r in reads:
            for ent in self.track.get(r[0], ()):
                if ent[1] == "w" and _overlap(ent[0], r):
                    if self._need_wait(eng, ent[2], True, is_dma):
                        self._add_wait(eng, ent[2], waits)
        for w in writes:
            for ent in self.track.get(w[0], ()):
                if _overlap(ent[0], w):
                    if self._need_wait(eng, ent[2], False, is_dma):
                        self._add_wait(eng, ent[2], waits)
        return waits

    def _record(self, reads, writes, tok):
        for w in writes:
            lst = self.track.setdefault(w[0], [])
            lst[:] = [e for e in lst if not _covers(w, e[0])]
            lst.append([w, "w", tok])
        for r in reads:
            lst = self.track.setdefault(r[0], [])
            if tok[0] == "c":
                lst[:] = [e for e in lst if not (e[1] == "r" and e[2][0] == "c" and e[2][1] == tok[1] and _covers(r, e[0]))]
            lst.append([r, "r", tok])

    def op(self, eng, meth, writes, reads, *args, **kw):
        wr = [_region(a) for a in writes]
        rr = [_region(a) for a in reads]
        waits = self._deps(eng, rr, wr)
        self.cnt[eng] += 1
        tok = ("c", eng, self.cnt[eng])
        self._record(rr, wr, tok)
        self.streams[eng].append((waits, meth, args, kw, (self.esem[eng], 1)))
        self.n_wait += len(waits)
        return tok

    def dma(self, out, in_, q="sp", extra_reads=(), **kw):
        wr = [_region(out)]
        rr = [_region(in_)] + [_region(a) for a in extra_reads]
        waits = self._deps(q, rr, wr, True)
        i = self.dcnt[q]
        n = len(self.dsem[q])
        slot, gen = i % n, i // n
        if gen > 0:
            self._add_wait(q, ("d", q, slot, 16 * gen), waits)
        self.dcnt[q] += 1
        tok = ("d", q, slot, 16 * (gen + 1))
        self._record(rr, wr, tok)
        meth = kw.pop("_meth", "dma_start")
        self.streams[q].append((waits, meth, (), dict(out=out, in_=in_, **kw), (self.dsem[q][slot], 16)))
        return tok

    def wait_tok(self, eng, tok):
        waits = []
        self._add_wait(eng, tok, waits)
        if waits:
            self.streams[eng].append((waits, None, (), {}, None))

    def barrier(self):
        toks = []
        for e in ("pe", "act", "dve", "pool"):
            if self.cnt[e] > 0:
                toks.append(("c", e, self.cnt[e]))
        for q in self.dsem:
            n = len(self.dsem[q])
            for i in range(max(0, self.dcnt[q] - n), self.dcnt[q]):
                toks.append(("d", q, i % n, 16 * (i // n + 1)))
        for e in ENGS:
            for t in toks:
                if t[0] == "c" and t[1] == e:
                    continue
                self.wait_tok(e, t)
        self.track = {}

    def mm(self, out, lhsT, rhs, start=True, stop=True, **kw):
        return self.op("pe", "matmul", [out], [lhsT, rhs], out, lhsT, rhs, start=start, stop=stop, **kw)

    def transpose(self, out, in_, ident):
        return self.op("pe", "transpose", [out], [in_, ident], out, in_, ident)

    def act(self, out, in_, func, bias=None, scale=None, accum_out=None, eng="act"):
        reads = [in_]
        kw = {}
        if bias is not None:
            kw["bias"] = bias
            if not isinstance(bias, (int, float)):
                reads.append(bias)
        if scale is not None:
            kw["scale"] = scale
            if not isinstance(scale, (int, float)):
                reads.append(scale)
        writes = [out]
        if accum_out is not None:
            kw["accum_out"] = accum_out
            writes.append(accum_out)
        return self.op(eng, "activation", writes, reads, out, in_, func, **kw)

    def tt(self, out, in0, in1, op, eng="dve"):
        return self.op(eng, "tensor_tensor", [out], [in0, in1], out, in0, in1, op)

    def ts(self, out, in0, s1, s2=None, op0=ALU.mult, op1=None, eng="dve", accum_out=None):
        reads = [in0]
        if not isinstance(s1, (int, float)):
            reads.append(s1)
        if s2 is not None and not isinstance(s2, (int, float)):
            reads.append(s2)
        kw = {}
        if op1 is not None:
            kw["op1"] = op1
        writes = [out]
        if accum_out is not None:
            kw["accum_out"] = accum_out
            writes.append(accum_out)
        return self.op(eng, "tensor_scalar", writes, reads, out, in0, s1, s2, op0, **kw)

    def stt(self, out, in0, scalar, in1, op0, op1, eng="dve"):
        reads = [in0, in1]
        if not isinstance(scalar, (int, float)):
            reads.append(scalar)
        return self.op(eng, "scalar_tensor_tensor", [out], reads, out, in0, scalar, in1, op0, op1)

    def copy(self, out, in_, eng="dve"):
        return self.op(eng, "tensor_copy", [out], [in_], out, in_)

    def memset(self, out, val, eng="dve"):
        return self.op(eng, "memset", [out], [], out, val)

    def reduce(self, out, in_, op, axis=AX.X, eng="dve"):
        return self.op(eng, "tensor_reduce", [out], [in_], out, in_, axis, op)

    def finish(self):
        nc = self.nc
        eng_obj = {"pe": "tensor", "act": "scalar", "dve": "vector", "pool": "gpsimd", "sp": "sync"}
        self.barrier()
        with nc.Block() as block:
            for e in ENGS:
                items = self.streams[e]

                def body(eo, items=items):
                    for waits, meth, args, kw, inc in items:
                        for sem, val in waits:
                            eo.wait_ge(sem, val)
                        if meth is None:
                            continue
                        ins = getattr(eo, meth)(*args, **kw)
                        if inc is not None:
                            ins.then_inc(inc[0], inc[1])
                getattr(block, eng_obj[e])(body)
        return nc

    def stats(self):
        return {e: len(self.streams[e]) for e in ENGS}, self.n_wait

import math
D = 4096
T = 1024
S = 2048
EPS = 1e-6
C_QM, C_KM, C_VM, C_OM, C_I, C_F, C_QS, C_KS, C_VS, C_GA, C_GB = 0, 1024, 2048, 4096, 6144, 6152, 6160, 8208, 10256, 12304, 16400
SB_SCALE = 1.0 / math.sqrt(128.0)
LN_SQRT_DK = 0.5 * math.log(128.0)
NEXP = 64
CAP = 128


def build(upto=99, dbg=()):
    k = K()

    def ext(n, shp, dt=F32):
        return k.dram(n, shp, dt, kind="ExternalInput")

    def scr(n, shp, dt=F32):
        return k.dram(n, shp, dt, kind=("ExternalOutput" if n in dbg else "Internal"))

    xs = ext("xs", [S, D]); memx = ext("mem", [256, D])
    w_in = ext("w_in", [D, 20496]); w_pa = ext("w_proj_a", [2048, D]); w_pb = ext("w_proj_b", [2048, D])
    w_out = ext("w_out", [D, D]); w_q = ext("w_q_mem", [D, D]); w_kv = ext("w_kv_mem", [D, 2 * D]); w_o = ext("w_o_mem", [D, D])
    if upto >= 6:
        w_g = ext("w_gate", [NEXP * D, 512]); w_u = ext("w_up", [NEXP * D, 512]); w_d = ext("w_down", [NEXP * 512, D])
    w_r = ext("w_r", [D, 72]); b_r = ext("b_r", [128, 72])
    g_mix = ext("g_mix", [128, 32]); g_x = ext("g_x", [128, 32]); g_mem = ext("g_mem", [128, 32]); g_moe = ext("g_moe", [128, 32])
    g_fin = ext("g_fin", [128, D]); g_ml = ext("g_ml", [128, 16])
    convT = ext("convT", [2048, 4]); b_i = ext("b_i", [8, 1]); b_f = ext("b_f", [8, 1])
    c_ident = ext("c_ident", [128, 128]); c_ones = ext("c_ones", [128, 128]); c_ut = ext("c_ut", [128, 128]); c_lt = ext("c_lt", [128, 128])
    c_mm = ext("c_mm", [128, 896]); c_ms = ext("c_ms", [128, 896])
    c_ltpos = ext("c_ltpos", [128, 128]); c_iota = ext("c_iota", [128, 128]); c_erow = ext("c_erow", [128, 64]); c_tokid = ext("c_tokid", [128, 8])
    out = k.dram("out", [T, D], F32, kind="ExternalOutput")

    QKm = scr("QKm", [2048, S]); Vm = scr("Vm", [S, 2048]); Om = scr("Om", [2048, T])
    I8d = scr("I8d", [8, S]); F8d = scr("F8d", [8, S])
    Qs = scr("Qs", [2048, T]); Ks = scr("Ks", [2048, S]); Vs = scr("Vs", [S, 2048])
    GA = scr("GA", [D, T]); GB = scr("GB", [D, T])
    HM = scr("HM", [2048, T]); HS = scr("HS", [2048, T])
    H1 = scr("H1", [T, D]); H2 = scr("H2", [T, D])
    KX = scr("KX", [D, 256]); VX = scr("VX", [256, D]); QX = scr("QX", [D, T])
    XH = scr("XH", [T, D]); YB = scr("YB", [NEXP * CAP, D])

    AR = k.sbuf("AR", [128, 16384], F32R); WR = k.sbuf("WR", [128, 6144], F32R); G = k.sbuf("G", [128, 26624])
    WS = [G[:, i * 2048:(i + 1) * 2048] for i in range(3)]
    WRs = [WR[:, i * 2048:(i + 1) * 2048] for i in range(3)]
    Eb = [G[:, 6144 + i * 2048:6144 + (i + 1) * 2048] for i in range(2)]
    OV0 = 10240

    def ov(a, b_):
        return G[:, OV0 + a:OV0 + b_]

    def ovr(r0, r1, a, b_):
        return G[r0:r1, OV0 + a:OV0 + b_]
    Xb = [ov(0, 4096), ov(4096, 8192)]
    sqjunk = ov(8192, 12288)
    ident = k.sbuf("ident", [128, 128]); ones = k.sbuf("ones", [128, 128]); ut = k.sbuf("ut", [128, 128]); lt = k.sbuf("lt", [128, 128])
    mmk = k.sbuf("mmk", [128, 896]); msk = k.sbuf("msk", [128, 896])
    gmix = k.sbuf("gmix", [128, 32]); gx = k.sbuf("gx", [128, 32]); gmem = k.sbuf("gmem", [128, 32]); gmoe = k.sbuf("gmoe", [128, 32]); gml = k.sbuf("gml", [128, 16])
    st = k.sbuf("st", [128, 64])
    wif = k.sbuf("wif", [128, 32, 16])
    biasT = k.sbuf("biasT", [128, 128])
    ps = [k.psum("ps%d" % i, [128, 512]) for i in range(8)]

    for dst, src in ((ident, c_ident), (ones, c_ones), (ut, c_ut), (lt, c_lt), (mmk, c_mm), (msk, c_ms),
                     (gmix, g_mix), (gx, g_x), (gmem, g_mem), (gmoe, g_moe), (gml, g_ml)):
        k.dma(dst[:], src)
    k.dma(wif[:], w_in[:, C_I:C_I + 16].rearrange("(kc p) c -> p kc c", p=128))

    cnt = {"w": 0, "e": 0, "bank": 0, "x": 0, "t": 0, "alt": 0}

    def load_w(w2d):
        nk = w2d.shape[0] // 128
        nc_ = w2d.shape[1]
        i = cnt["w"] % 3
        cnt["w"] += 1
        sv = WS[i][:, 0:nk * nc_].rearrange("p (kc c) -> p kc c", c=nc_)
        rv = WRs[i][:, 0:nk * nc_].rearrange("p (kc c) -> p kc c", c=nc_)
        k.dma(sv, w2d.rearrange("(kc p) c -> p kc c", p=128))
        h = max(1, nk // 2)
        k.copy(rv[:, 0:h, :], sv[:, 0:h, :])
        if h < nk:
            k.act(rv[:, h:nk, :], sv[:, h:nk, :], AF.Copy)
        return rv

    def bankset():
        b = cnt["bank"] % 2
        cnt["bank"] += 1
        return [ps[b * 4 + i] for i in range(4)]

    def ebuf():
        e = Eb[cnt["e"] % 2]
        cnt["e"] += 1
        return e

    def evac(out_ap, in_ap, func=None):
        if func is not None:
            k.act(out_ap, in_ap, func)
            return
        cnt["alt"] += 1
        if cnt["alt"] % 2:
            k.act(out_ap, in_ap, AF.Copy)
        else:
            k.copy(out_ap, in_ap)

    def rstd_from_ss(ss, rs, scale, tmp):
        k.ts(tmp, ss, scale, EPS, op0=ALU.mult, op1=ALU.add)
        k.act(tmp, tmp, AF.Sqrt)
        k.op("dve", "reciprocal", [rs], [tmp], rs, tmp)

    def norm_tile(src, gT, dstT, col0, xhat_dst=None):
        xb = Xb[cnt["x"] % 2]
        cnt["x"] += 1
        k.dma(xb, src)
        ss = st[:, 0:1]; rs = st[:, 1:2]; tmp = st[:, 2:3]
        k.memset(ss, 0.0)
        k.act(sqjunk, xb, AF.Square, accum_out=ss)
        rstd_from_ss(ss, rs, 1.0 / D, tmp)
        k.ts(xb, xb, rs, None, op0=ALU.mult)
        if xhat_dst is not None:
            k.dma(xhat_dst, xb)
        for c4 in range(8):
            pt = ps[4 + cnt["t"] % 4]
            cnt["t"] += 1
            for ci in range(4):
                c = c4 * 4 + ci
                k.transpose(pt[:, ci * 128:(ci + 1) * 128], xb[:, c * 128:(c + 1) * 128], ident[:])
            k.tt(dstT[:, c4 * 4:c4 * 4 + 4, col0:col0 + 128], pt[:].rearrange("p (a b) -> p a b", b=128),
                 gT[:, c4 * 4:c4 * 4 + 4].unsqueeze(2).to_broadcast([128, 4, 128]), ALU.mult)

    def proj(actT, nK, w2d, c0, ncols, mode, evac_fn):
        banks = bankset()
        nb = ncols // 128 if mode == "fm" else 4
        kstep = 4
        for kq in range(nK // kstep):
            wt = load_w(w2d[kq * kstep * 128:(kq + 1) * kstep * 128, c0:c0 + ncols])
            for m in range(nb):
                for kc in range(kstep):
                    kk = kq * kstep + kc
                    if mode == "fm":
                        k.mm(banks[m][:, :], wt[:, kc, m * 128:(m + 1) * 128], actT[:, kk, :], start=(kk == 0), stop=(kk == nK - 1))
                    else:
                        k.mm(banks[m][:, 0:ncols], actT[:, kk, m * 128:(m + 1) * 128], wt[:, kc, :], start=(kk == 0), stop=(kk == nK - 1))
        evac_fn(banks)

    def store_fm(dst, r0, t0, func=None):
        def f(banks):
            e = ebuf().rearrange("p (m t) -> p m t", t=512)
            for m in range(4):
                evac(e[:, m, :], banks[m][:, :], func)
            k.dma(dst[r0:r0 + 512, t0:t0 + 512].rearrange("(m p) t -> p m t", p=128), e)
        return f

    def store_tm(dst, t0, c0):
        def f(banks):
            e = ebuf().rearrange("p (m t) -> p m t", t=512)
            for m in range(4):
                evac(e[:, m, :], banks[m][:, :])
            k.dma(dst[t0:t0 + 512, c0:c0 + 512].rearrange("(m p) c -> p m c", p=128), e)
        return f

    xnT = AR[:, :].rearrange("p (c t) -> p c t", t=512)

    for tb in range(4):
        own = tb >= 2
        for i in range(4):
            norm_tile(xs[tb * 512 + i * 128: tb * 512 + (i + 1) * 128, :], gmix, xnT, i * 128)
        t0 = tb * 512
        to = t0 - T
        for cb in range(4):
            proj(xnT, 32, w_in, C_QM + cb * 512, 512, "fm", store_fm(QKm, cb * 512, t0))
        for cb in range(4):
            proj(xnT, 32, w_in, C_VM + cb * 512, 512, "tm", store_tm(Vm, t0, cb * 512))
        for cb in range(4):
            proj(xnT, 32, w_in, C_KS + cb * 512, 512, "fm", store_fm(Ks, cb * 512, t0))
        for cb in range(4):
            proj(xnT, 32, w_in, C_VS + cb * 512, 512, "tm", store_tm(Vs, t0, cb * 512))
        for gi, dstd in ((0, I8d), (1, F8d)):
            pb = ps[cnt["bank"] % 8]
            for kk in range(32):
                k.mm(pb[0:8, :], wif[:, kk, gi * 8:(gi + 1) * 8], xnT[:, kk, :].bitcast(F32), start=(kk == 0), stop=(kk == 31))
            e = ebuf()
            evac(e[0:8, 0:512], pb[0:8, :])
            k.dma(dstd[:, t0:t0 + 512], e[0:8, 0:512])
        if own:
            for cb in range(4):
                proj(xnT, 32, w_in, C_OM + cb * 512, 512, "fm", store_fm(Om, cb * 512, to, AF.Sigmoid))
            for cb in range(4):
                proj(xnT, 32, w_in, C_QS + cb * 512, 512, "fm", store_fm(Qs, cb * 512, to))
            for cb in range(8):
                proj(xnT, 32, w_in, C_GA + cb * 512, 512, "fm", store_fm(GA, cb * 512, to, AF.Sigmoid))
            for cb in range(8):
                proj(xnT, 32, w_in, C_GB + cb * 512, 512, "fm", store_fm(GB, cb * 512, to, AF.Sigmoid))
    if upto <= 1:
        return k

    k.barrier()
    I8 = ovr(0, 8, 13000, 15048); Fa = G[0:8, 8192:10240]; Fb_ = G[0:8, 4096:6144]; G8 = G[0:8, 6144:8192]
    bi = st[0:8, 8:9]; bf = st[0:8, 9:10]; nbf = st[0:8, 10:11]
    k.dma(bi, b_i); k.dma(bf, b_f)
    k.dma(I8, I8d); k.dma(Fa, F8d)
    k.ts(nbf, bf, -1.0, None, op0=ALU.mult)
    k.ts(I8, I8, bi, None, op0=ALU.add)
    k.act(Fa, Fa, AF.Exp, bias=nbf, scale=-1.0)
    k.act(Fa, Fa, AF.Ln, bias=1.0)
    k.ts(Fa, Fa, -1.0, None, op0=ALU.mult)
    src, dstb = Fa, Fb_
    dd = 1
    while dd < S:
        k.tt(dstb[:, dd:S], src[:, dd:S], src[:, 0:S - dd], ALU.add)
        k.copy(dstb[:, 0:dd], src[:, 0:dd])
        src, dstb = dstb, src
        dd *= 2
    Fc = src
    k.tt(G8, I8, Fc, ALU.subtract)
    k.ts(G8, G8, -LN_SQRT_DK, None, op0=ALU.add)
    pbt = ps[0]
    for j in range(16):
        k.transpose(pbt[:, j * 8:(j + 1) * 8], G8[:, j * 128:(j + 1) * 128], ident[0:8, 0:8])
    k.copy(biasT[:], pbt[:, 0:128])

    Qc = AR[:, 0:1024]; Kc = AR[:, 1024:3072]
    Pt = [[AR[:, 3072 + (c * 2 + i) * 512:3072 + (c * 2 + i + 1) * 512] for i in range(2)] for c in range(2)]
    Vh = AR[:, 5120:9216].rearrange("p (j c) -> p j c", c=256)
    onesR = AR[:, 9216:9344]
    k.copy(onesR, ones[:])
    ctmp = ov(0, 2048)
    FbS = [ov(2048 + c * 512, 2048 + (c + 1) * 512) for c in range(2)]
    Dt = [[ov(3072 + (c * 2 + i) * 512, 3072 + (c * 2 + i + 1) * 512) for i in range(2)] for c in range(2)]
    misc = [[ov(5120 + (c * 7 + i) * 512, 5120 + (c * 7 + i + 1) * 512) for i in range(7)] for c in range(2)]
    qpre = ov(12288, 13315); kpre = ov(13315, 15366)
    tmp8 = [G[0:8, 6144 + c * 512:6144 + (c + 1) * 512] for c in range(2)]
    Vst = G[:, 0:4096].rearrange("p (j c) -> p j c", c=256)
    cw = st[:, 16:24]
    for h in range(8):
        k.dma(cw[:, 0:4], convT[h * 128:(h + 1) * 128, :])
        k.dma(cw[:, 4:8], convT[1024 + h * 128:1024 + (h + 1) * 128, :])
        k.dma(qpre, QKm[h * 128:(h + 1) * 128, T - 3:S])
        k.memset(kpre[:, 0:3], 0.0)
        k.dma(kpre[:, 3:2051], QKm[1024 + h * 128:1024 + (h + 1) * 128, :])
        k.dma(Vst, Vm[:, h * 256:(h + 1) * 256].rearrange("(j p) c -> p j c", p=128))
        k.copy(Vh[:, 0:8, :], Vst[:, 0:8, :])
        k.act(Vh[:, 8:16, :], Vst[:, 8:16, :], AF.Copy)
        for (dst_, pre, n, wo) in ((Qc, qpre, T, 0), (Kc, kpre, S, 4)):
            ct = ctmp[:, 0:n]
            k.ts(ct, pre[:, 0:n], cw[:, wo:wo + 1], None, op0=ALU.mult)
            for kk in range(1, 4):
                k.stt(ct, pre[:, kk:kk + n], cw[:, wo + kk:wo + kk + 1], ct, ALU.mult, ALU.add)
            k.act(dst_, ct, AF.Silu)
        nfull = [8, 12]; njs = [12, 16]
        banks2 = [(ps[0], ps[2], ps[3], ps[4]), (ps[1], ps[5], ps[6], ps[7])]
        for tb in range(2):
            pF = banks2[tb][0]
            k.ts(tmp8[tb], Fc[:, T + tb * 512:T + (tb + 1) * 512], ident[0:8, h:h + 1], None, op0=ALU.mult)
            k.mm(pF[:, :], ones[0:8, :], tmp8[tb])
            k.copy(FbS[tb], pF[:, :])
        for j in range(16):
            for tb in range(2):
                nj = njs[tb]
                if j >= nj:
                    continue
                pS, num0, num1, den = banks2[tb]
                k.mm(pS[:, :], Kc[:, j * 128:(j + 1) * 128], Qc[:, tb * 512:(tb + 1) * 512])
                d_ = Dt[tb][j % 2]; p_ = Pt[tb][j % 2]
                k.act(d_, FbS[tb], AF.Exp, bias=biasT[:, j * 8 + h:j * 8 + h + 1])
                if j >= nfull[tb]:
                    jj = j - nfull[tb]
                    k.tt(d_, d_, mmk[:, 384 - 128 * jj:384 - 128 * jj + 512], ALU.mult, eng="pool")
                k.tt(p_, pS[:, :], d_, ALU.mult)
                k.mm(num0[:, :], Vh[:, j, 0:128], p_, start=(j == 0), stop=(j == nj - 1))
                k.mm(num1[:, :], Vh[:, j, 128:256], p_, start=(j == 0), stop=(j == nj - 1))
                k.mm(den[:, :], onesR, p_, start=(j == 0), stop=(j == nj - 1))
        for tb in range(2):
            pS, num0, num1, den = banks2[tb]
            rec, h0, h1, sq, rs_, og, sq1 = misc[tb]
            k.act(rec, den[:, :], AF.Abs)
            k.ts(rec, rec, 1.0, None, op0=ALU.max)
            k.op("dve", "reciprocal", [rec], [rec], rec, rec)
            k.tt(h0, num0[:, :], rec, ALU.mult)
            k.tt(h1, num1[:, :], rec, ALU.mult)
            pq = pS
            k.tt(sq, h0, h0, ALU.mult)
            k.mm(pq[:, :], ones[:], sq, start=True, stop=False)
            k.tt(sq1, h1, h1, ALU.mult)
            k.mm(pq[:, :], ones[:], sq1, start=False, stop=True)
            k.ts(rs_, pq[:, :], 1.0 / 256.0, EPS, op0=ALU.mult, op1=ALU.add)
            k.act(rs_, rs_, AF.Sqrt)
            k.op("dve", "reciprocal", [rs_], [rs_], rs_, rs_)
            e = ebuf().rearrange("p (m t) -> p m t", t=512)
            for c, hc in ((0, h0), (1, h1)):
                k.dma(og, Om[h * 256 + c * 128:h * 256 + (c + 1) * 128, tb * 512:(tb + 1) * 512])
                k.stt(hc, hc, gml[:, h * 2 + c:h * 2 + c + 1], rs_, ALU.mult, ALU.mult)
                k.tt(e[:, c, :], hc, og, ALU.mult)
            k.dma(HM[h * 256:(h + 1) * 256, tb * 512:(tb + 1) * 512].rearrange("(m p) t -> p m t", p=128), e[:, 0:2, :])
    if upto <= 2:
        return k

    k.barrier()
    Qh = AR[:, 0:1024]; Kh = AR[:, 1024:3072]
    Vsh = AR[:, 3072:5120].rearrange("p (j c) -> p j c", c=128)

    def four(base, arena_fn):
        return [[arena_fn(base + (c * 2 + i) * 512, base + (c * 2 + i + 1) * 512) for i in range(2)] for c in range(2)]
    arv = lambda a, b_: AR[:, a:b_]
    SPt = four(5120, arv); SMt = four(7168, arv); At = four(9216, arv)
    utR = AR[:, 11264:11392]; ltR = AR[:, 11392:11520]
    k.copy(utR, ut[:]); k.copy(ltR, lt[:])
    qst = ov(0, 1024); kst = ov(1024, 3072); vst = ov(3072, 5120).rearrange("p (j c) -> p j c", c=128)
    Et = four(5120, ov); ARt = four(7168, ov)
    negm = ov(9216, 10112)
    k.ts(negm, msk[:], 1.0, 1.0e4, op0=ALU.subtract, op1=ALU.mult)
    for h in range(16):
        k.dma(qst, Qs[h * 128:(h + 1) * 128, :])
        k.dma(kst, Ks[h * 128:(h + 1) * 128, :])
        k.dma(vst, Vs[:, h * 128:(h + 1) * 128].rearrange("(j p) c -> p j c", p=128))
        k.copy(Qh, qst)
        k.act(Kh, kst, AF.Copy)
        k.copy(Vsh, vst)
        nfull = [8, 12]; njs = [12, 16]
        for idx_ in range(16):
            for tb in range(2):
                nj = njs[tb]
                if idx_ >= nj:
                    continue
                j = nj - 1 - idx_
                acc = ps[4 + tb]; po = ps[6 + tb]
                pz = ps[2 * tb + idx_ % 2]
                e_ = Et[tb][idx_ % 2]; sp_ = SPt[tb][idx_ % 2]; ar_ = ARt[tb][idx_ % 2]; a_ = At[tb][idx_ % 2]
                k.mm(pz[:, :], Kh[:, j * 128:(j + 1) * 128], Qh[:, tb * 512:(tb + 1) * 512])
                k.act(e_, pz[:, :], AF.Exp, scale=SB_SCALE)
                k.act(sp_, e_, AF.Ln, bias=1.0)
                diag = j >= nfull[tb]
                if diag:
                    jj = j - nfull[tb]
                    sl = slice(384 - 128 * jj, 384 - 128 * jj + 512)
                    spm = SMt[tb][idx_ % 2]
                    k.tt(spm, sp_.bitcast(F32), msk[:, sl], ALU.mult)
                else:
                    spm = sp_
                k.mm(acc[:, :], utR, spm, start=(idx_ == 0), stop=False, skip_group_check=True)
                k.stt(ar_, pz[:, :], SB_SCALE, sp_.bitcast(F32), ALU.mult, ALU.subtract)
                k.tt(ar_, ar_, acc[:, :], ALU.subtract)
                if diag:
                    k.tt(ar_, ar_, negm[:, sl], ALU.add, eng="pool")
                k.act(a_, ar_, AF.Exp)
                k.mm(acc[:, :], ltR, spm, start=False, stop=(idx_ == nj - 1), skip_group_check=True)
                k.mm(po[:, :], Vsh[:, j, :], a_, start=(idx_ == 0), stop=(idx_ == nj - 1))
        for tb in range(2):
            e = ebuf()
            evac(e[:, 0:512], ps[6 + tb][:, :])
            k.dma(HS[h * 128:(h + 1) * 128, tb * 512:(tb + 1) * 512], e[:, 0:512])
    if upto <= 3:
        return k

    k.barrier()
    yT = AR[:, :].rearrange("p (c t) -> p c t", t=512)
    gtile = ov(0, 2048).rearrange("p (m t) -> p m t", t=512)
    xres = ov(2048, 4096).rearrange("p (m t) -> p m t", t=512)

    def resid_store(src_res, dst, t0, c0):
        def f(banks):
            k.dma(xres, src_res[t0:t0 + 512, c0:c0 + 512].rearrange("(m p) c -> p m c", p=128))
            e = ebuf().rearrange("p (m t) -> p m t", t=512)
            for m in range(4):
                k.tt(e[:, m, :], banks[m][:, :], xres[:, m, :], ALU.add)
            k.dma(dst[t0:t0 + 512, c0:c0 + 512].rearrange("(m p) c -> p m c", p=128), e)
        return f

    for tb in range(2):
        t0 = tb * 512
        for pi, (hsrc, wsrc, gsrc) in enumerate(((HM, w_pa, GA), (HS, w_pb, GB))):
            for cb in range(8):
                banks = bankset()
                for kq in range(4):
                    at = load_w(hsrc[kq * 512:(kq + 1) * 512, t0:t0 + 512])
                    wt = load_w(wsrc[kq * 512:(kq + 1) * 512, cb * 512:(cb + 1) * 512])
                    for m in range(4):
                        for kc in range(4):
                            kk = kq * 4 + kc
                            k.mm(banks[m][:, :], wt[:, kc, m * 128:(m + 1) * 128], at[:, kc, :], start=(kk == 0), stop=(kk == 15))
                k.dma(gtile, gsrc[cb * 512:(cb + 1) * 512, t0:t0 + 512].rearrange("(m p) t -> p m t", p=128))
                for m in range(4):
                    dsty = yT[:, cb * 4 + m, :]
                    if pi == 0:
                        k.tt(dsty, banks[m][:, :], gtile[:, m, :], ALU.mult)
                    else:
                        k.tt(gtile[:, m, :], banks[m][:, :], gtile[:, m, :], ALU.mult)
                        k.tt(dsty, dsty.bitcast(F32), gtile[:, m, :], ALU.add)
        for cb in range(8):
            proj(yT, 32, w_out, cb * 512, 512, "tm", resid_store(xs[T:S, :], H1, t0, cb * 512))
    if upto <= 4:
        return k

    k.barrier()
    memT = AR[:, 0:8192].rearrange("p (c t) -> p c t", t=256)
    for i in range(2):
        norm_tile(memx[i * 128:(i + 1) * 128, :], gmem, memT, i * 128)
    for cb in range(8):
        banks = bankset()
        for kq in range(8):
            wt = load_w(w_kv[kq * 512:(kq + 1) * 512, cb * 512:(cb + 1) * 512])
            for m in range(4):
                for kc in range(4):
                    kk = kq * 4 + kc
                    k.mm(banks[m][:, 0:256], wt[:, kc, m * 128:(m + 1) * 128], memT[:, kk, :], start=(kk == 0), stop=(kk == 31))
        e = ebuf().rearrange("p (m t) -> p m t", t=512)
        for m in range(4):
            evac(e[:, m, 0:256], banks[m][:, 0:256])
        k.dma(KX[cb * 512:(cb + 1) * 512, :].rearrange("(m p) t -> p m t", p=128), e[:, :, 0:256])
    for cb in range(8):
        banks = bankset()
        for kq in range(8):
            wt = load_w(w_kv[kq * 512:(kq + 1) * 512, D + cb * 512:D + (cb + 1) * 512])
            for m in range(2):
                for kc in range(4):
                    kk = kq * 4 + kc
                    k.mm(banks[m][:, :], memT[:, kk, m * 128:(m + 1) * 128], wt[:, kc, :], start=(kk == 0), stop=(kk == 31))
        e = ebuf().rearrange("p (m t) -> p m t", t=512)
        for m in range(2):
            evac(e[:, m, :], banks[m][:, :])
        k.dma(VX[:, cb * 512:(cb + 1) * 512].rearrange("(m p) c -> p m c", p=128), e[:, 0:2, :])

    oT = AR[:, :].rearrange("p (c t) -> p c t", t=512)
    qh = ov(0, 4096).rearrange("p (c t) -> p c t", t=512)
    khT = ov(4096, 6144).rearrange("p (c m) -> p c m", m=256)
    vh = ov(6144, 8192).rearrange("p (m d) -> p m d", d=1024)
    pT = ov(8192, 9216).rearrange("p (m t) -> p m t", t=512)
    pbuf = [ov(9216 + i * 256, 9216 + (i + 1) * 256) for i in range(2)]
    xres = ov(12288, 14336).rearrange("p (m t) -> p m t", t=512)
    for tb in range(2):
        t0 = tb * 512
        for i in range(4):
            norm_tile(H1[t0 + i * 128:t0 + (i + 1) * 128, :], gx, xnT, i * 128)
        for cb in range(8):
            proj(xnT, 32, w_q, cb * 512, 512, "fm", store_fm(QX, cb * 512, t0))
        for h in range(4):
            k.dma(qh, QX[h * 1024:(h + 1) * 1024, t0:t0 + 512].rearrange("(c p) t -> p c t", p=128))
            k.dma(khT, KX[h * 1024:(h + 1) * 1024, :].rearrange("(c p) m -> p c m", p=128))
            k.dma(vh, VX[:, h * 1024:(h + 1) * 1024].rearrange("(m p) d -> p m d", p=128))
            for i in range(4):
                psc = ps[i % 2]
                for c in range(8):
                    k.mm(psc[:, 0:256], qh[:, c, i * 128:(i + 1) * 128], khT[:, c, :], start=(c == 0), stop=(c == 7))
                mx = st[:, 32:33]; nb_ = st[:, 33:34]; sm = st[:, 34:35]; rsm = st[:, 35:36]
                k.reduce(mx, psc[:, 0:256], ALU.max)
                k.ts(nb_, mx, -1.0 / 32.0, None, op0=ALU.mult)
                pb_ = pbuf[i % 2]
                k.memset(sm, 0.0)
                k.act(pb_, psc[:, 0:256], AF.Exp, bias=nb_, scale=1.0 / 32.0, accum_out=sm)
                k.op("dve", "reciprocal", [rsm], [sm], rsm, sm)
                k.ts(pb_, pb_, rsm, None, op0=ALU.mult)
                ptp = ps[2 + i % 2]
                for m in range(2):
                    k.transpose(ptp[:, m * 128:(m + 1) * 128], pb_[:, m * 128:(m + 1) * 128], ident[:])
                k.copy(pT[:, :, i * 128:(i + 1) * 128], ptp[:, 0:256].rearrange("p (m t) -> p m t", t=128))
            for dc in range(8):
                pso = ps[4 + dc % 4]
                for m in range(2):
                    k.mm(pso[:, :], vh[:, m, dc * 128:(dc + 1) * 128], pT[:, m, :], start=(m == 0), stop=(m == 1))
                evac(oT[:, h * 8 + dc, :], pso[:, :])
        for cb in range(8):
            proj(oT, 32, w_o, cb * 512, 512, "tm", resid_store(H1, H2, t0, cb * 512))
    if upto <= 5:
        return k

    k.barrier()
    RB = 12288
    RL = ov(RB, RB + 576).rearrange("p (i c) -> p i c", c=72)
    Aasg = ov(RB + 576, RB + 1088).rearrange("p (i c) -> p i c", c=64)
    OH1 = ov(RB + 1088, RB + 1600).rearrange("p (i c) -> p i c", c=64)
    OH2 = ov(RB + 1600, RB + 2112).rearrange("p (i c) -> p i c", c=64)
    POS = ov(RB + 2112, RB + 2624).rearrange("p (i c) -> p i c", c=64)
    rw = ov(RB + 2624, RB + 3136)
    CW = ov(RB + 3136, RB + 3152).rearrange("p (i c) -> p i c", c=2)
    RI = ov(RB + 3152, RB + 3168).bitcast(I32).rearrange("p (i c) -> p i c", c=2)
    hb = ov(RB + 3168, RB + 3680)
    bR = ov(RB + 3680, RB + 3752); erow = ov(RB + 3752, RB + 3816); iota = ov(RB + 3816, RB + 3944); ltpos = ov(RB + 3944, RB + 4072)
    k.dma(bR, b_r); k.dma(erow, c_erow); k.dma(iota, c_iota); k.dma(ltpos, c_ltpos)
    wr = G[:, 0:2304].rearrange("p (kc c) -> p kc c", c=72)
    for tb in range(2):
        for i in range(4):
            r0 = tb * 512 + i * 128
            norm_tile(H2[r0:r0 + 128, :], gmoe, xnT, i * 128, xhat_dst=XH[r0:r0 + 128, :])
        k.dma(wr, w_r.rearrange("(kc p) c -> p kc c", p=128))
        for i in range(4):
            it = tb * 4 + i
            pr = ps[i % 4]
            for kk in range(32):
                k.mm(pr[:, 0:72], xnT[:, kk, i * 128:(i + 1) * 128].bitcast(F32), wr[:, kk, :], start=(kk == 0), stop=(kk == 31))
            k.tt(RL[:, it, :], pr[:, 0:72], bR, ALU.add)
    s_ = lambda a, b_: st[:, a:b_]
    for it in range(8):
        lg = RL[:, it, 0:8]; le = RL[:, it, 8:72]
        m1 = s_(40, 41); nm1 = s_(41, 42); sg = s_(42, 43); gw = s_(43, 44); m1e = s_(44, 45); m2e = s_(45, 46)
        dl = s_(46, 47); w1 = s_(47, 48); w2 = s_(48, 49)
        ohg = rw[:, 0:8]; pen = rw[:, 8:16]; egx = rw[:, 16:24]; lem = rw[:, 64:128]; lem2 = rw[:, 128:192]
        k.reduce(m1, lg, ALU.max)
        k.ts(ohg, lg, m1, None, op0=ALU.is_equal)
        k.ts(nm1, m1, -1.0, None, op0=ALU.mult)
        k.memset(sg, 0.0)
        k.act(egx, lg, AF.Exp, bias=nm1, accum_out=sg)
        k.op("dve", "reciprocal", [gw], [sg], gw, sg)
        k.ts(pen, ohg, 1.0, 1e30, op0=ALU.subtract, op1=ALU.mult)
        k.tt(lem.rearrange("p (g e) -> p g e", e=8), le.rearrange("p (g e) -> p g e", e=8),
             pen.unsqueeze(2).to_broadcast([128, 8, 8]), ALU.add)
        k.reduce(m1e, lem, ALU.max)
        k.ts(OH1[:, it, :], lem, m1e, None, op0=ALU.is_equal)
        k.stt(lem2, OH1[:, it, :], -1e30, lem, ALU.mult, ALU.add)
        k.reduce(m2e, lem2, ALU.max)
        k.ts(OH2[:, it, :], lem2, m2e, None, op0=ALU.is_equal)
        k.tt(dl, m2e, m1e, ALU.subtract)
        k.act(dl, dl, AF.Exp)
        k.ts(w1, dl, 1.0, None, op0=ALU.add)
        k.op("dve", "reciprocal", [w1], [w1], w1, w1)
        k.tt(w2, dl, w1, ALU.mult)
        k.tt(CW[:, it, 0:1], w1, gw, ALU.mult)
        k.tt(CW[:, it, 1:2], w2, gw, ALU.mult)
        k.tt(Aasg[:, it, :], OH1[:, it, :], OH2[:, it, :], ALU.add)
    for it in range(8):
        pp = ps[4 + it % 4]
        for i2 in range(it):
            k.mm(pp[:, 0:64], ones[:], Aasg[:, i2, :], start=(i2 == 0), stop=False)
        k.mm(pp[:, 0:64], ltpos, Aasg[:, it, :], start=(it == 0), stop=True)
        k.copy(POS[:, it, :], pp[:, 0:64])
        t64 = rw[:, 192:256]; t64b = rw[:, 256:320]; rf = s_(50, 51)
        k.tt(t64, POS[:, it, :], erow, ALU.add)
        for kk_, OH in ((0, OH1), (1, OH2)):
            k.tt(t64b, t64, OH[:, it, :], ALU.mult)
            k.reduce(rf, t64b, ALU.add)
            k.copy(RI[:, it, kk_:kk_ + 1], rf)

    tokid = ov(RB + 2624 + 384, RB + 2624 + 392)
    IDX = ov(RB + 2624 + 320, RB + 2624 + 384).bitcast(I32)
    k.dma(tokid, c_tokid)
    SelF = [ov(i * 1024, (i + 1) * 1024).rearrange("p (i s) -> p i s", s=128) for i in range(2)]
    pidx = ps[7]
    for e in range(NEXP):
        sf = SelF[e % 2]
        for it in range(8):
            k.ts(sf[:, it, :], iota, POS[:, it, e:e + 1], Aasg[:, it, e:e + 1], op0=ALU.is_equal, op1=ALU.mult)
        for it in range(8):
            k.mm(pidx[:, e:e + 1], sf[:, it, :], tokid[:, it:it + 1], start=(it == 0), stop=(it == 7))
    k.copy(IDX, pidx[:, 0:64])

    XeTs = [AR[:, i * 4096:(i + 1) * 4096].rearrange("p (c s) -> p c s", s=128) for i in range(2)]
    hbT = AR[:, 8192:8704].rearrange("p (f s) -> p f s", s=128)
    xes = [ov(2048, 6144), ov(6144, 10240)]
    for e in range(NEXP):
        xe = xes[e % 2]
        XeT = XeTs[e % 2]
        k.dma(xe, XH, q="pool", extra_reads=[IDX[:, e:e + 1]], _meth="indirect_dma_start", out_offset=None,
              in_offset=bass.IndirectOffsetOnAxis(ap=IDX[:, e:e + 1], axis=0))
        for c4 in range(8):
            pt = ps[4 + c4 % 3]
            for ci in range(4):
                c = c4 * 4 + ci
                k.transpose(pt[:, ci * 128:(ci + 1) * 128], xe[:, c * 128:(c + 1) * 128], ident[:])
            k.tt(XeT[:, c4 * 4:c4 * 4 + 4, :], pt[:].rearrange("p (a b) -> p a b", b=128),
                 gmoe[:, c4 * 4:c4 * 4 + 4].unsqueeze(2).to_broadcast([128, 4, 128]), ALU.mult)
        pg, pu, pt_ = ps[0], ps[1], ps[2]
        for wsrc, pacc in ((w_g, pg), (w_u, pu)):
            for kq in range(8):
                wt = load_w(wsrc[e * D + kq * 512:e * D + (kq + 1) * 512, :])
                for kc in range(4):
                    kk = kq * 4 + kc
                    k.mm(pacc[:, :], XeT[:, kk, :], wt[:, kc, :], start=(kk == 0), stop=(kk == 31))
        k.act(hb, pg[:, :], AF.Silu)
        k.tt(hb, hb, pu[:, :], ALU.mult)
        for fc in range(4):
            k.transpose(pt_[:, fc * 128:(fc + 1) * 128], hb[:, fc * 128:(fc + 1) * 128], ident[:])
        k.copy(hbT, pt_[:, :].rearrange("p (f s) -> p f s", s=128))
        for dq2 in range(4):
            ee = ebuf()
            for hf in range(2):
                dq = dq2 * 2 + hf
                yb_ = ps[3] if (dq % 2 == 0) else ps[7]
                wd = load_w(w_d[e * 512:(e + 1) * 512, dq * 512:(dq + 1) * 512])
                for fc in range(4):
                    k.mm(yb_[:, :], hbT[:, fc, :], wd[:, fc, :], start=(fc == 0), stop=(fc == 3))
                evac(ee[:, hf * 512:(hf + 1) * 512], yb_[:, :])
            k.dma(YB[e * CAP:(e + 1) * CAP, dq2 * 1024:(dq2 + 1) * 1024], ee[:, 0:1024])

    Y1 = ov(0, 4096); Y2 = ov(4096, 8192); junk = ov(8192, 12288)
    gfin = G[:, 0:4096]; h2t = G[:, 4096:8192]
    k.dma(gfin, g_fin)
    for it in range(8):
        r0 = it * 128
        k.dma(h2t, H2[r0:r0 + 128, :])
        for kk_, Y in ((0, Y1), (1, Y2)):
            k.dma(Y, YB, q="pool", extra_reads=[RI[:, it, kk_:kk_ + 1]], _meth="indirect_dma_start", out_offset=None,
                  in_offset=bass.IndirectOffsetOnAxis(ap=RI[:, it, kk_:kk_ + 1], axis=0))
            k.stt(h2t, Y, CW[:, it, kk_:kk_ + 1], h2t, ALU.mult, ALU.add)
        ss = s_(52, 53); rs = s_(53, 54); tmp = s_(54, 55)
        k.memset(ss, 0.0)
        k.act(junk, h2t, AF.Square, accum_out=ss)
        rstd_from_ss(ss, rs, 1.0 / D, tmp)
        k.ts(h2t, h2t, rs, None, op0=ALU.mult)
        k.tt(h2t, h2t, gfin, ALU.mult)
        k.dma(out[r0:r0 + 128, :], h2t)
    return k


def _consts():
    f = np.float32
    i = np.arange(128)
    c = {}
    c["c_ident"] = np.eye(128, dtype=f)
    c["c_ones"] = np.ones((128, 128), f)
    c["c_ut"] = (i[:, None] > i[None, :]).astype(f)
    c["c_lt"] = (i[:, None] <= i[None, :]).astype(f)
    u = np.arange(896)
    c["c_mm"] = ((u[None, :] - 384) >= i[:, None]).astype(f)
    c["c_ms"] = ((u[None, :] - 384) > i[:, None]).astype(f)
    c["c_ltpos"] = (i[:, None] < i[None, :]).astype(f)
    c["c_iota"] = np.broadcast_to(np.arange(128, dtype=f)[None, :], (128, 128)).copy()
    c["c_tokid"] = (np.arange(8, dtype=f)[None, :] * 128.0 + np.arange(128, dtype=f)[:, None]).copy()
    c["c_erow"] = np.broadcast_to((np.arange(64, dtype=f) * 128.0)[None, :], (128, 64)).copy()
    return c


def _gl(g, n):
    return np.ascontiguousarray(np.asarray(g, np.float32).reshape(n, 128).T)


def make_in_maps(inp, cores=range(8), moe=True):
    f = np.float32
    x = np.asarray(inp["x"], f); mem = np.asarray(inp["mem"], f)
    sh = _consts()
    sh["w_in"] = np.asarray(inp["w_in"], f)[0]
    sh["w_proj_a"] = np.asarray(inp["w_proj_a"], f)[0]
    sh["w_proj_b"] = np.asarray(inp["w_proj_b"], f)[0]
    sh["w_out"] = np.asarray(inp["w_out"], f)[0]
    sh["w_q_mem"] = np.asarray(inp["w_q_mem"], f)[0]
    sh["w_kv_mem"] = np.asarray(inp["w_kv_mem"], f)[0]
    sh["w_o_mem"] = np.asarray(inp["w_o_mem"], f)[0]
    if moe:
        sh["w_gate"] = np.asarray(inp["w_gate"], f)[0].reshape(NEXP * D, 512)
        sh["w_up"] = np.asarray(inp["w_up"], f)[0].reshape(NEXP * D, 512)
        sh["w_down"] = np.asarray(inp["w_down"], f)[0].reshape(NEXP * 512, D)
    sh["w_r"] = np.ascontiguousarray(np.concatenate([np.asarray(inp["w_router_group"], f)[0], np.asarray(inp["w_router_expert"], f)[0]], axis=1))
    br = np.concatenate([np.asarray(inp["b_router_group"], f)[0], np.asarray(inp["b_router_expert"], f)[0]])
    sh["b_r"] = np.broadcast_to(br[None, :], (128, 72)).copy()
    sh["g_mix"] = _gl(inp["norm_mix"][0], 32); sh["g_x"] = _gl(inp["norm_xattn"][0], 32)
    sh["g_mem"] = _gl(inp["norm_mem"][0], 32); sh["g_moe"] = _gl(inp["norm_moe"][0], 32)
    sh["g_fin"] = np.broadcast_to(np.asarray(inp["norm_final"], f)[None, :], (128, D)).copy()
    sh["g_ml"] = _gl(inp["g_mlstm"][0], 16)
    sh["convT"] = np.ascontiguousarray(np.asarray(inp["conv_qk"], f)[0].T)
    bg = np.asarray(inp["b_gates"], f)[0]
    sh["b_i"] = bg[:8].reshape(8, 1).copy(); sh["b_f"] = bg[8:].reshape(8, 1).copy()
    maps = []
    for c in cores:
        b, half = c // 2, c % 2
        xs_ = np.zeros((S, D), f)
        if half == 1:
            xs_[:] = x[b]
        else:
            xs_[T:] = x[b, :T]
        m = dict(sh)
        m["xs"] = xs_
        m["mem"] = np.ascontiguousarray(mem[b])
        maps.append(m)
    return maps


def kernel(**inputs):
    kb = build()
    nc = kb.finish()
    maps = make_in_maps(inputs)
    res = run_bass_kernel_spmd(nc, maps, core_ids=list(range(8)))
    outp = np.zeros((4, S, D), np.float32)
    for c in range(8):
        b, half = c // 2, c % 2
        outp[b, half * T:(half + 1) * T] = res.results[c]["out"]
    return outp
```

```python
import numpy as np
from contextlib import ExitStack
import concourse.bass as bass
import concourse.mybir as mybir
from concourse.bass_utils import run_bass_kernel_spmd

F32 = mybir.dt.float32
F32R = mybir.dt.float32r
I32 = mybir.dt.int32
ALU = mybir.AluOpType
AF = mybir.ActivationFunctionType
AX = mybir.AxisListType

ENGS = ("pe", "act", "dve", "pool", "sp")
NDMASEM = {"sp": 24, "pool": 8, "act": 8}


def _region(ap):
    t = ap.tensor
    shape = list(t.shape)
    rowsize = 1
    for s in shape[1:]:
        rowsize *= int(s)
    off = int(ap.offset)
    r_lo, c_lo = off // rowsize, off % rowsize
    r_ext, c_ext = 0, 0
    for step, cnt in ap.ap:
        step, cnt = int(step), int(cnt)
        if cnt <= 1 or step == 0:
            continue
        if step % rowsize == 0:
            r_ext += (cnt - 1) * (step // rowsize)
        else:
            c_ext += (cnt - 1) * abs(step)
    return (t.name, r_lo, r_lo + r_ext + 1, c_lo, c_lo + c_ext + 1)


def _overlap(a, b):
    return a[1] < b[2] and b[1] < a[2] and a[3] < b[4] and b[3] < a[4]


def _covers(a, b):
    return a[1] <= b[1] and a[2] >= b[2] and a[3] <= b[3] and a[4] >= b[4]


class K:
    def __init__(self):
        self.nc = bass.Bass("TRN2", target_bir_lowering=False)
        self.es = ExitStack()
        self.streams = {e: [] for e in ENGS}
        self.cnt = {e: 0 for e in ENGS}
        self.esem = {}
        for e in ("pe", "act", "dve", "pool"):
            self.esem[e] = self.es.enter_context(self.nc.semaphore("sem_" + e))
        self.dsem = {}
        self.dcnt = {}
        self.dlast = {}
        for q, n in NDMASEM.items():
            self.dsem[q] = [self.es.enter_context(self.nc.semaphore("dsem_%s_%d" % (q, i))) for i in range(n)]
            self.dcnt[q] = 0
        self.track = {}
        self.waited = {e: {} for e in ENGS}
        self.n_wait = 0
        self.out_tokens = []

    def sbuf(self, name, shape, dt=F32):
        return self.es.enter_context(self.nc.sbuf_tensor(name, list(shape), dt))

    def psum(self, name, shape, dt=F32):
        return self.es.enter_context(self.nc.psum_tensor(name, list(shape), dt))

    def dram(self, name, shape, dt=F32, kind="Internal"):
        return self.nc.dram_tensor(name, list(shape), dt, kind=kind).ap()

    def _semkey(self, tok):
        if tok[0] == "c":
            return ("c", tok[1]), tok[2]
        return ("d", tok[1], tok[2]), tok[3]

    def _need_wait(self, eng, tok, kind_raw, is_dma=False):
        if tok[0] == "c" and tok[1] == eng and not is_dma:
            if eng == "pe":
                return False
            if not kind_raw:
                return False
        key, val = self._semkey(tok)
        return self.waited[eng].get(key, 0) < val

    def _add_wait(self, eng, tok, waits):
        key, val = self._semkey(tok)
        if self.waited[eng].get(key, 0) >= val:
            return
        self.waited[eng][key] = val
        if tok[0] == "c":
            sem = self.esem[tok[1]]
        else:
            sem = self.dsem[tok[1]][tok[2]]
        waits.append((sem, val))

    def _deps(self, eng, reads, writes, is_dma=False):
        waits = []
        for r in reads:
            for ent in self.track.get(r[0], ()):
                if ent[1] == "w" and _overlap(ent[0], r):
                    if self._need_wait(eng, ent[2], True, is_dma):
                        self._add_wait(eng, ent[2], waits)
        for w in writes:
            for ent in self.track.get(w[0], ()):
                if _overlap(ent[0], w):
                    if self._need_wait(eng, ent[2], False, is_dma):
                        self._add_wait(eng, ent[2], waits)
        return waits

    def _record(self, reads, writes, tok):
        for w in writes:
            lst = self.track.setdefault(w[0], [])
            lst[:] = [e for e in lst if not _covers(w, e[0])]
            lst.append([w, "w", tok])
        for r in reads:
            lst = self.track.setdefault(r[0], [])
            if tok[0] == "c":
                lst[:] = [e for e in lst if not (e[1] == "r" and e[2][0] == "c" and e[2][1] == tok[1] and _covers(r, e[0]))]
            lst.append([r, "r", tok])

    def op(self, eng, meth, writes, reads, *args, **kw):
        wr = [_region(a) for a in writes]
        rr = [_region(a) for a in reads]
        waits = self._deps(eng, rr, wr)
        self.cnt[eng] += 1
        tok = ("c", eng, self.cnt[eng])
        self._record(rr, wr, tok)
        self.streams[eng].append((waits, meth, args, kw, (self.esem[eng], 1)))
        self.n_wait += len(waits)
        return tok

    def dma(self, out, in_, q="sp", extra_reads=(), **kw):
        wr = [_region(out)]
        rr = [_region(in_)] + [_region(a) for a in extra_reads]
        waits = self._deps(q, rr, wr, True)
        i = self.dcnt[q]
        n = len(self.dsem[q])
        slot, gen = i % n, i // n
        if gen > 0:
            self._add_wait(q, ("d", q, slot, 16 * gen), waits)
        self.dcnt[q] += 1
        tok = ("d", q, slot, 16 * (gen + 1))
        self._record(rr, wr, tok)
        meth = kw.pop("_meth", "dma_start")
        self.streams[q].append((waits, meth, (), dict(out=out, in_=in_, **kw), (self.dsem[q][slot], 16)))
        return tok

    def wait_tok(self, eng, tok):
        waits = []
        self._add_wait(eng, tok, waits)
        if waits:
            self.streams[eng].append((waits, None, (), {}, None))

    def barrier(self):
        toks = []
        for e in ("pe", "act", "dve", "pool"):
            if self.cnt[e] > 0:
                toks.append(("c", e, self.cnt[e]))
        for q in self.dsem:
            n = len(self.dsem[q])
            for i in range(max(0, self.dcnt[q] - n), self.dcnt[q]):
                toks.append(("d", q, i % n, 16 * (i // n + 1)))
        for e in ENGS:
            for t in toks:
                if t[0] == "c" and t[1] == e:
                    continue
                self.wait_tok(e, t)
        self.track = {}

    def mm(self, out, lhsT, rhs, start=True, stop=True, **kw):
        return self.op("pe", "matmul", [out], [lhsT, rhs], out, lhsT, rhs, start=start, stop=stop, **kw)

    def transpose(self, out, in_, ident):
        return self.op("pe", "transpose", [out], [in_, ident], out, in_, ident)

    def act(self, out, in_, func, bias=None, scale=None, accum_out=None, eng="act"):
        reads = [in_]
        kw = {}
        if bias is not None:
            kw["bias"] = bias
            if not isinstance(bias, (int, float)):
                reads.append(bias)
        if scale is not None:
            kw["scale"] = scale
            if not isinstance(scale, (int, float)):
                reads.append(scale)
        writes = [out]
        if accum_out is not None:
            kw["accum_out"] = accum_out
            writes.append(accum_out)
        return self.op(eng, "activation", writes, reads, out, in_, func, **kw)

    def tt(self, out, in0, in1, op, eng="dve"):
        return self.op(eng, "tensor_tensor", [out], [in0, in1], out, in0, in1, op)

    def ts(self, out, in0, s1, s2=None, op0=ALU.mult, op1=None, eng="dve", accum_out=None):
        reads = [in0]
        if not isinstance(s1, (int, float)):
            reads.append(s1)
        if s2 is not None and not isinstance(s2, (int, float)):
            reads.append(s2)
        kw = {}
        if op1 is not None:
            kw["op1"] = op1
        writes = [out]
        if accum_out is not None:
            kw["accum_out"] = accum_out
            writes.append(accum_out)
        return self.op(eng, "tensor_scalar", writes, reads, out, in0, s1, s2, op0, **kw)

    def stt(self, out, in0, scalar, in1, op0, op1, eng="dve"):
        reads = [in0, in1]
        if not isinstance(scalar, (int, float)):
            reads.append(scalar)
        return self.op(eng, "scalar_tensor_tensor", [out], reads, out, in0, scalar, in1, op0, op1)

    def copy(self, out, in_, eng="dve"):
        return self.op(eng, "tensor_copy", [out], [in_], out, in_)

    def memset(self, out, val, eng="dve"):
        return self.op(eng, "memset", [out], [], out, val)

    def reduce(self, out, in_, op, axis=AX.X, eng="dve"):
        return self.op(eng, "tensor_reduce", [out], [in_], out, in_, axis, op)

    def finish(self):
        nc = self.nc
        eng_obj = {"pe": "tensor", "act": "scalar", "dve": "vector", "pool": "gpsimd", "sp": "sync"}
        self.barrier()
        with nc.Block() as block:
            for e in ENGS:
                items = self.streams[e]

                def body(eo, items=items):
                    for waits, meth, args, kw, inc in items:
                        for sem, val in waits:
                            eo.wait_ge(sem, val)
                        if meth is None:
                            continue
                        ins = getattr(eo, meth)(*args, **kw)
                        if inc is not None:
                            ins.then_inc(inc[0], inc[1])
                getattr(block, eng_obj[e])(body)
        return nc

    def stats(self):
        return {e: len(self.streams[e]) for e in ENGS}, self.n_wait

import math
D = 4096
T = 1024
S = 2048
EPS = 1e-6
C_QM, C_KM, C_VM, C_OM, C_I, C_F, C_QS, C_KS, C_VS, C_GA, C_GB = 0, 1024, 2048, 4096, 6144, 6152, 6160, 8208, 10256, 12304, 16400
SB_SCALE = 1.0 / math.sqrt(128.0)
LN_SQRT_DK = 0.5 * math.log(128.0)
NEXP = 64
CAP = 128


def build(upto=99, dbg=()):
    k = K()

    def ext(n, shp, dt=F32):
        return k.dram(n, shp, dt, kind="ExternalInput")

    def scr(n, shp, dt=F32):
        return k.dram(n, shp, dt, kind=("ExternalOutput" if n in dbg else "Internal"))

    xs = ext("xs", [S, D]); memx = ext("mem", [256, D])
    w_in = ext("w_in", [D, 20496]); w_pa = ext("w_proj_a", [2048, D]); w_pb = ext("w_proj_b", [2048, D])
    w_out = ext("w_out", [D, D]); w_q = ext("w_q_mem", [D, D]); w_kv = ext("w_kv_mem", [D, 2 * D]); w_o = ext("w_o_mem", [D, D])
    if upto >= 6:
        w_g = ext("w_gate", [NEXP * D, 512]); w_u = ext("w_up", [NEXP * D, 512]); w_d = ext("w_down", [NEXP * 512, D])
    w_r = ext("w_r", [D, 72]); b_r = ext("b_r", [128, 72])
    g_mix = ext("g_mix", [128, 32]); g_x = ext("g_x", [128, 32]); g_mem = ext("g_mem", [128, 32]); g_moe = ext("g_moe", [128, 32])
    g_fin = ext("g_fin", [128, D]); g_ml = ext("g_ml", [128, 16])
    convT = ext("convT", [2048, 4]); b_i = ext("b_i", [8, 1]); b_f = ext("b_f", [8, 1])
    c_ident = ext("c_ident", [128, 128]); c_ones = ext("c_ones", [128, 128]); c_ut = ext("c_ut", [128, 128]); c_lt = ext("c_lt", [128, 128])
    c_mm = ext("c_mm", [128, 896]); c_ms = ext("c_ms", [128, 896])
    c_ltpos = ext("c_ltpos", [128, 128]); c_iota = ext("c_iota", [128, 128]); c_erow = ext("c_erow", [128, 64]); c_tokid = ext("c_tokid", [128, 8])
    out = k.dram("out", [T, D], F32, kind="ExternalOutput")

    QKm = scr("QKm", [2048, S]); Vm = scr("Vm", [S, 2048]); Om = scr("Om", [2048, T])
    I8d = scr("I8d", [8, S]); F8d = scr("F8d", [8, S])
    Qs = scr("Qs", [2048, T]); Ks = scr("Ks", [2048, S]); Vs = scr("Vs", [S, 2048])
    GA = scr("GA", [D, T]); GB = scr("GB", [D, T])
    HM = scr("HM", [2048, T]); HS = scr("HS", [2048, T])
    H1 = scr("H1", [T, D]); H2 = scr("H2", [T, D])
    KX = scr("KX", [D, 256]); VX = scr("VX", [256, D]); QX = scr("QX", [D, T])
    XH = scr("XH", [T, D]); YB = scr("YB", [NEXP * CAP, D])

    AR = k.sbuf("AR", [128, 16384], F32R); WR = k.sbuf("WR", [128, 6144], F32R); G = k.sbuf("G", [128, 26624])
    WS = [G[:, i * 2048:(i + 1) * 2048] for i in range(3)]
    WRs = [WR[:, i * 2048:(i + 1) * 2048] for i in range(3)]
    Eb = [G[:, 6144 + i * 2048:6144 + (i + 1) * 2048] for i in range(2)]
    OV0 = 10240

    def ov(a, b_):
        return G[:, OV0 + a:OV0 + b_]

    def ovr(r0, r1, a, b_):
        return G[r0:r1, OV0 + a:OV0 + b_]
    Xb = [ov(0, 4096), ov(4096, 8192)]
    sqjunk = ov(8192, 12288)
    ident = k.sbuf("ident", [128, 128]); ones = k.sbuf("ones", [128, 128]); ut = k.sbuf("ut", [128, 128]); lt = k.sbuf("lt", [128, 128])
    mmk = k.sbuf("mmk", [128, 896]); msk = k.sbuf("msk", [128, 896])
    gmix = k.sbuf("gmix", [128, 32]); gx = k.sbuf("gx", [128, 32]); gmem = k.sbuf("gmem", [128, 32]); gmoe = k.sbuf("gmoe", [128, 32]); gml = k.sbuf("gml", [128, 16])
    st = k.sbuf("st", [128, 64])
    wif = k.sbuf("wif", [128, 32, 16])
    biasT = k.sbuf("biasT", [128, 128])
    ps = [k.psum("ps%d" % i, [128, 512]) for i in range(8)]

    for dst, src in ((ident, c_ident), (ones, c_ones), (ut, c_ut), (lt, c_lt), (mmk, c_mm), (msk, c_ms),
                     (gmix, g_mix), (gx, g_x), (gmem, g_mem), (gmoe, g_moe), (gml, g_ml)):
        k.dma(dst[:], src)
    k.dma(wif[:], w_in[:, C_I:C_I + 16].rearrange("(kc p) c -> p kc c", p=128))

    cnt = {"w": 0, "e": 0, "bank": 0, "x": 0, "t": 0, "alt": 0}

    def load_w(w2d):
        nk = w2d.shape[0] // 128
        nc_ = w2d.shape[1]
        i = cnt["w"] % 3
        cnt["w"] += 1
        sv = WS[i][:, 0:nk * nc_].rearrange("p (kc c) -> p kc c", c=nc_)
        rv = WRs[i][:, 0:nk * nc_].rearrange("p (kc c) -> p kc c", c=nc_)
        k.dma(sv, w2d.rearrange("(kc p) c -> p kc c", p=128))
        h = max(1, nk // 2)
        k.copy(rv[:, 0:h, :], sv[:, 0:h, :])
        if h < nk:
            k.act(rv[:, h:nk, :], sv[:, h:nk, :], AF.Copy)
        return rv

    def bankset():
        b = cnt["bank"] % 2
        cnt["bank"] += 1
        return [ps[b * 4 + i] for i in range(4)]

    def ebuf():
        e = Eb[cnt["e"] % 2]
        cnt["e"] += 1
        return e

    def evac(out_ap, in_ap, func=None):
        if func is not None:
            k.act(out_ap, in_ap, func)
            return
        cnt["alt"] += 1
        if cnt["alt"] % 2:
            k.act(out_ap, in_ap, AF.Copy)
        else:
            k.copy(out_ap, in_ap)

    def rstd_from_ss(ss, rs, scale, tmp):
        k.ts(tmp, ss, scale, EPS, op0=ALU.mult, op1=ALU.add)
        k.act(tmp, tmp, AF.Sqrt)
        k.op("dve", "reciprocal", [rs], [tmp], rs, tmp)

    def norm_tile(src, gT, dstT, col0, xhat_dst=None):
        xb = Xb[cnt["x"] % 2]
        cnt["x"] += 1
        k.dma(xb, src)
        ss = st[:, 0:1]; rs = st[:, 1:2]; tmp = st[:, 2:3]
        k.memset(ss, 0.0)
        k.act(sqjunk, xb, AF.Square, accum_out=ss)
        rstd_from_ss(ss, rs, 1.0 / D, tmp)
        k.ts(xb, xb, rs, None, op0=ALU.mult)
        if xhat_dst is not None:
            k.dma(xhat_dst, xb)
        for c4 in range(8):
            pt = ps[4 + cnt["t"] % 4]
            cnt["t"] += 1
            for ci in range(4):
                c = c4 * 4 + ci
                k.transpose(pt[:, ci * 128:(ci + 1) * 128], xb[:, c * 128:(c + 1) * 128], ident[:])
            k.tt(dstT[:, c4 * 4:c4 * 4 + 4, col0:col0 + 128], pt[:].rearrange("p (a b) -> p a b", b=128),
                 gT[:, c4 * 4:c4 * 4 + 4].unsqueeze(2).to_broadcast([128, 4, 128]), ALU.mult)

    def proj(actT, nK, w2d, c0, ncols, mode, evac_fn):
        banks = bankset()
        nb = ncols // 128 if mode == "fm" else 4
        kstep = 4
        for kq in range(nK // kstep):
            wt = load_w(w2d[kq * kstep * 128:(kq + 1) * kstep * 128, c0:c0 + ncols])
            for m in range(nb):
                for kc in range(kstep):
                    kk = kq * kstep + kc
                    if mode == "fm":
                        k.mm(banks[m][:, :], wt[:, kc, m * 128:(m + 1) * 128], actT[:, kk, :], start=(kk == 0), stop=(kk == nK - 1))
                    else:
                        k.mm(banks[m][:, 0:ncols], actT[:, kk, m * 128:(m + 1) * 128], wt[:, kc, :], start=(kk == 0), stop=(kk == nK - 1))
        evac_fn(banks)

    def store_fm(dst, r0, t0, func=None):
        def f(banks):
            e = ebuf().rearrange("p (m t) -> p m t", t=512)
            for m in range(4):
                evac(e[:, m, :], banks[m][:, :], func)
            k.dma(dst[r0:r0 + 512, t0:t0 + 512].rearrange("(m p) t -> p m t", p=128), e)
        return f

    def store_tm(dst, t0, c0):
        def f(banks):
            e = ebuf().rearrange("p (m t) -> p m t", t=512)
            for m in range(4):
                evac(e[:, m, :], banks[m][:, :])
            k.dma(dst[t0:t0 + 512, c0:c0 + 512].rearrange("(m p) c -> p m c", p=128), e)
        return f

    xnT = AR[:, :].rearrange("p (c t) -> p c t", t=512)

    for tb in range(4):
        own = tb >= 2
        for i in range(4):
            norm_tile(xs[tb * 512 + i * 128: tb * 512 + (i + 1) * 128, :], gmix, xnT, i * 128)
        t0 = tb * 512
        to = t0 - T
        for cb in range(4):
            proj(xnT, 32, w_in, C_QM + cb * 512, 512, "fm", store_fm(QKm, cb * 512, t0))
        for cb in range(4):
            proj(xnT, 32, w_in, C_VM + cb * 512, 512, "tm", store_tm(Vm, t0, cb * 512))
        for cb in range(4):
            proj(xnT, 32, w_in, C_KS + cb * 512, 512, "fm", store_fm(Ks, cb * 512, t0))
        for cb in range(4):
            proj(xnT, 32, w_in, C_VS + cb * 512, 512, "tm", store_tm(Vs, t0, cb * 512))
        for gi, dstd in ((0, I8d), (1, F8d)):
            pb = ps[cnt["bank"] % 8]
            for kk in range(32):
                k.mm(pb[0:8, :], wif[:, kk, gi * 8:(gi + 1) * 8], xnT[:, kk, :].bitcast(F32), start=(kk == 0), stop=(kk == 31))
            e = ebuf()
            evac(e[0:8, 0:512], pb[0:8, :])
            k.dma(dstd[:, t0:t0 + 512], e[0:8, 0:512])
        if own:
            for cb in range(4):
                proj(xnT, 32, w_in, C_OM + cb * 512, 512, "fm", store_fm(Om, cb * 512, to, AF.Sigmoid))
            for cb in range(4):
                proj(xnT, 32, w_in, C_QS + cb * 512, 512, "fm", store_fm(Qs, cb * 512, to))
            for cb in range(8):
                proj(xnT, 32, w_in, C_GA + cb * 512, 512, "fm", store_fm(GA, cb * 512, to, AF.Sigmoid))
            for cb in range(8):
                proj(xnT, 32, w_in, C_GB + cb * 512, 512, "fm", store_fm(GB, cb * 512, to, AF.Sigmoid))
    if upto <= 1:
        return k

    k.barrier()
    I8 = ovr(0, 8, 13000, 15048); Fa = G[0:8, 8192:10240]; Fb_ = G[0:8, 4096:6144]; G8 = G[0:8, 6144:8192]
    bi = st[0:8, 8:9]; bf = st[0:8, 9:10]; nbf = st[0:8, 10:11]
    k.dma(bi, b_i); k.dma(bf, b_f)
    k.dma(I8, I8d); k.dma(Fa, F8d)
    k.ts(nbf, bf, -1.0, None, op0=ALU.mult)
    k.ts(I8, I8, bi, None, op0=ALU.add)
    k.act(Fa, Fa, AF.Exp, bias=nbf, scale=-1.0)
    k.act(Fa, Fa, AF.Ln, bias=1.0)
    k.ts(Fa, Fa, -1.0, None, op0=ALU.mult)
    src, dstb = Fa, Fb_
    dd = 1
    while dd < S:
        k.tt(dstb[:, dd:S], src[:, dd:S], src[:, 0:S - dd], ALU.add)
        k.copy(dstb[:, 0:dd], src[:, 0:dd])
        src, dstb = dstb, src
        dd *= 2
    Fc = src
    k.tt(G8, I8, Fc, ALU.subtract)
    k.ts(G8, G8, -LN_SQRT_DK, None, op0=ALU.add)
    pbt = ps[0]
    for j in range(16):
        k.transpose(pbt[:, j * 8:(j + 1) * 8], G8[:, j * 128:(j + 1) * 128], ident[0:8, 0:8])
    k.copy(biasT[:], pbt[:, 0:128])

    Qc = AR[:, 0:1024]; Kc = AR[:, 1024:3072]
    Pt = [[AR[:, 3072 + (c * 2 + i) * 512:3072 + (c * 2 + i + 1) * 512] for i in range(2)] for c in range(2)]
    Vh = AR[:, 5120:9216].rearrange("p (j c) -> p j c", c=256)
    onesR = AR[:, 9216:9344]
    k.copy(onesR, ones[:])
    ctmp = ov(0, 2048)
    FbS = [ov(2048 + c * 512, 2048 + (c + 1) * 512) for c in range(2)]
    Dt = [[ov(3072 + (c * 2 + i) * 512, 3072 + (c * 2 + i + 1) * 512) for i in range(2)] for c in range(2)]
    misc = [[ov(5120 + (c * 7 + i) * 512, 5120 + (c * 7 + i + 1) * 512) for i in range(7)] for c in range(2)]
    qpre = ov(12288, 13315); kpre = ov(13315, 15366)
    tmp8 = [G[0:8, 6144 + c * 512:6144 + (c + 1) * 512] for c in range(2)]
    Vst = G[:, 0:4096].rearrange("p (j c) -> p j c", c=256)
    cw = st[:, 16:24]
    for h in range(8):
        k.dma(cw[:, 0:4], convT[h * 128:(h + 1) * 128, :])
        k.dma(cw[:, 4:8], convT[1024 + h * 128:1024 + (h + 1) * 128, :])
        k.dma(qpre, QKm[h * 128:(h + 1) * 128, T - 3:S])
        k.memset(kpre[:, 0:3], 0.0)
        k.dma(kpre[:, 3:2051], QKm[1024 + h * 128:1024 + (h + 1) * 128, :])
        k.dma(Vst, Vm[:, h * 256:(h + 1) * 256].rearrange("(j p) c -> p j c", p=128))
        k.copy(Vh[:, 0:8, :], Vst[:, 0:8, :])
        k.act(Vh[:, 8:16, :], Vst[:, 8:16, :], AF.Copy)
        for (dst_, pre, n, wo) in ((Qc, qpre, T, 0), (Kc, kpre, S, 4)):
            ct = ctmp[:, 0:n]
            k.ts(ct, pre[:, 0:n], cw[:, wo:wo + 1], None, op0=ALU.mult)
            for kk in range(1, 4):
                k.stt(ct, pre[:, kk:kk + n], cw[:, wo + kk:wo + kk + 1], ct, ALU.mult, ALU.add)
            k.act(dst_, ct, AF.Silu)
        nfull = [8, 12]; njs = [12, 16]
        banks2 = [(ps[0], ps[2], ps[3], ps[4]), (ps[1], ps[5], ps[6], ps[7])]
        for tb in range(2):
            pF = banks2[tb][0]
            k.ts(tmp8[tb], Fc[:, T + tb * 512:T + (tb + 1) * 512], ident[0:8, h:h + 1], None, op0=ALU.mult)
            k.mm(pF[:, :], ones[0:8, :], tmp8[tb])
            k.copy(FbS[tb], pF[:, :])
        for j in range(16):
            for tb in range(2):
                nj = njs[tb]
                if j >= nj:
                    continue
                pS, num0, num1, den = banks2[tb]
                k.mm(pS[:, :], Kc[:, j * 128:(j + 1) * 128], Qc[:, tb * 512:(tb + 1) * 512])
                d_ = Dt[tb][j % 2]; p_ = Pt[tb][j % 2]
                k.act(d_, FbS[tb], AF.Exp, bias=biasT[:, j * 8 + h:j * 8 + h + 1])
                if j >= nfull[tb]:
                    jj = j - nfull[tb]
                    k.tt(d_, d_, mmk[:, 384 - 128 * jj:384 - 128 * jj + 512], ALU.mult, eng="pool")
                k.tt(p_, pS[:, :], d_, ALU.mult)
            for tb in range(2):
                nj = njs[tb]
                if j >= nj:
                    continue
                pS, num0, num1, den = banks2[tb]
                p_ = Pt[tb][j % 2]
                k.mm(num0[:, :], Vh[:, j, 0:128], p_, start=(j == 0), stop=(j == nj - 1))
                k.mm(num1[:, :], Vh[:, j, 128:256], p_, start=(j == 0), stop=(j == nj - 1))
                k.mm(den[:, :], onesR, p_, start=(j == 0), stop=(j == nj - 1))
        for tb in range(2):
            pS, num0, num1, den = banks2[tb]
            rec, h0, h1, sq, rs_, og, sq1 = misc[tb]
            k.act(rec, den[:, :], AF.Abs)
            k.ts(rec, rec, 1.0, None, op0=ALU.max)
            k.op("dve", "reciprocal", [rec], [rec], rec, rec)
            k.tt(h0, num0[:, :], rec, ALU.mult)
            k.tt(h1, num1[:, :], rec, ALU.mult)
            pq = pS
            k.tt(sq, h0, h0, ALU.mult)
            k.mm(pq[:, :], ones[:], sq, start=True, stop=False)
            k.tt(sq1, h1, h1, ALU.mult)
            k.mm(pq[:, :], ones[:], sq1, start=False, stop=True)
            k.ts(rs_, pq[:, :], 1.0 / 256.0, EPS, op0=ALU.mult, op1=ALU.add)
            k.act(rs_, rs_, AF.Sqrt)
            k.op("dve", "reciprocal", [rs_], [rs_], rs_, rs_)
            e = ebuf().rearrange("p (m t) -> p m t", t=512)
            for c, hc in ((0, h0), (1, h1)):
                k.dma(og, Om[h * 256 + c * 128:h * 256 + (c + 1) * 128, tb * 512:(tb + 1) * 512])
                k.stt(hc, hc, gml[:, h * 2 + c:h * 2 + c + 1], rs_, ALU.mult, ALU.mult)
                k.tt(e[:, c, :], hc, og, ALU.mult)
            k.dma(HM[h * 256:(h + 1) * 256, tb * 512:(tb + 1) * 512].rearrange("(m p) t -> p m t", p=128), e[:, 0:2, :])
    if upto <= 2:
        return k

    k.barrier()
    Qh = AR[:, 0:1024]; Kh = AR[:, 1024:3072]
    Vsh = AR[:, 3072:5120].rearrange("p (j c) -> p j c", c=128)

    def four(base, arena_fn):
        return [[arena_fn(base + (c * 2 + i) * 512, base + (c * 2 + i + 1) * 512) for i in range(2)] for c in range(2)]
    arv = lambda a, b_: AR[:, a:b_]
    SPt = four(5120, arv); SMt = four(7168, arv); At = four(9216, arv)
    utR = AR[:, 11264:11392]; ltR = AR[:, 11392:11520]
    k.copy(utR, ut[:]); k.copy(ltR, lt[:])
    qst = ov(0, 1024); kst = ov(1024, 3072); vst = ov(3072, 5120).rearrange("p (j c) -> p j c", c=128)
    Et = four(5120, ov); ARt = four(7168, ov)
    negm = ov(9216, 10112)
    k.ts(negm, msk[:], 1.0, 1.0e4, op0=ALU.subtract, op1=ALU.mult)
    for h in range(16):
        k.dma(qst, Qs[h * 128:(h + 1) * 128, :])
        k.dma(kst, Ks[h * 128:(h + 1) * 128, :])
        k.dma(vst, Vs[:, h * 128:(h + 1) * 128].rearrange("(j p) c -> p j c", p=128))
        k.copy(Qh, qst)
        k.act(Kh, kst, AF.Copy)
        k.copy(Vsh, vst)
        nfull = [8, 12]; njs = [12, 16]

        def bufs(tb, idx_):
            i2 = idx_ % 2
            return Et[tb][i2], SPt[tb][i2], ARt[tb][i2], At[tb][i2], SMt[tb][i2]

        def tile_of(tb, idx_):
            j = njs[tb] - 1 - idx_
            diag = j >= nfull[tb]
            sl = None
            if diag:
                jj = j - nfull[tb]
                sl = slice(384 - 128 * jj, 384 - 128 * jj + 512)
            return j, diag, sl

        def S1(tb, idx_):
            if idx_ >= njs[tb]:
                return
            j, diag, sl = tile_of(tb, idx_)
            e_, sp_, ar_, a_, sm_ = bufs(tb, idx_)
            pz = ps[2 * tb + idx_ % 2]
            k.mm(pz[:, :], Kh[:, j * 128:(j + 1) * 128], Qh[:, tb * 512:(tb + 1) * 512])
            k.act(e_, pz[:, :], AF.Exp, scale=SB_SCALE)
            k.act(sp_, e_, AF.Ln, bias=1.0)
            k.stt(ar_, pz[:, :], SB_SCALE, sp_.bitcast(F32), ALU.mult, ALU.subtract)
            if diag:
                k.tt(sm_, sp_.bitcast(F32), msk[:, sl], ALU.mult)

        def S2a(tb, idx_):
            if idx_ >= njs[tb]:
                return
            j, diag, sl = tile_of(tb, idx_)
            e_, sp_, ar_, a_, sm_ = bufs(tb, idx_)
            spm = sm_ if diag else sp_
            acc = ps[4 + tb]
            k.mm(acc[:, :], utR, spm, start=(idx_ == 0), stop=False, skip_group_check=True)
            k.tt(ar_, ar_, acc[:, :], ALU.subtract)
            if diag:
                k.tt(ar_, ar_, negm[:, sl], ALU.add, eng="pool")
            k.act(a_, ar_, AF.Exp)

        def S2b(tb, idx_):
            if idx_ >= njs[tb]:
                return
            j, diag, sl = tile_of(tb, idx_)
            e_, sp_, ar_, a_, sm_ = bufs(tb, idx_)
            spm = sm_ if diag else sp_
            acc = ps[4 + tb]; po = ps[6 + tb]
            nj = njs[tb]
            k.mm(acc[:, :], ltR, spm, start=False, stop=(idx_ == nj - 1), skip_group_check=True)
            k.mm(po[:, :], Vsh[:, j, :], a_, start=(idx_ == 0), stop=(idx_ == nj - 1))

        S1(0, 0); S1(1, 0)
        for idx_ in range(16):
            S1(0, idx_ + 1); S1(1, idx_ + 1)
            S2a(0, idx_); S2a(1, idx_)
            S2b(0, idx_); S2b(1, idx_)
        for tb in range(2):
            e = ebuf()
            evac(e[:, 0:512], ps[6 + tb][:, :])
            k.dma(HS[h * 128:(h + 1) * 128, tb * 512:(tb + 1) * 512], e[:, 0:512])
    if upto <= 3:
        return k

    k.barrier()
    yT = AR[:, :].rearrange("p (c t) -> p c t", t=512)
    gtile = ov(0, 2048).rearrange("p (m t) -> p m t", t=512)
    xres = ov(2048, 4096).rearrange("p (m t) -> p m t", t=512)

    def resid_store(src_res, dst, t0, c0):
        def f(banks):
            k.dma(xres, src_res[t0:t0 + 512, c0:c0 + 512].rearrange("(m p) c -> p m c", p=128))
            e = ebuf().rearrange("p (m t) -> p m t", t=512)
            for m in range(4):
                k.tt(e[:, m, :], banks[m][:, :], xres[:, m, :], ALU.add)
            k.dma(dst[t0:t0 + 512, c0:c0 + 512].rearrange("(m p) c -> p m c", p=128), e)
        return f

    for tb in range(2):
        t0 = tb * 512
        for pi, (hsrc, wsrc, gsrc) in enumerate(((HM, w_pa, GA), (HS, w_pb, GB))):
            for cb in range(8):
                banks = bankset()
                for kq in range(4):
                    at = load_w(hsrc[kq * 512:(kq + 1) * 512, t0:t0 + 512])
                    wt = load_w(wsrc[kq * 512:(kq + 1) * 512, cb * 512:(cb + 1) * 512])
                    for m in range(4):
                        for kc in range(4):
                            kk = kq * 4 + kc
                            k.mm(banks[m][:, :], wt[:, kc, m * 128:(m + 1) * 128], at[:, kc, :], start=(kk == 0), stop=(kk == 15))
                k.dma(gtile, gsrc[cb * 512:(cb + 1) * 512, t0:t0 + 512].rearrange("(m p) t -> p m t", p=128))
                for m in range(4):
                    dsty = yT[:, cb * 4 + m, :]
                    if pi == 0:
                        k.tt(dsty, banks[m][:, :], gtile[:, m, :], ALU.mult)
                    else:
                        k.tt(gtile[:, m, :], banks[m][:, :], gtile[:, m, :], ALU.mult)
                        k.tt(dsty, dsty.bitcast(F32), gtile[:, m, :], ALU.add)
        for cb in range(8):
            proj(yT, 32, w_out, cb * 512, 512, "tm", resid_store(xs[T:S, :], H1, t0, cb * 512))
    if upto <= 4:
        return k

    k.barrier()
    memT = AR[:, 0:8192].rearrange("p (c t) -> p c t", t=256)
    for i in range(2):
        norm_tile(memx[i * 128:(i + 1) * 128, :], gmem, memT, i * 128)
    for cb in range(8):
        banks = bankset()
        for kq in range(8):
            wt = load_w(w_kv[kq * 512:(kq + 1) * 512, cb * 512:(cb + 1) * 512])
            for m in range(4):
                for kc in range(4):
                    kk = kq * 4 + kc
                    k.mm(banks[m][:, 0:256], wt[:, kc, m * 128:(m + 1) * 128], memT[:, kk, :], start=(kk == 0), stop=(kk == 31))
        e = ebuf().rearrange("p (m t) -> p m t", t=512)
        for m in range(4):
            evac(e[:, m, 0:256], banks[m][:, 0:256])
        k.dma(KX[cb * 512:(cb + 1) * 512, :].rearrange("(m p) t -> p m t", p=128), e[:, :, 0:256])
    for cb in range(8):
        banks = bankset()
        for kq in range(8):
            wt = load_w(w_kv[kq * 512:(kq + 1) * 512, D + cb * 512:D + (cb + 1) * 512])
            for m in range(2):
                for kc in range(4):
                    kk = kq * 4 + kc
                    k.mm(banks[m][:, :], memT[:, kk, m * 128:(m + 1) * 128], wt[:, kc, :], start=(kk == 0), stop=(kk == 31))
        e = ebuf().rearrange("p (m t) -> p m t", t=512)
        for m in range(2):
            evac(e[:, m, :], banks[m][:, :])
        k.dma(VX[:, cb * 512:(cb + 1) * 512].rearrange("(m p) c -> p m c", p=128), e[:, 0:2, :])

    oT = AR[:, :].rearrange("p (c t) -> p c t", t=512)
    qh = ov(0, 4096).rearrange("p (c t) -> p c t", t=512)
    khT = ov(4096, 6144).rearrange("p (c m) -> p c m", m=256)
    vh = ov(6144, 8192).rearrange("p (m d) -> p m d", d=1024)
    pT = ov(8192, 9216).rearrange("p (m t) -> p m t", t=512)
    pbuf = [ov(9216 + i * 256, 9216 + (i + 1) * 256) for i in range(2)]
    xres = ov(12288, 14336).rearrange("p (m t) -> p m t", t=512)
    for tb in range(2):
        t0 = tb * 512
        for i in range(4):
            norm_tile(H1[t0 + i * 128:t0 + (i + 1) * 128, :], gx, xnT, i * 128)
        for cb in range(8):
            proj(xnT, 32, w_q, cb * 512, 512, "fm", store_fm(QX, cb * 512, t0))
        for h in range(4):
            k.dma(qh, QX[h * 1024:(h + 1) * 1024, t0:t0 + 512].rearrange("(c p) t -> p c t", p=128))
            k.dma(khT, KX[h * 1024:(h + 1) * 1024, :].rearrange("(c p) m -> p c m", p=128))
            k.dma(vh, VX[:, h * 1024:(h + 1) * 1024].rearrange("(m p) d -> p m d", p=128))
            for i in range(4):
                psc = ps[i % 2]
                for c in range(8):
                    k.mm(psc[:, 0:256], qh[:, c, i * 128:(i + 1) * 128], khT[:, c, :], start=(c == 0), stop=(c == 7))
                mx = st[:, 32:33]; nb_ = st[:, 33:34]; sm = st[:, 34:35]; rsm = st[:, 35:36]
                k.reduce(mx, psc[:, 0:256], ALU.max)
                k.ts(nb_, mx, -1.0 / 32.0, None, op0=ALU.mult)
                pb_ = pbuf[i % 2]
                k.memset(sm, 0.0)
                k.act(pb_, psc[:, 0:256], AF.Exp, bias=nb_, scale=1.0 / 32.0, accum_out=sm)
                k.op("dve", "reciprocal", [rsm], [sm], rsm, sm)
                k.ts(pb_, pb_, rsm, None, op0=ALU.mult)
                ptp = ps[2 + i % 2]
                for m in range(2):
                    k.transpose(ptp[:, m * 128:(m + 1) * 128], pb_[:, m * 128:(m + 1) * 128], ident[:])
                k.copy(pT[:, :, i * 128:(i + 1) * 128], ptp[:, 0:256].rearrange("p (m t) -> p m t", t=128))
            for dc in range(8):
                pso = ps[4 + dc % 4]
                for m in range(2):
                    k.mm(pso[:, :], vh[:, m, dc * 128:(dc + 1) * 128], pT[:, m, :], start=(m == 0), stop=(m == 1))
                evac(oT[:, h * 8 + dc, :], pso[:, :])
        for cb in range(8):
            proj(oT, 32, w_o, cb * 512, 512, "tm", resid_store(H1, H2, t0, cb * 512))
    if upto <= 5:
        return k

    k.barrier()
    RB = 12288
    RL = ov(RB, RB + 576).rearrange("p (i c) -> p i c", c=72)
    Aasg = ov(RB + 576, RB + 1088).rearrange("p (i c) -> p i c", c=64)
    OH1 = ov(RB + 1088, RB + 1600).rearrange("p (i c) -> p i c", c=64)
    OH2 = ov(RB + 1600, RB + 2112).rearrange("p (i c) -> p i c", c=64)
    POS = ov(RB + 2112, RB + 2624).rearrange("p (i c) -> p i c", c=64)
    rw = ov(RB + 2624, RB + 3136)
    CW = ov(RB + 3136, RB + 3152).rearrange("p (i c) -> p i c", c=2)
    RI = ov(RB + 3152, RB + 3168).bitcast(I32).rearrange("p (i c) -> p i c", c=2)
    hb = ov(RB + 3168, RB + 3680)
    bR = ov(RB + 3680, RB + 3752); erow = ov(RB + 3752, RB + 3816); iota = ov(RB + 3816, RB + 3944); ltpos = ov(RB + 3944, RB + 4072)
    k.dma(bR, b_r); k.dma(erow, c_erow); k.dma(iota, c_iota); k.dma(ltpos, c_ltpos)
    wr = G[:, 0:2304].rearrange("p (kc c) -> p kc c", c=72)
    for tb in range(2):
        for i in range(4):
            r0 = tb * 512 + i * 128
            norm_tile(H2[r0:r0 + 128, :], gmoe, xnT, i * 128, xhat_dst=XH[r0:r0 + 128, :])
        k.dma(wr, w_r.rearrange("(kc p) c -> p kc c", p=128))
        for i in range(4):
            it = tb * 4 + i
            pr = ps[i % 4]
            for kk in range(32):
                k.mm(pr[:, 0:72], xnT[:, kk, i * 128:(i + 1) * 128].bitcast(F32), wr[:, kk, :], start=(kk == 0), stop=(kk == 31))
            k.tt(RL[:, it, :], pr[:, 0:72], bR, ALU.add)
    s_ = lambda a, b_: st[:, a:b_]
    for it in range(8):
        lg = RL[:, it, 0:8]; le = RL[:, it, 8:72]
        m1 = s_(40, 41); nm1 = s_(41, 42); sg = s_(42, 43); gw = s_(43, 44); m1e = s_(44, 45); m2e = s_(45, 46)
        dl = s_(46, 47); w1 = s_(47, 48); w2 = s_(48, 49)
        ohg = rw[:, 0:8]; pen = rw[:, 8:16]; egx = rw[:, 16:24]; lem = rw[:, 64:128]; lem2 = rw[:, 128:192]
        k.reduce(m1, lg, ALU.max)
        k.ts(ohg, lg, m1, None, op0=ALU.is_equal)
        k.ts(nm1, m1, -1.0, None, op0=ALU.mult)
        k.memset(sg, 0.0)
        k.act(egx, lg, AF.Exp, bias=nm1, accum_out=sg)
        k.op("dve", "reciprocal", [gw], [sg], gw, sg)
        k.ts(pen, ohg, 1.0, 1e30, op0=ALU.subtract, op1=ALU.mult)
        k.tt(lem.rearrange("p (g e) -> p g e", e=8), le.rearrange("p (g e) -> p g e", e=8),
             pen.unsqueeze(2).to_broadcast([128, 8, 8]), ALU.add)
        k.reduce(m1e, lem, ALU.max)
        k.ts(OH1[:, it, :], lem, m1e, None, op0=ALU.is_equal)
        k.stt(lem2, OH1[:, it, :], -1e30, lem, ALU.mult, ALU.add)
        k.reduce(m2e, lem2, ALU.max)
        k.ts(OH2[:, it, :], lem2, m2e, None, op0=ALU.is_equal)
        k.tt(dl, m2e, m1e, ALU.subtract)
        k.act(dl, dl, AF.Exp)
        k.ts(w1, dl, 1.0, None, op0=ALU.add)
        k.op("dve", "reciprocal", [w1], [w1], w1, w1)
        k.tt(w2, dl, w1, ALU.mult)
        k.tt(CW[:, it, 0:1], w1, gw, ALU.mult)
        k.tt(CW[:, it, 1:2], w2, gw, ALU.mult)
        k.tt(Aasg[:, it, :], OH1[:, it, :], OH2[:, it, :], ALU.add)
    for it in range(8):
        pp = ps[4 + it % 4]
        for i2 in range(it):
            k.mm(pp[:, 0:64], ones[:], Aasg[:, i2, :], start=(i2 == 0), stop=False)
        k.mm(pp[:, 0:64], ltpos, Aasg[:, it, :], start=(it == 0), stop=True)
        k.copy(POS[:, it, :], pp[:, 0:64])
        t64 = rw[:, 192:256]; t64b = rw[:, 256:320]; rf = s_(50, 51)
        k.tt(t64, POS[:, it, :], erow, ALU.add)
        for kk_, OH in ((0, OH1), (1, OH2)):
            k.tt(t64b, t64, OH[:, it, :], ALU.mult)
            k.reduce(rf, t64b, ALU.add)
            k.copy(RI[:, it, kk_:kk_ + 1], rf)

    tokid = ov(RB + 2624 + 384, RB + 2624 + 392)
    IDX = ov(RB + 2624 + 320, RB + 2624 + 384).bitcast(I32)
    k.dma(tokid, c_tokid)
    SelF = [ov(i * 1024, (i + 1) * 1024).rearrange("p (i s) -> p i s", s=128) for i in range(2)]
    pidx = ps[7]
    for e in range(NEXP):
        sf = SelF[e % 2]
        for it in range(8):
            k.ts(sf[:, it, :], iota, POS[:, it, e:e + 1], Aasg[:, it, e:e + 1], op0=ALU.is_equal, op1=ALU.mult)
        for it in range(8):
            k.mm(pidx[:, e:e + 1], sf[:, it, :], tokid[:, it:it + 1], start=(it == 0), stop=(it == 7))
    k.copy(IDX, pidx[:, 0:64])

    XeTs = [AR[:, i * 4096:(i + 1) * 4096].rearrange("p (c s) -> p c s", s=128) for i in range(2)]
    hbT = AR[:, 8192:8704].rearrange("p (f s) -> p f s", s=128)
    xes = [ov(2048, 6144), ov(6144, 10240)]
    for e in range(NEXP):
        xe = xes[e % 2]
        XeT = XeTs[e % 2]
        k.dma(xe, XH, q="pool", extra_reads=[IDX[:, e:e + 1]], _meth="indirect_dma_start", out_offset=None,
              in_offset=bass.IndirectOffsetOnAxis(ap=IDX[:, e:e + 1], axis=0))
        for c4 in range(8):
            pt = ps[4 + c4 % 3]
            for ci in range(4):
                c = c4 * 4 + ci
                k.transpose(pt[:, ci * 128:(ci + 1) * 128], xe[:, c * 128:(c + 1) * 128], ident[:])
            k.tt(XeT[:, c4 * 4:c4 * 4 + 4, :], pt[:].rearrange("p (a b) -> p a b", b=128),
                 gmoe[:, c4 * 4:c4 * 4 + 4].unsqueeze(2).to_broadcast([128, 4, 128]), ALU.mult)
        pg, pu, pt_ = ps[0], ps[1], ps[2]
        for wsrc, pacc in ((w_g, pg), (w_u, pu)):
            for kq in range(8):
                wt = load_w(wsrc[e * D + kq * 512:e * D + (kq + 1) * 512, :])
                for kc in range(4):
                    kk = kq * 4 + kc
                    k.mm(pacc[:, :], XeT[:, kk, :], wt[:, kc, :], start=(kk == 0), stop=(kk == 31))
        k.act(hb, pg[:, :], AF.Silu)
        k.tt(hb, hb, pu[:, :], ALU.mult)
        for fc in range(4):
            k.transpose(pt_[:, fc * 128:(fc + 1) * 128], hb[:, fc * 128:(fc + 1) * 128], ident[:])
        k.copy(hbT, pt_[:, :].rearrange("p (f s) -> p f s", s=128))
        for dq2 in range(4):
            ee = ebuf()
            for hf in range(2):
                dq = dq2 * 2 + hf
                yb_ = ps[3] if (dq % 2 == 0) else ps[7]
                wd = load_w(w_d[e * 512:(e + 1) * 512, dq * 512:(dq + 1) * 512])
                for fc in range(4):
                    k.mm(yb_[:, :], hbT[:, fc, :], wd[:, fc, :], start=(fc == 0), stop=(fc == 3))
                evac(ee[:, hf * 512:(hf + 1) * 512], yb_[:, :])
            k.dma(YB[e * CAP:(e + 1) * CAP, dq2 * 1024:(dq2 + 1) * 1024], ee[:, 0:1024])

    Y1 = ov(0, 4096); Y2 = ov(4096, 8192); junk = ov(8192, 12288)
    gfin = G[:, 0:4096]; h2t = G[:, 4096:8192]
    k.dma(gfin, g_fin)
    for it in range(8):
        r0 = it * 128
        k.dma(h2t, H2[r0:r0 + 128, :])
        for kk_, Y in ((0, Y1), (1, Y2)):
            k.dma(Y, YB, q="pool", extra_reads=[RI[:, it, kk_:kk_ + 1]], _meth="indirect_dma_start", out_offset=None,
                  in_offset=bass.IndirectOffsetOnAxis(ap=RI[:, it, kk_:kk_ + 1], axis=0))
            k.stt(h2t, Y, CW[:, it, kk_:kk_ + 1], h2t, ALU.mult, ALU.add)
        ss = s_(52, 53); rs = s_(53, 54); tmp = s_(54, 55)
        k.memset(ss, 0.0)
        k.act(junk, h2t, AF.Square, accum_out=ss)
        rstd_from_ss(ss, rs, 1.0 / D, tmp)
        k.ts(h2t, h2t, rs, None, op0=ALU.mult)
        k.tt(h2t, h2t, gfin, ALU.mult)
        k.dma(out[r0:r0 + 128, :], h2t)
    return k


def _consts():
    f = np.float32
    i = np.arange(128)
    c = {}
    c["c_ident"] = np.eye(128, dtype=f)
    c["c_ones"] = np.ones((128, 128), f)
    c["c_ut"] = (i[:, None] > i[None, :]).astype(f)
    c["c_lt"] = (i[:, None] <= i[None, :]).astype(f)
    u = np.arange(896)
    c["c_mm"] = ((u[None, :] - 384) >= i[:, None]).astype(f)
    c["c_ms"] = ((u[None, :] - 384) > i[:, None]).astype(f)
    c["c_ltpos"] = (i[:, None] < i[None, :]).astype(f)
    c["c_iota"] = np.broadcast_to(np.arange(128, dtype=f)[None, :], (128, 128)).copy()
    c["c_tokid"] = (np.arange(8, dtype=f)[None, :] * 128.0 + np.arange(128, dtype=f)[:, None]).copy()
    c["c_erow"] = np.broadcast_to((np.arange(64, dtype=f) * 128.0)[None, :], (128, 64)).copy()
    return c


def _gl(g, n):
    return np.ascontiguousarray(np.asarray(g, np.float32).reshape(n, 128).T)


def make_in_maps(inp, cores=range(8), moe=True):
    f = np.float32
    x = np.asarray(inp["x"], f); mem = np.asarray(inp["mem"], f)
    sh = _consts()
    sh["w_in"] = np.asarray(inp["w_in"], f)[0]
    sh["w_proj_a"] = np.asarray(inp["w_proj_a"], f)[0]
    sh["w_proj_b"] = np.asarray(inp["w_proj_b"], f)[0]
    sh["w_out"] = np.asarray(inp["w_out"], f)[0]
    sh["w_q_mem"] = np.asarray(inp["w_q_mem"], f)[0]
    sh["w_kv_mem"] = np.asarray(inp["w_kv_mem"], f)[0]
    sh["w_o_mem"] = np.asarray(inp["w_o_mem"], f)[0]
    if moe:
        sh["w_gate"] = np.asarray(inp["w_gate"], f)[0].reshape(NEXP * D, 512)
        sh["w_up"] = np.asarray(inp["w_up"], f)[0].reshape(NEXP * D, 512)
        sh["w_down"] = np.asarray(inp["w_down"], f)[0].reshape(NEXP * 512, D)
    sh["w_r"] = np.ascontiguousarray(np.concatenate([np.asarray(inp["w_router_group"], f)[0], np.asarray(inp["w_router_expert"], f)[0]], axis=1))
    br = np.concatenate([np.asarray(inp["b_router_group"], f)[0], np.asarray(inp["b_router_expert"], f)[0]])
    sh["b_r"] = np.broadcast_to(br[None, :], (128, 72)).copy()
    sh["g_mix"] = _gl(inp["norm_mix"][0], 32); sh["g_x"] = _gl(inp["norm_xattn"][0], 32)
    sh["g_mem"] = _gl(inp["norm_mem"][0], 32); sh["g_moe"] = _gl(inp["norm_moe"][0], 32)
    sh["g_fin"] = np.broadcast_to(np.asarray(inp["norm_final"], f)[None, :], (128, D)).copy()
    sh["g_ml"] = _gl(inp["g_mlstm"][0], 16)
    sh["convT"] = np.ascontiguousarray(np.asarray(inp["conv_qk"], f)[0].T)
    bg = np.asarray(inp["b_gates"], f)[0]
    sh["b_i"] = bg[:8].reshape(8, 1).copy(); sh["b_f"] = bg[8:].reshape(8, 1).copy()
    maps = []
    for c in cores:
        b, half = c // 2, c % 2
        xs_ = np.zeros((S, D), f)
        if half == 1:
            xs_[:] = x[b]
        else:
            xs_[T:] = x[b, :T]
        m = dict(sh)
        m["xs"] = xs_
        m["mem"] = np.ascontiguousarray(mem[b])
        maps.append(m)
    return maps


def kernel(**inputs):
    kb = build()
    nc = kb.finish()
    maps = make_in_maps(inputs)
    res = run_bass_kernel_spmd(nc, maps, core_ids=list(range(8)))
    outp = np.zeros((4, S, D), np.float32)
    for c in range(8):
        b, half = c // 2, c % 2
        outp[b, half * T:(half + 1) * T] = res.results[c]["out"]
    return outp
```

```python
import numpy as np
from contextlib import ExitStack
import concourse.bass as bass
import concourse.mybir as mybir
from concourse.bass_utils import run_bass_kernel_spmd

F32 = mybir.dt.float32
F32R = mybir.dt.float32r
I32 = mybir.dt.int32
ALU = mybir.AluOpType
AF = mybir.ActivationFunctionType
AX = mybir.AxisListType

ENGS = ("pe", "act", "dve", "pool", "sp")
NDMASEM = {"sp": 24, "pool": 8, "act": 8}


def _region(ap):
    t = ap.tensor
    shape = list(t.shape)
    rowsize = 1
    for s in shape[1:]:
        rowsize *= int(s)
    off = int(ap.offset)
    r_lo, c_lo = off // rowsize, off % rowsize
    r_ext, c_ext = 0, 0
    for step, cnt in ap.ap:
        step, cnt = int(step), int(cnt)
        if cnt <= 1 or step == 0:
            continue
        if step % rowsize == 0:
            r_ext += (cnt - 1) * (step // rowsize)
        else:
            c_ext += (cnt - 1) * abs(step)
    return (t.name, r_lo, r_lo + r_ext + 1, c_lo, c_lo + c_ext + 1)


def _overlap(a, b):
    return a[1] < b[2] and b[1] < a[2] and a[3] < b[4] and b[3] < a[4]


def _covers(a, b):
    return a[1] <= b[1] and a[2] >= b[2] and a[3] <= b[3] and a[4] >= b[4]


class K:
    def __init__(self):
        self.nc = bass.Bass("TRN2", target_bir_lowering=False)
        self.es = ExitStack()
        self.streams = {e: [] for e in ENGS}
        self.cnt = {e: 0 for e in ENGS}
        self.esem = {}
        for e in ("pe", "act", "dve", "pool"):
            self.esem[e] = self.es.enter_context(self.nc.semaphore("sem_" + e))
        self.dsem = {}
        self.dcnt = {}
        self.dlast = {}
        for q, n in NDMASEM.items():
            self.dsem[q] = [self.es.enter_context(self.nc.semaphore("dsem_%s_%d" % (q, i))) for i in range(n)]
            self.dcnt[q] = 0
        self.track = {}
        self.waited = {e: {} for e in ENGS}
        self.n_wait = 0
        self.out_tokens = []

    def sbuf(self, name, shape, dt=F32):
        return self.es.enter_context(self.nc.sbuf_tensor(name, list(shape), dt))

    def psum(self, name, shape, dt=F32):
        return self.es.enter_context(self.nc.psum_tensor(name, list(shape), dt))

    def dram(self, name, shape, dt=F32, kind="Internal"):
        return self.nc.dram_tensor(name, list(shape), dt, kind=kind).ap()

    def _semkey(self, tok):
        if tok[0] == "c":
            return ("c", tok[1]), tok[2]
        return ("d", tok[1], tok[2]), tok[3]

    def _need_wait(self, eng, tok, kind_raw, is_dma=False):
        if tok[0] == "c" and tok[1] == eng and not is_dma:
            if eng == "pe":
                return False
            if not kind_raw:
                return False
        key, val = self._semkey(tok)
        return self.waited[eng].get(key, 0) < val

    def _add_wait(self, eng, tok, waits):
        key, val = self._semkey(tok)
        if self.waited[eng].get(key, 0) >= val:
            return
        self.waited[eng][key] = val
        if tok[0] == "c":
            sem = self.esem[tok[1]]
        else:
            sem = self.dsem[tok[1]][tok[2]]
        waits.append((sem, val))

    def _deps(self, eng, reads, writes, is_dma=False):
        waits = []
        for r in reads:
            for ent in self.track.get(r[0], ()):
                if ent[1] == "w" and _overlap(ent[0], r):
                    if self._need_wait(eng, ent[2], True, is_dma):
                        self._add_wait(eng, ent[2], waits)
        for w in writes:
            for ent in self.track.get(w[0], ()):
                if _overlap(ent[0], w):
                    if self._need_wait(eng, ent[2], False, is_dma):
                        self._add_wait(eng, ent[2], waits)
        return waits

    def _record(self, reads, writes, tok):
        for w in writes:
            lst = self.track.setdefault(w[0], [])
            lst[:] = [e for e in lst if not _covers(w, e[0])]
            lst.append([w, "w", tok])
        for r in reads:
            lst = self.track.setdefault(r[0], [])
            if tok[0] == "c":
                lst[:] = [e for e in lst if not (e[1] == "r" and e[2][0] == "c" and e[2][1] == tok[1] and _covers(r, e[0]))]
            lst.append([r, "r", tok])

    def op(self, eng, meth, writes, reads, *args, **kw):
        wr = [_region(a) for a in writes]
        rr = [_region(a) for a in reads]
        waits = self._deps(eng, rr, wr)
        self.cnt[eng] += 1
        tok = ("c", eng, self.cnt[eng])
        self._record(rr, wr, tok)
        self.streams[eng].append((waits, meth, args, kw, (self.esem[eng], 1)))
        self.n_wait += len(waits)
        return tok

    def dma(self, out, in_, q="sp", extra_reads=(), **kw):
        wr = [_region(out)]
        rr = [_region(in_)] + [_region(a) for a in extra_reads]
        waits = self._deps(q, rr, wr, True)
        i = self.dcnt[q]
        n = len(self.dsem[q])
        slot, gen = i % n, i // n
        if gen > 0:
            self._add_wait(q, ("d", q, slot, 16 * gen), waits)
        self.dcnt[q] += 1
        tok = ("d", q, slot, 16 * (gen + 1))
        self._record(rr, wr, tok)
        meth = kw.pop("_meth", "dma_start")
        self.streams[q].append((waits, meth, (), dict(out=out, in_=in_, **kw), (self.dsem[q][slot], 16)))
        return tok

    def wait_tok(self, eng, tok):
        waits = []
        self._add_wait(eng, tok, waits)
        if waits:
            self.streams[eng].append((waits, None, (), {}, None))

    def barrier(self):
        toks = []
        for e in ("pe", "act", "dve", "pool"):
            if self.cnt[e] > 0:
                toks.append(("c", e, self.cnt[e]))
        for q in self.dsem:
            n = len(self.dsem[q])
            for i in range(max(0, self.dcnt[q] - n), self.dcnt[q]):
                toks.append(("d", q, i % n, 16 * (i // n + 1)))
        for e in ENGS:
            for t in toks:
                if t[0] == "c" and t[1] == e:
                    continue
                self.wait_tok(e, t)
        self.track = {}

    def mm(self, out, lhsT, rhs, start=True, stop=True, **kw):
        return self.op("pe", "matmul", [out], [lhsT, rhs], out, lhsT, rhs, start=start, stop=stop, **kw)

    def transpose(self, out, in_, ident):
        return self.op("pe", "transpose", [out], [in_, ident], out, in_, ident)

    def act(self, out, in_, func, bias=None, scale=None, accum_out=None, eng="act"):
        reads = [in_]
        kw = {}
        if bias is not None:
            kw["bias"] = bias
            if not isinstance(bias, (int, float)):
                reads.append(bias)
        if scale is not None:
            kw["scale"] = scale
            if not isinstance(scale, (int, float)):
                reads.append(scale)
        writes = [out]
        if accum_out is not None:
            kw["accum_out"] = accum_out
            writes.append(accum_out)
        return self.op(eng, "activation", writes, reads, out, in_, func, **kw)

    def tt(self, out, in0, in1, op, eng="dve"):
        return self.op(eng, "tensor_tensor", [out], [in0, in1], out, in0, in1, op)

    def ts(self, out, in0, s1, s2=None, op0=ALU.mult, op1=None, eng="dve", accum_out=None):
        reads = [in0]
        if not isinstance(s1, (int, float)):
            reads.append(s1)
        if s2 is not None and not isinstance(s2, (int, float)):
            reads.append(s2)
        kw = {}
        if op1 is not None:
            kw["op1"] = op1
        writes = [out]
        if accum_out is not None:
            kw["accum_out"] = accum_out
            writes.append(accum_out)
        return self.op(eng, "tensor_scalar", writes, reads, out, in0, s1, s2, op0, **kw)

    def stt(self, out, in0, scalar, in1, op0, op1, eng="dve"):
        reads = [in0, in1]
        if not isinstance(scalar, (int, float)):
            reads.append(scalar)
        return self.op(eng, "scalar_tensor_tensor", [out], reads, out, in0, scalar, in1, op0, op1)

    def copy(self, out, in_, eng="dve"):
        return self.op(eng, "tensor_copy", [out], [in_], out, in_)

    def memset(self, out, val, eng="dve"):
        return self.op(eng, "memset", [out], [], out, val)

    def reduce(self, out, in_, op, axis=AX.X, eng="dve"):
        return self.op(eng, "tensor_reduce", [out], [in_], out, in_, axis, op)

    def finish(self):
        nc = self.nc
        eng_obj = {"pe": "tensor", "act": "scalar", "dve": "vector", "pool": "gpsimd", "sp": "sync"}
        self.barrier()
        with nc.Block() as block:
            for e in ENGS:
                items = self.streams[e]

                def body(eo, items=items):
                    for waits, meth, args, kw, inc in items:
                        for sem, val in waits:
                            eo.wait_ge(sem, val)
                        if meth is None:
                            continue
                        ins = getattr(eo, meth)(*args, **kw)
                        if inc is not None:
                            ins.then_inc(inc[0], inc[1])
                getattr(block, eng_obj[e])(body)
        return nc

    def stats(self):
        return {e: len(self.streams[e]) for e in ENGS}, self.n_wait

import math
D = 4096
T = 1024
S = 2048
EPS = 1e-6
C_QM, C_KM, C_VM, C_OM, C_I, C_F, C_QS, C_KS, C_VS, C_GA, C_GB = 0, 1024, 2048, 4096, 6144, 6152, 6160, 8208, 10256, 12304, 16400
SB_SCALE = 1.0 / math.sqrt(128.0)
LN_SQRT_DK = 0.5 * math.log(128.0)
NEXP = 64
CAP = 128


def build(upto=99, dbg=()):
    k = K()

    def ext(n, shp, dt=F32):
        return k.dram(n, shp, dt, kind="ExternalInput")

    def scr(n, shp, dt=F32):
        return k.dram(n, shp, dt, kind=("ExternalOutput" if n in dbg else "Internal"))

    xs = ext("xs", [S, D]); memx = ext("mem", [256, D])
    w_in = ext("w_in", [D, 20496]); w_pa = ext("w_proj_a", [2048, D]); w_pb = ext("w_proj_b", [2048, D])
    w_out = ext("w_out", [D, D]); w_q = ext("w_q_mem", [D, D]); w_kv = ext("w_kv_mem", [D, 2 * D]); w_o = ext("w_o_mem", [D, D])
    if upto >= 6:
        w_g = ext("w_gate", [NEXP * D, 512]); w_u = ext("w_up", [NEXP * D, 512]); w_d = ext("w_down", [NEXP * 512, D])
    w_r = ext("w_r", [D, 72]); b_r = ext("b_r", [128, 72])
    g_mix = ext("g_mix", [128, 32]); g_x = ext("g_x", [128, 32]); g_mem = ext("g_mem", [128, 32]); g_moe = ext("g_moe", [128, 32])
    g_fin = ext("g_fin", [128, D]); g_ml = ext("g_ml", [128, 16])
    convT = ext("convT", [2048, 4]); b_i = ext("b_i", [8, 1]); b_f = ext("b_f", [8, 1])
    c_ident = ext("c_ident", [128, 128]); c_ones = ext("c_ones", [128, 128]); c_ut = ext("c_ut", [128, 128]); c_lt = ext("c_lt", [128, 128])
    c_mm = ext("c_mm", [128, 896]); c_ms = ext("c_ms", [128, 896])
    c_ltpos = ext("c_ltpos", [128, 128]); c_iota = ext("c_iota", [128, 128]); c_erow = ext("c_erow", [128, 64]); c_tokid = ext("c_tokid", [128, 8])
    out = k.dram("out", [T, D], F32, kind="ExternalOutput")

    QKm = scr("QKm", [2048, S]); Vm = scr("Vm", [S, 2048]); Om = scr("Om", [2048, T])
    I8d = scr("I8d", [8, S]); F8d = scr("F8d", [8, S])
    Qs = scr("Qs", [2048, T]); Ks = scr("Ks", [2048, S]); Vs = scr("Vs", [S, 2048])
    GA = scr("GA", [D, T]); GB = scr("GB", [D, T])
    HM = scr("HM", [2048, T]); HS = scr("HS", [2048, T])
    H1 = scr("H1", [T, D]); H2 = scr("H2", [T, D])
    KX = scr("KX", [D, 256]); VX = scr("VX", [256, D]); QX = scr("QX", [D, T])
    XH = scr("XH", [T, D]); YB = scr("YB", [NEXP * CAP, D])

    AR = k.sbuf("AR", [128, 16384], F32R); WR = k.sbuf("WR", [128, 6144], F32R); G = k.sbuf("G", [128, 26624])
    WS = [G[:, i * 2048:(i + 1) * 2048] for i in range(3)]
    WRs = [WR[:, i * 2048:(i + 1) * 2048] for i in range(3)]
    Eb = [G[:, 6144 + i * 2048:6144 + (i + 1) * 2048] for i in range(2)]
    OV0 = 10240

    def ov(a, b_):
        return G[:, OV0 + a:OV0 + b_]

    def ovr(r0, r1, a, b_):
        return G[r0:r1, OV0 + a:OV0 + b_]
    Xb = [ov(0, 4096), ov(4096, 8192)]
    ws_list = list(WS)

    def set_ws(extra):
        ws_list[:] = list(WS) + list(extra)
    sqjunk = ov(8192, 12288)
    ident = k.sbuf("ident", [128, 128]); ones = k.sbuf("ones", [128, 128]); ut = k.sbuf("ut", [128, 128]); lt = k.sbuf("lt", [128, 128])
    mmk = k.sbuf("mmk", [128, 896]); msk = k.sbuf("msk", [128, 896])
    gmix = k.sbuf("gmix", [128, 32]); gx = k.sbuf("gx", [128, 32]); gmem = k.sbuf("gmem", [128, 32]); gmoe = k.sbuf("gmoe", [128, 32]); gml = k.sbuf("gml", [128, 16])
    st = k.sbuf("st", [128, 64])
    wif = k.sbuf("wif", [128, 32, 16])
    biasT = k.sbuf("biasT", [128, 128])
    ps = [k.psum("ps%d" % i, [128, 512]) for i in range(8)]

    for dst, src in ((ident, c_ident), (ones, c_ones), (ut, c_ut), (lt, c_lt), (mmk, c_mm), (msk, c_ms),
                     (gmix, g_mix), (gx, g_x), (gmem, g_mem), (gmoe, g_moe), (gml, g_ml)):
        k.dma(dst[:], src)
    k.dma(wif[:], w_in[:, C_I:C_I + 16].rearrange("(kc p) c -> p kc c", p=128))

    cnt = {"w": 0, "e": 0, "bank": 0, "x": 0, "t": 0, "alt": 0}

    def load_w(w2d):
        nk = w2d.shape[0] // 128
        nc_ = w2d.shape[1]
        i = cnt["w"] % 3
        si = cnt["w"] % len(ws_list)
        cnt["w"] += 1
        sv = ws_list[si][:, 0:nk * nc_].rearrange("p (kc c) -> p kc c", c=nc_)
        rv = WRs[i][:, 0:nk * nc_].rearrange("p (kc c) -> p kc c", c=nc_)
        k.dma(sv, w2d.rearrange("(kc p) c -> p kc c", p=128))
        h = max(1, nk // 2)
        k.copy(rv[:, 0:h, :], sv[:, 0:h, :])
        if h < nk:
            k.act(rv[:, h:nk, :], sv[:, h:nk, :], AF.Copy)
        return rv

    def bankset():
        b = cnt["bank"] % 2
        cnt["bank"] += 1
        return [ps[b * 4 + i] for i in range(4)]

    def ebuf():
        e = Eb[cnt["e"] % 2]
        cnt["e"] += 1
        return e

    def evac(out_ap, in_ap, func=None):
        if func is not None:
            k.act(out_ap, in_ap, func)
            return
        cnt["alt"] += 1
        if cnt["alt"] % 2:
            k.act(out_ap, in_ap, AF.Copy)
        else:
            k.copy(out_ap, in_ap)

    def rstd_from_ss(ss, rs, scale, tmp):
        k.ts(tmp, ss, scale, EPS, op0=ALU.mult, op1=ALU.add)
        k.act(tmp, tmp, AF.Sqrt)
        k.op("dve", "reciprocal", [rs], [tmp], rs, tmp)

    def norm_tile(src, gT, dstT, col0, xhat_dst=None):
        xb = Xb[cnt["x"] % 2]
        cnt["x"] += 1
        k.dma(xb, src)
        ss = st[:, 0:1]; rs = st[:, 1:2]; tmp = st[:, 2:3]
        k.memset(ss, 0.0)
        k.act(sqjunk, xb, AF.Square, accum_out=ss)
        rstd_from_ss(ss, rs, 1.0 / D, tmp)
        k.ts(xb, xb, rs, None, op0=ALU.mult)
        if xhat_dst is not None:
            k.dma(xhat_dst, xb)
        for c4 in range(8):
            pt = ps[4 + cnt["t"] % 4]
            cnt["t"] += 1
            for ci in range(4):
                c = c4 * 4 + ci
                k.transpose(pt[:, ci * 128:(ci + 1) * 128], xb[:, c * 128:(c + 1) * 128], ident[:])
            k.tt(dstT[:, c4 * 4:c4 * 4 + 4, col0:col0 + 128], pt[:].rearrange("p (a b) -> p a b", b=128),
                 gT[:, c4 * 4:c4 * 4 + 4].unsqueeze(2).to_broadcast([128, 4, 128]), ALU.mult)

    def proj(actT, nK, w2d, c0, ncols, mode, evac_fn):
        banks = bankset()
        nb = ncols // 128 if mode == "fm" else 4
        kstep = 4
        for kq in range(nK // kstep):
            wt = load_w(w2d[kq * kstep * 128:(kq + 1) * kstep * 128, c0:c0 + ncols])
            for m in range(nb):
                for kc in range(kstep):
                    kk = kq * kstep + kc
                    if mode == "fm":
                        k.mm(banks[m][:, :], wt[:, kc, m * 128:(m + 1) * 128], actT[:, kk, :], start=(kk == 0), stop=(kk == nK - 1))
                    else:
                        k.mm(banks[m][:, 0:ncols], actT[:, kk, m * 128:(m + 1) * 128], wt[:, kc, :], start=(kk == 0), stop=(kk == nK - 1))
        evac_fn(banks)

    def store_fm(dst, r0, t0, func=None):
        def f(banks):
            e = ebuf().rearrange("p (m t) -> p m t", t=512)
            for m in range(4):
                evac(e[:, m, :], banks[m][:, :], func)
            k.dma(dst[r0:r0 + 512, t0:t0 + 512].rearrange("(m p) t -> p m t", p=128), e)
        return f

    def store_tm(dst, t0, c0):
        def f(banks):
            e = ebuf().rearrange("p (m t) -> p m t", t=512)
            for m in range(4):
                evac(e[:, m, :], banks[m][:, :])
            k.dma(dst[t0:t0 + 512, c0:c0 + 512].rearrange("(m p) c -> p m c", p=128), e)
        return f

    xnT = AR[:, :].rearrange("p (c t) -> p c t", t=512)

    set_ws([ov(12288, 14336), ov(14336, 16384)])
    for tb in range(4):
        own = tb >= 2
        for i in range(4):
            norm_tile(xs[tb * 512 + i * 128: tb * 512 + (i + 1) * 128, :], gmix, xnT, i * 128)
        t0 = tb * 512
        to = t0 - T
        for cb in range(4):
            proj(xnT, 32, w_in, C_QM + cb * 512, 512, "fm", store_fm(QKm, cb * 512, t0))
        for cb in range(4):
            proj(xnT, 32, w_in, C_VM + cb * 512, 512, "tm", store_tm(Vm, t0, cb * 512))
        for cb in range(4):
            proj(xnT, 32, w_in, C_KS + cb * 512, 512, "fm", store_fm(Ks, cb * 512, t0))
        for cb in range(4):
            proj(xnT, 32, w_in, C_VS + cb * 512, 512, "tm", store_tm(Vs, t0, cb * 512))
        for gi, dstd in ((0, I8d), (1, F8d)):
            pb = ps[cnt["bank"] % 8]
            for kk in range(32):
                k.mm(pb[0:8, :], wif[:, kk, gi * 8:(gi + 1) * 8], xnT[:, kk, :].bitcast(F32), start=(kk == 0), stop=(kk == 31))
            e = ebuf()
            evac(e[0:8, 0:512], pb[0:8, :])
            k.dma(dstd[:, t0:t0 + 512], e[0:8, 0:512])
        if own:
            for cb in range(4):
                proj(xnT, 32, w_in, C_OM + cb * 512, 512, "fm", store_fm(Om, cb * 512, to, AF.Sigmoid))
            for cb in range(4):
                proj(xnT, 32, w_in, C_QS + cb * 512, 512, "fm", store_fm(Qs, cb * 512, to))
            for cb in range(8):
                proj(xnT, 32, w_in, C_GA + cb * 512, 512, "fm", store_fm(GA, cb * 512, to, AF.Sigmoid))
            for cb in range(8):
                proj(xnT, 32, w_in, C_GB + cb * 512, 512, "fm", store_fm(GB, cb * 512, to, AF.Sigmoid))
    if upto <= 1:
        return k

    k.barrier()
    set_ws([])
    I8 = ovr(0, 8, 13000, 15048); Fa = G[0:8, 8192:10240]; Fb_ = G[0:8, 4096:6144]; G8 = G[0:8, 6144:8192]
    bi = st[0:8, 8:9]; bf = st[0:8, 9:10]; nbf = st[0:8, 10:11]
    k.dma(bi, b_i); k.dma(bf, b_f)
    k.dma(I8, I8d); k.dma(Fa, F8d)
    k.ts(nbf, bf, -1.0, None, op0=ALU.mult)
    k.ts(I8, I8, bi, None, op0=ALU.add)
    k.act(Fa, Fa, AF.Exp, bias=nbf, scale=-1.0)
    k.act(Fa, Fa, AF.Ln, bias=1.0)
    k.ts(Fa, Fa, -1.0, None, op0=ALU.mult)
    src, dstb = Fa, Fb_
    dd = 1
    while dd < S:
        k.tt(dstb[:, dd:S], src[:, dd:S], src[:, 0:S - dd], ALU.add)
        k.copy(dstb[:, 0:dd], src[:, 0:dd])
        src, dstb = dstb, src
        dd *= 2
    Fc = src
    k.tt(G8, I8, Fc, ALU.subtract)
    k.ts(G8, G8, -LN_SQRT_DK, None, op0=ALU.add)
    pbt = ps[0]
    for j in range(16):
        k.transpose(pbt[:, j * 8:(j + 1) * 8], G8[:, j * 128:(j + 1) * 128], ident[0:8, 0:8])
    k.copy(biasT[:], pbt[:, 0:128])

    Qc = AR[:, 0:1024]; Kc = AR[:, 1024:3072]
    Pt = [[AR[:, 3072 + (c * 2 + i) * 512:3072 + (c * 2 + i + 1) * 512] for i in range(2)] for c in range(2)]
    Vh = AR[:, 5120:9216].rearrange("p (j c) -> p j c", c=256)
    onesR = AR[:, 9216:9344]
    k.copy(onesR, ones[:])
    ctmp = ov(0, 2048)
    FbS = [ov(2048 + c * 512, 2048 + (c + 1) * 512) for c in range(2)]
    Dt = [[ov(3072 + (c * 2 + i) * 512, 3072 + (c * 2 + i + 1) * 512) for i in range(2)] for c in range(2)]
    misc = [[ov(5120 + (c * 7 + i) * 512, 5120 + (c * 7 + i + 1) * 512) for i in range(7)] for c in range(2)]
    qpre = ov(12288, 13315); kpre = ov(13315, 15366)
    tmp8 = [G[0:8, 6144 + c * 512:6144 + (c + 1) * 512] for c in range(2)]
    Vst = G[:, 0:4096].rearrange("p (j c) -> p j c", c=256)
    cw = st[:, 16:24]
    for h in range(8):
        k.dma(cw[:, 0:4], convT[h * 128:(h + 1) * 128, :])
        k.dma(cw[:, 4:8], convT[1024 + h * 128:1024 + (h + 1) * 128, :])
        k.dma(qpre, QKm[h * 128:(h + 1) * 128, T - 3:S])
        k.memset(kpre[:, 0:3], 0.0)
        k.dma(kpre[:, 3:2051], QKm[1024 + h * 128:1024 + (h + 1) * 128, :])
        k.dma(Vst, Vm[:, h * 256:(h + 1) * 256].rearrange("(j p) c -> p j c", p=128))
        k.copy(Vh[:, 0:8, :], Vst[:, 0:8, :])
        k.act(Vh[:, 8:16, :], Vst[:, 8:16, :], AF.Copy)
        for (dst_, pre, n, wo) in ((Qc, qpre, T, 0), (Kc, kpre, S, 4)):
            ct = ctmp[:, 0:n]
            k.ts(ct, pre[:, 0:n], cw[:, wo:wo + 1], None, op0=ALU.mult)
            for kk in range(1, 4):
                k.stt(ct, pre[:, kk:kk + n], cw[:, wo + kk:wo + kk + 1], ct, ALU.mult, ALU.add)
            k.act(dst_, ct, AF.Silu)
        nfull = [8, 12]; njs = [12, 16]
        banks2 = [(ps[0], ps[2], ps[3], ps[4]), (ps[1], ps[5], ps[6], ps[7])]
        for tb in range(2):
            pF = banks2[tb][0]
            k.ts(tmp8[tb], Fc[:, T + tb * 512:T + (tb + 1) * 512], ident[0:8, h:h + 1], None, op0=ALU.mult)
            k.mm(pF[:, :], ones[0:8, :], tmp8[tb])
            k.copy(FbS[tb], pF[:, :])
        for j in range(16):
            for tb in range(2):
                nj = njs[tb]
                if j >= nj:
                    continue
                pS, num0, num1, den = banks2[tb]
                k.mm(pS[:, :], Kc[:, j * 128:(j + 1) * 128], Qc[:, tb * 512:(tb + 1) * 512])
                d_ = Dt[tb][j % 2]; p_ = Pt[tb][j % 2]
                k.act(d_, FbS[tb], AF.Exp, bias=biasT[:, j * 8 + h:j * 8 + h + 1])
                if j >= nfull[tb]:
                    jj = j - nfull[tb]
                    k.tt(d_, d_, mmk[:, 384 - 128 * jj:384 - 128 * jj + 512], ALU.mult, eng="pool")
                k.tt(p_, pS[:, :], d_, ALU.mult)
            for tb in range(2):
                nj = njs[tb]
                if j >= nj:
                    continue
                pS, num0, num1, den = banks2[tb]
                p_ = Pt[tb][j % 2]
                k.mm(num0[:, :], Vh[:, j, 0:128], p_, start=(j == 0), stop=(j == nj - 1))
                k.mm(num1[:, :], Vh[:, j, 128:256], p_, start=(j == 0), stop=(j == nj - 1))
                k.mm(den[:, :], onesR, p_, start=(j == 0), stop=(j == nj - 1))
        for tb in range(2):
            pS, num0, num1, den = banks2[tb]
            rec, h0, h1, sq, rs_, og, sq1 = misc[tb]
            k.act(rec, den[:, :], AF.Abs)
            k.ts(rec, rec, 1.0, None, op0=ALU.max)
            k.op("dve", "reciprocal", [rec], [rec], rec, rec)
            k.tt(h0, num0[:, :], rec, ALU.mult)
            k.tt(h1, num1[:, :], rec, ALU.mult)
            pq = pS
            k.tt(sq, h0, h0, ALU.mult)
            k.mm(pq[:, :], ones[:], sq, start=True, stop=False)
            k.tt(sq1, h1, h1, ALU.mult)
            k.mm(pq[:, :], ones[:], sq1, start=False, stop=True)
            k.ts(rs_, pq[:, :], 1.0 / 256.0, EPS, op0=ALU.mult, op1=ALU.add)
            k.act(rs_, rs_, AF.Sqrt)
            k.op("dve", "reciprocal", [rs_], [rs_], rs_, rs_)
            e = ebuf().rearrange("p (m t) -> p m t", t=512)
            for c, hc in ((0, h0), (1, h1)):
                k.dma(og, Om[h * 256 + c * 128:h * 256 + (c + 1) * 128, tb * 512:(tb + 1) * 512])
                k.stt(hc, hc, gml[:, h * 2 + c:h * 2 + c + 1], rs_, ALU.mult, ALU.mult)
                k.tt(e[:, c, :], hc, og, ALU.mult)
            k.dma(HM[h * 256:(h + 1) * 256, tb * 512:(tb + 1) * 512].rearrange("(m p) t -> p m t", p=128), e[:, 0:2, :])
    if upto <= 2:
        return k

    k.barrier()
    Qh = AR[:, 0:1024]; Kh = AR[:, 1024:3072]
    Vsh = AR[:, 3072:5120].rearrange("p (j c) -> p j c", c=128)

    def four(base, arena_fn):
        return [[arena_fn(base + (c * 2 + i) * 512, base + (c * 2 + i + 1) * 512) for i in range(2)] for c in range(2)]
    arv = lambda a, b_: AR[:, a:b_]
    SPt = four(5120, arv); SMt = four(7168, arv); At = four(9216, arv)
    utR = AR[:, 11264:11392]; ltR = AR[:, 11392:11520]
    k.copy(utR, ut[:]); k.copy(ltR, lt[:])
    qst = ov(0, 1024); kst = ov(1024, 3072); vst = ov(3072, 5120).rearrange("p (j c) -> p j c", c=128)
    Et = four(5120, ov); ARt = four(7168, ov)
    negm = ov(9216, 10112)
    k.ts(negm, msk[:], 1.0, 1.0e4, op0=ALU.subtract, op1=ALU.mult)
    for h in range(16):
        k.dma(qst, Qs[h * 128:(h + 1) * 128, :])
        k.dma(kst, Ks[h * 128:(h + 1) * 128, :])
        k.dma(vst, Vs[:, h * 128:(h + 1) * 128].rearrange("(j p) c -> p j c", p=128))
        k.copy(Qh, qst)
        k.act(Kh, kst, AF.Copy)
        k.copy(Vsh, vst)
        nfull = [8, 12]; njs = [12, 16]

        def bufs(tb, idx_):
            i2 = idx_ % 2
            return Et[tb][i2], SPt[tb][i2], ARt[tb][i2], At[tb][i2], SMt[tb][i2]

        def tile_of(tb, idx_):
            j = njs[tb] - 1 - idx_
            diag = j >= nfull[tb]
            sl = None
            if diag:
                jj = j - nfull[tb]
                sl = slice(384 - 128 * jj, 384 - 128 * jj + 512)
            return j, diag, sl

        def S1(tb, idx_):
            if idx_ >= njs[tb]:
                return
            j, diag, sl = tile_of(tb, idx_)
            e_, sp_, ar_, a_, sm_ = bufs(tb, idx_)
            pz = ps[2 * tb + idx_ % 2]
            k.mm(pz[:, :], Kh[:, j * 128:(j + 1) * 128], Qh[:, tb * 512:(tb + 1) * 512])
            k.act(e_, pz[:, :], AF.Exp, scale=SB_SCALE)
            k.act(sp_, e_, AF.Ln, bias=1.0)
            k.stt(ar_, pz[:, :], SB_SCALE, sp_.bitcast(F32), ALU.mult, ALU.subtract)
            if diag:
                k.tt(sm_, sp_.bitcast(F32), msk[:, sl], ALU.mult)

        def S2a(tb, idx_):
            if idx_ >= njs[tb]:
                return
            j, diag, sl = tile_of(tb, idx_)
            e_, sp_, ar_, a_, sm_ = bufs(tb, idx_)
            spm = sm_ if diag else sp_
            acc = ps[4 + tb]
            k.mm(acc[:, :], utR, spm, start=(idx_ == 0), stop=False, skip_group_check=True)
            k.tt(ar_, ar_, acc[:, :], ALU.subtract)
            if diag:
                k.tt(ar_, ar_, negm[:, sl], ALU.add, eng="pool")
            k.act(a_, ar_, AF.Exp)

        def S2b(tb, idx_):
            if idx_ >= njs[tb]:
                return
            j, diag, sl = tile_of(tb, idx_)
            e_, sp_, ar_, a_, sm_ = bufs(tb, idx_)
            spm = sm_ if diag else sp_
            acc = ps[4 + tb]; po = ps[6 + tb]
            nj = njs[tb]
            k.mm(acc[:, :], ltR, spm, start=False, stop=(idx_ == nj - 1), skip_group_check=True)
            k.mm(po[:, :], Vsh[:, j, :], a_, start=(idx_ == 0), stop=(idx_ == nj - 1))

        S1(0, 0); S1(1, 0)
        for idx_ in range(16):
            S1(0, idx_ + 1); S1(1, idx_ + 1)
            S2a(0, idx_); S2a(1, idx_)
            S2b(0, idx_); S2b(1, idx_)
        for tb in range(2):
            e = ebuf()
            evac(e[:, 0:512], ps[6 + tb][:, :])
            k.dma(HS[h * 128:(h + 1) * 128, tb * 512:(tb + 1) * 512], e[:, 0:512])
    if upto <= 3:
        return k

    k.barrier()
    set_ws([ov(8192 + i * 2048, 8192 + (i + 1) * 2048) for i in range(4)])
    yT = AR[:, :].rearrange("p (c t) -> p c t", t=512)
    gtile = ov(0, 2048).rearrange("p (m t) -> p m t", t=512)
    xres = ov(2048, 4096).rearrange("p (m t) -> p m t", t=512)

    def resid_store(src_res, dst, t0, c0):
        def f(banks):
            k.dma(xres, src_res[t0:t0 + 512, c0:c0 + 512].rearrange("(m p) c -> p m c", p=128))
            e = ebuf().rearrange("p (m t) -> p m t", t=512)
            for m in range(4):
                k.tt(e[:, m, :], banks[m][:, :], xres[:, m, :], ALU.add)
            k.dma(dst[t0:t0 + 512, c0:c0 + 512].rearrange("(m p) c -> p m c", p=128), e)
        return f

    for tb in range(2):
        t0 = tb * 512
        for pi, (hsrc, wsrc, gsrc) in enumerate(((HM, w_pa, GA), (HS, w_pb, GB))):
            for cb in range(8):
                banks = bankset()
                for kq in range(4):
                    at = load_w(hsrc[kq * 512:(kq + 1) * 512, t0:t0 + 512])
                    wt = load_w(wsrc[kq * 512:(kq + 1) * 512, cb * 512:(cb + 1) * 512])
                    for m in range(4):
                        for kc in range(4):
                            kk = kq * 4 + kc
                            k.mm(banks[m][:, :], wt[:, kc, m * 128:(m + 1) * 128], at[:, kc, :], start=(kk == 0), stop=(kk == 15))
                k.dma(gtile, gsrc[cb * 512:(cb + 1) * 512, t0:t0 + 512].rearrange("(m p) t -> p m t", p=128))
                for m in range(4):
                    dsty = yT[:, cb * 4 + m, :]
                    if pi == 0:
                        k.tt(dsty, banks[m][:, :], gtile[:, m, :], ALU.mult)
                    else:
                        k.tt(gtile[:, m, :], banks[m][:, :], gtile[:, m, :], ALU.mult)
                        k.tt(dsty, dsty.bitcast(F32), gtile[:, m, :], ALU.add)
        for cb in range(8):
            proj(yT, 32, w_out, cb * 512, 512, "tm", resid_store(xs[T:S, :], H1, t0, cb * 512))
    if upto <= 4:
        return k

    k.barrier()
    set_ws([ov(12288, 14336), ov(14336, 16384)])
    memT = AR[:, 0:8192].rearrange("p (c t) -> p c t", t=256)
    for i in range(2):
        norm_tile(memx[i * 128:(i + 1) * 128, :], gmem, memT, i * 128)
    for cb in range(8):
        banks = bankset()
        for kq in range(8):
            wt = load_w(w_kv[kq * 512:(kq + 1) * 512, cb * 512:(cb + 1) * 512])
            for m in range(4):
                for kc in range(4):
                    kk = kq * 4 + kc
                    k.mm(banks[m][:, 0:256], wt[:, kc, m * 128:(m + 1) * 128], memT[:, kk, :], start=(kk == 0), stop=(kk == 31))
        e = ebuf().rearrange("p (m t) -> p m t", t=512)
        for m in range(4):
            evac(e[:, m, 0:256], banks[m][:, 0:256])
        k.dma(KX[cb * 512:(cb + 1) * 512, :].rearrange("(m p) t -> p m t", p=128), e[:, :, 0:256])
    for cb in range(8):
        banks = bankset()
        for kq in range(8):
            wt = load_w(w_kv[kq * 512:(kq + 1) * 512, D + cb * 512:D + (cb + 1) * 512])
            for m in range(2):
                for kc in range(4):
                    kk = kq * 4 + kc
                    k.mm(banks[m][:, :], memT[:, kk, m * 128:(m + 1) * 128], wt[:, kc, :], start=(kk == 0), stop=(kk == 31))
        e = ebuf().rearrange("p (m t) -> p m t", t=512)
        for m in range(2):
            evac(e[:, m, :], banks[m][:, :])
        k.dma(VX[:, cb * 512:(cb + 1) * 512].rearrange("(m p) c -> p m c", p=128), e[:, 0:2, :])

    set_ws([ov(14336, 16384)])
    oT = AR[:, :].rearrange("p (c t) -> p c t", t=512)
    qh = ov(0, 4096).rearrange("p (c t) -> p c t", t=512)
    khT = ov(4096, 6144).rearrange("p (c m) -> p c m", m=256)
    vh = ov(6144, 8192).rearrange("p (m d) -> p m d", d=1024)
    pT = ov(8192, 9216).rearrange("p (m t) -> p m t", t=512)
    pbuf = [ov(9216 + i * 256, 9216 + (i + 1) * 256) for i in range(2)]
    xres = ov(12288, 14336).rearrange("p (m t) -> p m t", t=512)
    for tb in range(2):
        t0 = tb * 512
        for i in range(4):
            norm_tile(H1[t0 + i * 128:t0 + (i + 1) * 128, :], gx, xnT, i * 128)
        for cb in range(8):
            proj(xnT, 32, w_q, cb * 512, 512, "fm", store_fm(QX, cb * 512, t0))
        for h in range(4):
            k.dma(qh, QX[h * 1024:(h + 1) * 1024, t0:t0 + 512].rearrange("(c p) t -> p c t", p=128))
            k.dma(khT, KX[h * 1024:(h + 1) * 1024, :].rearrange("(c p) m -> p c m", p=128))
            k.dma(vh, VX[:, h * 1024:(h + 1) * 1024].rearrange("(m p) d -> p m d", p=128))
            for i in range(4):
                psc = ps[i % 2]
                for c in range(8):
                    k.mm(psc[:, 0:256], qh[:, c, i * 128:(i + 1) * 128], khT[:, c, :], start=(c == 0), stop=(c == 7))
                mx = st[:, 32:33]; nb_ = st[:, 33:34]; sm = st[:, 34:35]; rsm = st[:, 35:36]
                k.reduce(mx, psc[:, 0:256], ALU.max)
                k.ts(nb_, mx, -1.0 / 32.0, None, op0=ALU.mult)
                pb_ = pbuf[i % 2]
                k.memset(sm, 0.0)
                k.act(pb_, psc[:, 0:256], AF.Exp, bias=nb_, scale=1.0 / 32.0, accum_out=sm)
                k.op("dve", "reciprocal", [rsm], [sm], rsm, sm)
                k.ts(pb_, pb_, rsm, None, op0=ALU.mult)
                ptp = ps[2 + i % 2]
                for m in range(2):
                    k.transpose(ptp[:, m * 128:(m + 1) * 128], pb_[:, m * 128:(m + 1) * 128], ident[:])
                k.copy(pT[:, :, i * 128:(i + 1) * 128], ptp[:, 0:256].rearrange("p (m t) -> p m t", t=128))
            for dc in range(8):
                pso = ps[4 + dc % 4]
                for m in range(2):
                    k.mm(pso[:, :], vh[:, m, dc * 128:(dc + 1) * 128], pT[:, m, :], start=(m == 0), stop=(m == 1))
                evac(oT[:, h * 8 + dc, :], pso[:, :])
        for cb in range(8):
            proj(oT, 32, w_o, cb * 512, 512, "tm", resid_store(H1, H2, t0, cb * 512))
    if upto <= 5:
        return k

    k.barrier()
    set_ws([ov(10240, 12288)])
    RB = 12288
    RL = ov(RB, RB + 576).rearrange("p (i c) -> p i c", c=72)
    Aasg = ov(RB + 576, RB + 1088).rearrange("p (i c) -> p i c", c=64)
    OH1 = ov(RB + 1088, RB + 1600).rearrange("p (i c) -> p i c", c=64)
    OH2 = ov(RB + 1600, RB + 2112).rearrange("p (i c) -> p i c", c=64)
    POS = ov(RB + 2112, RB + 2624).rearrange("p (i c) -> p i c", c=64)
    rw = ov(RB + 2624, RB + 3136)
    CW = ov(RB + 3136, RB + 3152).rearrange("p (i c) -> p i c", c=2)
    RI = ov(RB + 3152, RB + 3168).bitcast(I32).rearrange("p (i c) -> p i c", c=2)
    hb = ov(RB + 3168, RB + 3680)
    bR = ov(RB + 3680, RB + 3752); erow = ov(RB + 3752, RB + 3816); iota = ov(RB + 3816, RB + 3944); ltpos = ov(RB + 3944, RB + 4072)
    k.dma(bR, b_r); k.dma(erow, c_erow); k.dma(iota, c_iota); k.dma(ltpos, c_ltpos)
    wr = G[:, 0:2304].rearrange("p (kc c) -> p kc c", c=72)
    for tb in range(2):
        for i in range(4):
            r0 = tb * 512 + i * 128
            norm_tile(H2[r0:r0 + 128, :], gmoe, xnT, i * 128, xhat_dst=XH[r0:r0 + 128, :])
        k.dma(wr, w_r.rearrange("(kc p) c -> p kc c", p=128))
        for i in range(4):
            it = tb * 4 + i
            pr = ps[i % 4]
            for kk in range(32):
                k.mm(pr[:, 0:72], xnT[:, kk, i * 128:(i + 1) * 128].bitcast(F32), wr[:, kk, :], start=(kk == 0), stop=(kk == 31))
            k.tt(RL[:, it, :], pr[:, 0:72], bR, ALU.add)
    s_ = lambda a, b_: st[:, a:b_]
    for it in range(8):
        lg = RL[:, it, 0:8]; le = RL[:, it, 8:72]
        m1 = s_(40, 41); nm1 = s_(41, 42); sg = s_(42, 43); gw = s_(43, 44); m1e = s_(44, 45); m2e = s_(45, 46)
        dl = s_(46, 47); w1 = s_(47, 48); w2 = s_(48, 49)
        ohg = rw[:, 0:8]; pen = rw[:, 8:16]; egx = rw[:, 16:24]; lem = rw[:, 64:128]; lem2 = rw[:, 128:192]
        k.reduce(m1, lg, ALU.max)
        k.ts(ohg, lg, m1, None, op0=ALU.is_equal)
        k.ts(nm1, m1, -1.0, None, op0=ALU.mult)
        k.memset(sg, 0.0)
        k.act(egx, lg, AF.Exp, bias=nm1, accum_out=sg)
        k.op("dve", "reciprocal", [gw], [sg], gw, sg)
        k.ts(pen, ohg, 1.0, 1e30, op0=ALU.subtract, op1=ALU.mult)
        k.tt(lem.rearrange("p (g e) -> p g e", e=8), le.rearrange("p (g e) -> p g e", e=8),
             pen.unsqueeze(2).to_broadcast([128, 8, 8]), ALU.add)
        k.reduce(m1e, lem, ALU.max)
        k.ts(OH1[:, it, :], lem, m1e, None, op0=ALU.is_equal)
        k.stt(lem2, OH1[:, it, :], -1e30, lem, ALU.mult, ALU.add)
        k.reduce(m2e, lem2, ALU.max)
        k.ts(OH2[:, it, :], lem2, m2e, None, op0=ALU.is_equal)
        k.tt(dl, m2e, m1e, ALU.subtract)
        k.act(dl, dl, AF.Exp)
        k.ts(w1, dl, 1.0, None, op0=ALU.add)
        k.op("dve", "reciprocal", [w1], [w1], w1, w1)
        k.tt(w2, dl, w1, ALU.mult)
        k.tt(CW[:, it, 0:1], w1, gw, ALU.mult)
        k.tt(CW[:, it, 1:2], w2, gw, ALU.mult)
        k.tt(Aasg[:, it, :], OH1[:, it, :], OH2[:, it, :], ALU.add)
    for it in range(8):
        pp = ps[4 + it % 4]
        for i2 in range(it):
            k.mm(pp[:, 0:64], ones[:], Aasg[:, i2, :], start=(i2 == 0), stop=False)
        k.mm(pp[:, 0:64], ltpos, Aasg[:, it, :], start=(it == 0), stop=True)
        k.copy(POS[:, it, :], pp[:, 0:64])
        t64 = rw[:, 192:256]; t64b = rw[:, 256:320]; rf = s_(50, 51)
        k.tt(t64, POS[:, it, :], erow, ALU.add)
        for kk_, OH in ((0, OH1), (1, OH2)):
            k.tt(t64b, t64, OH[:, it, :], ALU.mult)
            k.reduce(rf, t64b, ALU.add)
            k.copy(RI[:, it, kk_:kk_ + 1], rf)

    tokid = ov(RB + 2624 + 384, RB + 2624 + 392)
    IDX = ov(RB + 2624 + 320, RB + 2624 + 384).bitcast(I32)
    k.dma(tokid, c_tokid)
    SelF = [ov(i * 1024, (i + 1) * 1024).rearrange("p (i s) -> p i s", s=128) for i in range(2)]
    pidx = ps[7]
    for e in range(NEXP):
        sf = SelF[e % 2]
        for it in range(8):
            k.ts(sf[:, it, :], iota, POS[:, it, e:e + 1], Aasg[:, it, e:e + 1], op0=ALU.is_equal, op1=ALU.mult)
        for it in range(8):
            k.mm(pidx[:, e:e + 1], sf[:, it, :], tokid[:, it:it + 1], start=(it == 0), stop=(it == 7))
    k.copy(IDX, pidx[:, 0:64])

    XeTs = [AR[:, i * 4096:(i + 1) * 4096].rearrange("p (c s) -> p c s", s=128) for i in range(2)]
    hbT = AR[:, 8192:8704].rearrange("p (f s) -> p f s", s=128)
    xes = [ov(2048, 6144), ov(6144, 10240)]
    for e in range(NEXP):
        xe = xes[e % 2]
        XeT = XeTs[e % 2]
        k.dma(xe, XH, q="pool", extra_reads=[IDX[:, e:e + 1]], _meth="indirect_dma_start", out_offset=None,
              in_offset=bass.IndirectOffsetOnAxis(ap=IDX[:, e:e + 1], axis=0))
        for c4 in range(8):
            pt = ps[4 + c4 % 3]
            for ci in range(4):
                c = c4 * 4 + ci
                k.transpose(pt[:, ci * 128:(ci + 1) * 128], xe[:, c * 128:(c + 1) * 128], ident[:])
            k.tt(XeT[:, c4 * 4:c4 * 4 + 4, :], pt[:].rearrange("p (a b) -> p a b", b=128),
                 gmoe[:, c4 * 4:c4 * 4 + 4].unsqueeze(2).to_broadcast([128, 4, 128]), ALU.mult)
        pg, pu, pt_ = ps[0], ps[1], ps[2]
        for wsrc, pacc in ((w_g, pg), (w_u, pu)):
            for kq in range(8):
                wt = load_w(wsrc[e * D + kq * 512:e * D + (kq + 1) * 512, :])
                for kc in range(4):
                    kk = kq * 4 + kc
                    k.mm(pacc[:, :], XeT[:, kk, :], wt[:, kc, :], start=(kk == 0), stop=(kk == 31))
        k.act(hb, pg[:, :], AF.Silu)
        k.tt(hb, hb, pu[:, :], ALU.mult)
        for fc in range(4):
            k.transpose(pt_[:, fc * 128:(fc + 1) * 128], hb[:, fc * 128:(fc + 1) * 128], ident[:])
        k.copy(hbT, pt_[:, :].rearrange("p (f s) -> p f s", s=128))
        for dq2 in range(4):
            ee = ebuf()
            for hf in range(2):
                dq = dq2 * 2 + hf
                yb_ = ps[3] if (dq % 2 == 0) else ps[7]
                wd = load_w(w_d[e * 512:(e + 1) * 512, dq * 512:(dq + 1) * 512])
                for fc in range(4):
                    k.mm(yb_[:, :], hbT[:, fc, :], wd[:, fc, :], start=(fc == 0), stop=(fc == 3))
                evac(ee[:, hf * 512:(hf + 1) * 512], yb_[:, :])
            k.dma(YB[e * CAP:(e + 1) * CAP, dq2 * 1024:(dq2 + 1) * 1024], ee[:, 0:1024])

    Y1 = ov(0, 4096); Y2 = ov(4096, 8192); junk = ov(8192, 12288)
    gfin = G[:, 0:4096]; h2t = G[:, 4096:8192]
    k.dma(gfin, g_fin)
    for it in range(8):
        r0 = it * 128
        k.dma(h2t, H2[r0:r0 + 128, :])
        for kk_, Y in ((0, Y1), (1, Y2)):
            k.dma(Y, YB, q="pool", extra_reads=[RI[:, it, kk_:kk_ + 1]], _meth="indirect_dma_start", out_offset=None,
                  in_offset=bass.IndirectOffsetOnAxis(ap=RI[:, it, kk_:kk_ + 1], axis=0))
            k.stt(h2t, Y, CW[:, it, kk_:kk_ + 1], h2t, ALU.mult, ALU.add)
        ss = s_(52, 53); rs = s_(53, 54); tmp = s_(54, 55)
        k.memset(ss, 0.0)
        k.act(junk, h2t, AF.Square, accum_out=ss)
        rstd_from_ss(ss, rs, 1.0 / D, tmp)
        k.ts(h2t, h2t, rs, None, op0=ALU.mult)
        k.tt(h2t, h2t, gfin, ALU.mult)
        k.dma(out[r0:r0 + 128, :], h2t)
    return k


def _consts():
    f = np.float32
    i = np.arange(128)
    c = {}
    c["c_ident"] = np.eye(128, dtype=f)
    c["c_ones"] = np.ones((128, 128), f)
    c["c_ut"] = (i[:, None] > i[None, :]).astype(f)
    c["c_lt"] = (i[:, None] <= i[None, :]).astype(f)
    u = np.arange(896)
    c["c_mm"] = ((u[None, :] - 384) >= i[:, None]).astype(f)
    c["c_ms"] = ((u[None, :] - 384) > i[:, None]).astype(f)
    c["c_ltpos"] = (i[:, None] < i[None, :]).astype(f)
    c["c_iota"] = np.broadcast_to(np.arange(128, dtype=f)[None, :], (128, 128)).copy()
    c["c_tokid"] = (np.arange(8, dtype=f)[None, :] * 128.0 + np.arange(128, dtype=f)[:, None]).copy()
    c["c_erow"] = np.broadcast_to((np.arange(64, dtype=f) * 128.0)[None, :], (128, 64)).copy()
    return c


def _gl(g, n):
    return np.ascontiguousarray(np.asarray(g, np.float32).reshape(n, 128).T)


def make_in_maps(inp, cores=range(8), moe=True):
    f = np.float32
    x = np.asarray(inp["x"], f); mem = np.asarray(inp["mem"], f)
    sh = _consts()
    sh["w_in"] = np.asarray(inp["w_in"], f)[0]
    sh["w_proj_a"] = np.asarray(inp["w_proj_a"], f)[0]
    sh["w_proj_b"] = np.asarray(inp["w_proj_b"], f)[0]
    sh["w_out"] = np.asarray(inp["w_out"], f)[0]
    sh["w_q_mem"] = np.asarray(inp["w_q_mem"], f)[0]
    sh["w_kv_mem"] = np.asarray(inp["w_kv_mem"], f)[0]
    sh["w_o_mem"] = np.asarray(inp["w_o_mem"], f)[0]
    if moe:
        sh["w_gate"] = np.asarray(inp["w_gate"], f)[0].reshape(NEXP * D, 512)
        sh["w_up"] = np.asarray(inp["w_up"], f)[0].reshape(NEXP * D, 512)
        sh["w_down"] = np.asarray(inp["w_down"], f)[0].reshape(NEXP * 512, D)
    sh["w_r"] = np.ascontiguousarray(np.concatenate([np.asarray(inp["w_router_group"], f)[0], np.asarray(inp["w_router_expert"], f)[0]], axis=1))
    br = np.concatenate([np.asarray(inp["b_router_group"], f)[0], np.asarray(inp["b_router_expert"], f)[0]])
    sh["b_r"] = np.broadcast_to(br[None, :], (128, 72)).copy()
    sh["g_mix"] = _gl(inp["norm_mix"][0], 32); sh["g_x"] = _gl(inp["norm_xattn"][0], 32)
    sh["g_mem"] = _gl(inp["norm_mem"][0], 32); sh["g_moe"] = _gl(inp["norm_moe"][0], 32)
    sh["g_fin"] = np.broadcast_to(np.asarray(inp["norm_final"], f)[None, :], (128, D)).copy()
    sh["g_ml"] = _gl(inp["g_mlstm"][0], 16)
    sh["convT"] = np.ascontiguousarray(np.asarray(inp["conv_qk"], f)[0].T)
    bg = np.asarray(inp["b_gates"], f)[0]
    sh["b_i"] = bg[:8].reshape(8, 1).copy(); sh["b_f"] = bg[8:].reshape(8, 1).copy()
    maps = []
    for c in cores:
        b, half = c // 2, c % 2
        xs_ = np.zeros((S, D), f)
        if half == 1:
            xs_[:] = x[b]
        else:
            xs_[T:] = x[b, :T]
        m = dict(sh)
        m["xs"] = xs_
        m["mem"] = np.ascontiguousarray(mem[b])
        maps.append(m)
    return maps


def kernel(**inputs):
    kb = build()
    nc = kb.finish()
    maps = make_in_maps(inputs)
    res = run_bass_kernel_spmd(nc, maps, core_ids=list(range(8)))
    outp = np.zeros((4, S, D), np.float32)
    for c in range(8):
        b, half = c // 2, c % 2
        outp[b, half * T:(half + 1) * T] = res.results[c]["out"]
    return outp
```
